# Optimizing a Trainium2 kernel written in Bass

```python
import math
import jax
import jax.numpy as jnp
from jax import lax
import numpy as np

D_MODEL = 1024
BATCH = 2
SEQ = 8192
DEPTH = 2

LRU_WIDTH = 512
LRU_BLOCKS = 8
LRU_BD = LRU_WIDTH // LRU_BLOCKS
LRU_C = 8.0
CONV_W = 4
DIFF_HEADS = 4
DIFF_HD = 64
DIFF_QBLK = 128
SWA_HEADS = 8
SWA_KV_HEADS = 2
SWA_GROUP = SWA_HEADS // SWA_KV_HEADS
SWA_HD = 64
WINDOW = 128
SWA_BLK = WINDOW
N_BRANCHES = 3
BRANCH_WIDTH = 512
REL_BUCKETS = 32
REL_MAX_DIST = 128
N_ATTN_HEADS = DIFF_HEADS + SWA_HEADS
N_EXPERTS = 32
TOP_K = 4
D_FF = 1024
SWIGLU_LIMIT = 7.0
SWIGLU_ALPHA = 1.702
MOE_BLK = 128
NORM_EPS = 1e-6
NEG_INF = -1e30

IN_SPLITS = (LRU_WIDTH, LRU_WIDTH,
             DIFF_HEADS * 2 * DIFF_HD, DIFF_HEADS * 2 * DIFF_HD, DIFF_HEADS * 2 * DIFF_HD,
             SWA_HEADS * SWA_HD, SWA_KV_HEADS * SWA_HD, SWA_KV_HEADS * SWA_HD,
             N_BRANCHES * D_MODEL)
IN_WIDTH = sum(IN_SPLITS)

kernel_name = 'hybrid_rglru_diffattn_swa_moe_block'


def rms_norm(x, gain):
    xf = x.astype(jnp.float32)
    y = xf * lax.rsqrt(jnp.mean(xf * xf, axis=-1, keepdims=True) + NORM_EPS)
    return (y * gain.astype(jnp.float32)).astype(x.dtype)


def t5_bucket(dist):
    exact = REL_BUCKETS // 2
    is_small = dist < exact
    log_ratio = jnp.log(jnp.maximum(dist, 1).astype(jnp.float32) / exact) / math.log(REL_MAX_DIST / exact)
    large = exact + (log_ratio * (REL_BUCKETS - exact)).astype(jnp.int32)
    return jnp.where(is_small, dist, jnp.minimum(large, REL_BUCKETS - 1))


def rglru_branch(xr, gr, conv_w, conv_b, wa, ba, wx, bx, lam):
    B, S, C = xr.shape
    xp = jnp.pad(xr, ((0, 0), (CONV_W - 1, 0), (0, 0)))
    xc = conv_b + sum(xp[:, tap:tap + S] * conv_w[tap] for tap in range(CONV_W))
    xh = xc.reshape(B, S, LRU_BLOCKS, LRU_BD)
    gate_a = jnp.einsum('bshi,hij->bshj', xh, wa).reshape(B, S, C) + ba
    gate_x = jnp.einsum('bshi,hij->bshj', xh, wx).reshape(B, S, C) + bx
    r = jax.nn.sigmoid(gate_a.astype(jnp.float32))
    i = jax.nn.sigmoid(gate_x.astype(jnp.float32))
    log_a = -LRU_C * r * jax.nn.softplus(-lam.astype(jnp.float32))
    a = jnp.exp(log_a)
    mult = jnp.sqrt(-jnp.expm1(2.0 * log_a))
    b = xc.astype(jnp.float32) * i * mult

    def combine(left, right):
        a1, b1 = left
        a2, b2 = right
        return a1 * a2, a2 * b1 + b2

    _, h = lax.associative_scan(combine, (a, b), axis=1)
    return (h * jax.nn.gelu(gr.astype(jnp.float32), approximate=True)).astype(xr.dtype)


def diff_attention(q, k, v, q_gain, k_gain, lam_vecs, subln_gain, bias_table, lambda_init):
    B, S = q.shape[:2]
    nb = S // DIFF_QBLK
    q = rms_norm(q, q_gain)
    k = rms_norm(k, k_gain)
    lv = lam_vecs.astype(jnp.float32)
    lam = jnp.exp(jnp.sum(lv[0] * lv[1])) - jnp.exp(jnp.sum(lv[2] * lv[3])) + lambda_init
    bias_by_dist = bias_table[t5_bucket(jnp.arange(S))][:, :DIFF_HEADS].astype(jnp.float32)
    k_pos = jnp.arange(S)
    q_blocks = jnp.moveaxis(q.reshape(B, nb, DIFF_QBLK, DIFF_HEADS, 2, DIFF_HD), 1, 0)

    def attend_block(args):
        q_blk, blk = args
        dist = (blk * DIFF_QBLK + jnp.arange(DIFF_QBLK))[:, None] - k_pos[None, :]
        bias = jnp.moveaxis(bias_by_dist[jnp.maximum(dist, 0)], -1, 0)
        s = jnp.einsum('bqhmd,bkhmd->bhmqk', q_blk, k).astype(jnp.float32) * DIFF_HD ** -0.5
        s = jnp.where(dist >= 0, s + bias[None, :, None], NEG_INF)
        p = jax.nn.softmax(s, axis=-1)
        attn = (p[:, :, 0] - lam * p[:, :, 1]).astype(v.dtype)
        return jnp.einsum('bhqk,bkhe->bqhe', attn, v)

    o = lax.map(attend_block, (q_blocks, jnp.arange(nb)))
    o = jnp.moveaxis(o, 0, 1).reshape(B, S, DIFF_HEADS, 2 * DIFF_HD)
    o = rms_norm(o, subln_gain) * (1.0 - lambda_init)
    return o.reshape(B, S, DIFF_HEADS * 2 * DIFF_HD)


def swa_attention(q, k, v, q_gain, k_gain, sinks, bias_table):
    B, S = q.shape[:2]
    nb = S // SWA_BLK
    q = rms_norm(q, q_gain).reshape(B, nb, SWA_BLK, SWA_KV_HEADS, SWA_GROUP, SWA_HD)
    k = rms_norm(k, k_gain).reshape(B, nb, SWA_BLK, SWA_KV_HEADS, SWA_HD)
    v = v.reshape(B, nb, SWA_BLK, SWA_KV_HEADS, SWA_HD)

    def band(t):
        prev = jnp.pad(t, ((0, 0), (1, 0), (0, 0), (0, 0), (0, 0)))[:, :-1]
        return jnp.concatenate([prev, t], axis=2)

    k_band, v_band = band(k), band(v)
    qi = jnp.arange(SWA_BLK)[:, None]
    kj = jnp.arange(2 * SWA_BLK)[None, :]
    dist = SWA_BLK + qi - kj
    in_window = (dist >= 0) & (dist < WINDOW)
    has_prev = (jnp.arange(nb) > 0)[:, None, None] | (kj >= SWA_BLK)[None]
    mask = in_window[None] & has_prev
    bias = bias_table[t5_bucket(jnp.clip(dist, 0, WINDOW - 1))][..., DIFF_HEADS:].astype(jnp.float32)
    bias = bias.reshape(SWA_BLK, 2 * SWA_BLK, SWA_KV_HEADS, SWA_GROUP).transpose(2, 3, 0, 1)
    s = jnp.einsum('bnqhgd,bnkhd->bnhgqk', q, k_band).astype(jnp.float32) * SWA_HD ** -0.5 + bias
    s = jnp.where(mask[None, :, None, None], s, NEG_INF)
    sink = jnp.broadcast_to(sinks.astype(jnp.float32).reshape(SWA_KV_HEADS, SWA_GROUP, 1, 1),
                            s.shape[:-1] + (1,))
    p = jax.nn.softmax(jnp.concatenate([s, sink], axis=-1), axis=-1)[..., :-1]
    o = jnp.einsum('bnhgqk,bnkhd->bnqhgd', p.astype(v.dtype), v_band)
    return o.reshape(B, S, SWA_HEADS * SWA_HD)


def hybrid_mixer(h, w_in, conv_w, conv_b, lru_wa, lru_ba, lru_wx, lru_bx, lru_lambda,
                 diff_qnorm, diff_knorm, diff_lambda, diff_subln,
                 swa_qnorm, swa_knorm, swa_sinks, rel_bias, w_branch, w_out, lambda_init):
    B, S, D = h.shape
    proj = h @ w_in
    offsets = []
    acc = 0
    for width in IN_SPLITS[:-1]:
        acc += width
        offsets.append(acc)
    xr, gr, dq, dk, dv, sq, sk, sv, gl = jnp.split(proj, offsets, axis=-1)
    o_lru = rglru_branch(xr, gr, conv_w, conv_b, lru_wa, lru_ba, lru_wx, lru_bx, lru_lambda)
    o_diff = diff_attention(dq.reshape(B, S, DIFF_HEADS, 2, DIFF_HD),
                            dk.reshape(B, S, DIFF_HEADS, 2, DIFF_HD),
                            dv.reshape(B, S, DIFF_HEADS, 2 * DIFF_HD),
                            diff_qnorm, diff_knorm, diff_lambda, diff_subln, rel_bias, lambda_init)
    o_swa = swa_attention(sq.reshape(B, S, SWA_HEADS, SWA_HD),
                          sk.reshape(B, S, SWA_KV_HEADS, SWA_HD),
                          sv.reshape(B, S, SWA_KV_HEADS, SWA_HD),
                          swa_qnorm, swa_knorm, swa_sinks, rel_bias)
    branches = jnp.stack([o_lru, o_diff, o_swa], axis=2)
    branch_proj = jnp.einsum('bsnc,ncd->bsnd', branches, w_branch)
    gates = jax.nn.sigmoid(gl).reshape(B, S, N_BRANCHES, D)
    merged = jnp.einsum('bsnd,bsnd->bsd', gates, branch_proj)
    return merged @ w_out


def moe_ffn(h, w_router, b_router, w1, b1, w2, b2):
    B, S, D = h.shape
    T = B * S
    xt = h.reshape(T, D)
    logits = xt.astype(jnp.float32) @ w_router.astype(jnp.float32) + b_router.astype(jnp.float32)
    top_v, top_e = lax.top_k(logits, TOP_K)
    weights = jax.nn.softmax(top_v, axis=-1).astype(xt.dtype)
    n_pairs = T * TOP_K
    flat_e = top_e.reshape(-1)
    order = jnp.argsort(flat_e)
    sorted_e = flat_e[order]
    counts = jnp.bincount(flat_e, length=N_EXPERTS)
    starts = jnp.cumsum(counts) - counts
    padded = (counts + MOE_BLK - 1) // MOE_BLK * MOE_BLK
    pad_ends = jnp.cumsum(padded)
    pad_starts = pad_ends - padded
    dest_sorted = pad_starts[sorted_e] + (jnp.arange(n_pairs) - starts[sorted_e])
    n_rows = (n_pairs + MOE_BLK - 1) // MOE_BLK * MOE_BLK + N_EXPERTS * MOE_BLK
    n_blocks = n_rows // MOE_BLK
    row_token = jnp.full((n_rows,), T, jnp.int32).at[dest_sorted].set((order // TOP_K).astype(jnp.int32))
    dest = jnp.zeros_like(order).at[order].set(dest_sorted)
    block_expert = jnp.minimum(jnp.searchsorted(pad_ends, jnp.arange(n_blocks) * MOE_BLK, side='right'),
                               N_EXPERTS - 1)
    x_pad = jnp.concatenate([xt, jnp.zeros((1, D), xt.dtype)], axis=0)

    def expert_block(args):
        tok, e = args
        hcat = x_pad[tok] @ w1[e] + b1[e]
        x_glu = jnp.minimum(hcat[:, ::2], SWIGLU_LIMIT)
        x_lin = jnp.clip(hcat[:, 1::2], -SWIGLU_LIMIT, SWIGLU_LIMIT)
        act = x_glu * jax.nn.sigmoid(SWIGLU_ALPHA * x_glu) * (x_lin + 1.0)
        return act @ w2[e] + b2[e]

    y = lax.map(expert_block, (row_token.reshape(n_blocks, MOE_BLK), block_expert)).reshape(n_rows, D)
    out = jnp.sum(y[dest].reshape(T, TOP_K, D) * weights[..., None], axis=1)
    return out.reshape(B, S, D)


def setup_inputs(seed: int = 0) -> dict:
    key = jax.random.key(seed)
    keys = jax.random.split(key, 40)
    counter = [0]

    def next_key():
        k = keys[counter[0]]
        counter[0] += 1
        return k

    def nrm(shape, scale):
        return jax.random.normal(next_key(), shape, jnp.float32) * scale

    L = DEPTH
    D = D_MODEL
    x = nrm((BATCH, SEQ, D), 1.0)
    c = nrm((BATCH, D), 1.0)
    w_ada = nrm((L, D, 6 * D), 0.5 * D ** -0.5)
    b_ada = nrm((L, 6 * D), 0.02)
    norm_mix = 1.0 + nrm((L, D), 0.02)
    norm_ffn = 1.0 + nrm((L, D), 0.02)
    w_in = nrm((L, D, IN_WIDTH), D ** -0.5)
    conv_w = nrm((L, CONV_W, LRU_WIDTH), CONV_W ** -0.5)
    conv_b = nrm((L, LRU_WIDTH), 0.02)
    lru_wa = nrm((L, LRU_BLOCKS, LRU_BD, LRU_BD), LRU_BD ** -0.5)
    lru_ba = nrm((L, LRU_WIDTH), 0.02)
    lru_wx = nrm((L, LRU_BLOCKS, LRU_BD, LRU_BD), LRU_BD ** -0.5)
    lru_bx = nrm((L, LRU_WIDTH), 0.02)
    radius = jax.random.uniform(next_key(), (L, LRU_WIDTH), jnp.float32, 0.9, 0.999)
    lru_lambda = -jnp.log(jnp.expm1(-jnp.log(radius) / LRU_C))
    diff_qnorm = 1.0 + nrm((L, DIFF_HD), 0.02)
    diff_knorm = 1.0 + nrm((L, DIFF_HD), 0.02)
    diff_lambda = nrm((L, 4, DIFF_HD), 0.1)
    diff_subln = 1.0 + nrm((L, 2 * DIFF_HD), 0.02)
    swa_qnorm = 1.0 + nrm((L, SWA_HD), 0.02)
    swa_knorm = 1.0 + nrm((L, SWA_HD), 0.02)
    swa_sinks = nrm((L, SWA_HEADS), 0.5)
    rel_bias = nrm((REL_BUCKETS, N_ATTN_HEADS), 0.5)
    w_branch = nrm((L, N_BRANCHES, BRANCH_WIDTH, D), BRANCH_WIDTH ** -0.5)
    w_out = nrm((L, D, D), D ** -0.5)
    w_router = nrm((L, D, N_EXPERTS), D ** -0.5)
    b_router = nrm((L, N_EXPERTS), 0.01)
    w1 = nrm((L, N_EXPERTS, D, 2 * D_FF), D ** -0.5)
    b1 = nrm((L, N_EXPERTS, 2 * D_FF), 0.02)
    w2 = nrm((L, N_EXPERTS, D_FF, D), D_FF ** -0.5)
    b2 = nrm((L, N_EXPERTS, D), 0.02)
    return {'x': x, 'c': c, 'w_ada': w_ada, 'b_ada': b_ada, 'norm_mix': norm_mix, 'norm_ffn': norm_ffn,
            'w_in': w_in, 'conv_w': conv_w, 'conv_b': conv_b, 'lru_wa': lru_wa, 'lru_ba': lru_ba,
            'lru_wx': lru_wx, 'lru_bx': lru_bx, 'lru_lambda': lru_lambda, 'diff_qnorm': diff_qnorm,
            'diff_knorm': diff_knorm, 'diff_lambda': diff_lambda, 'diff_subln': diff_subln,
            'swa_qnorm': swa_qnorm, 'swa_knorm': swa_knorm, 'swa_sinks': swa_sinks, 'rel_bias': rel_bias,
            'w_branch': w_branch, 'w_out': w_out, 'w_router': w_router, 'b_router': b_router,
            'w1': w1, 'b1': b1, 'w2': w2, 'b2': b2}


def reference(x, c, w_ada, b_ada, norm_mix, norm_ffn, w_in, conv_w, conv_b, lru_wa, lru_ba,
              lru_wx, lru_bx, lru_lambda, diff_qnorm, diff_knorm, diff_lambda, diff_subln,
              swa_qnorm, swa_knorm, swa_sinks, rel_bias, w_branch, w_out, w_router, b_router,
              w1, b1, w2, b2):
    cond = jax.nn.silu(c)
    for layer in range(DEPTH):
        mod = cond @ w_ada[layer] + b_ada[layer]
        sh_m, sc_m, g_m, sh_f, sc_f, g_f = [m[:, None, :] for m in jnp.split(mod, 6, axis=-1)]
        lambda_init = 0.8 - 0.6 * math.exp(-0.3 * layer)
        h = rms_norm(x, norm_mix[layer]) * (1.0 + sc_m) + sh_m
        x = x + g_m * hybrid_mixer(h, w_in[layer], conv_w[layer], conv_b[layer], lru_wa[layer],
                                   lru_ba[layer], lru_wx[layer], lru_bx[layer], lru_lambda[layer],
                                   diff_qnorm[layer], diff_knorm[layer], diff_lambda[layer],
                                   diff_subln[layer], swa_qnorm[layer], swa_knorm[layer],
                                   swa_sinks[layer], rel_bias, w_branch[layer], w_out[layer],
                                   lambda_init)
        h = rms_norm(x, norm_ffn[layer]) * (1.0 + sc_f) + sh_f
        x = x + g_f * moe_ffn(h, w_router[layer], b_router[layer], w1[layer], b1[layer],
                              w2[layer], b2[layer])
    return x
```

```python
import contextlib
import math
import numpy as np
import ml_dtypes
import concourse.bass as bass
import concourse.mybir as mybir
from concourse.bass_utils import run_bass_kernel_spmd

F32 = mybir.dt.float32
BF16 = mybir.dt.bfloat16
AF = mybir.ActivationFunctionType
ALU = mybir.AluOpType
AX = mybir.AxisListType

D = 1024
NEXP = 32
DFF = 1024
EPS = 1e-6
NEG = -30000.0


class Buf:
    __slots__ = ("w", "r")

    def __init__(self):
        self.w = None
        self.r = {}


class KB:
    def __init__(self):
        self.nc = bass.Bass("TRN2", target_bir_lowering=False)
        self.es = contextlib.ExitStack()
        nc = self.nc
        self.eng = {"pe": nc.tensor, "act": nc.scalar, "dve": nc.vector, "pool": nc.gpsimd, "sp": nc.sync}
        self.sems = []
        self.esem = {}
        self.cnt = {}
        for e in self.eng:
            self.esem[e] = self._newsem("e_" + e)
            self.cnt[e] = 0
        self.seen = {e: {} for e in self.eng}
        self.ring = {}
        self.rpos = {}
        for q, n in (("sp", 12), ("act", 4), ("pool", 8)):
            self.ring[q] = [[self._newsem("d_%s%d" % (q, i)), 0] for i in range(n)]
            self.rpos[q] = 0
        self.out_tickets = []
        self.stack = [self.es]

    def push(self):
        self.stack.append(contextlib.ExitStack())

    def pop(self):
        self.barrier()
        self.stack.pop().close()

    def barrier(self):
        deps = []
        for e in self.eng:
            if self.cnt[e] > 0:
                deps.append((self.esem[e], self.cnt[e]))
        for q in self.ring:
            for slot in self.ring[q]:
                if slot[1] > 0:
                    deps.append((slot[0], slot[1]))
        for e in self.eng:
            self._wait(e, deps)

    def _newsem(self, name):
        s = self.es.enter_context(self.nc.semaphore(name))
        self.sems.append(s)
        return len(self.sems) - 1

    def sb(self, name, shape, dt):
        return self.stack[-1].enter_context(self.nc.sbuf_tensor(name, list(shape), dt))

    def ps(self, name, shape, dt=F32):
        return self.es.enter_context(self.nc.psum_tensor(name, list(shape), dt))

    def dram(self, name, shape, dt, kind):
        return self.nc.dram_tensor(name, list(shape), dt, kind=kind).ap()

    def _wait(self, e, deps):
        h = self.eng[e]
        best = {}
        for (s, v) in deps:
            if best.get(s, 0) < v:
                best[s] = v
        for s, v in best.items():
            if s == self.esem[e] and e in ("pe", "sp"):
                continue
            if self.seen[e].get(s, 0) < v:
                h.wait_ge(self.sems[s], v)
                self.seen[e][s] = v

    def _deps(self, reads, writes):
        deps = []
        for b in reads:
            if b.w is not None:
                deps.append(b.w)
        for b in writes:
            if b.w is not None:
                deps.append(b.w)
            deps.extend(b.r.items())
        return deps

    def _mark(self, t, reads, writes):
        for b in reads:
            if b.r.get(t[0], 0) < t[1]:
                b.r[t[0]] = t[1]
        for b in writes:
            b.w = t
            b.r = {}

    def op(self, e, fn, reads=(), writes=()):
        self._wait(e, self._deps(reads, writes))
        ins = fn(self.eng[e])
        self.cnt[e] += 1
        ins.then_inc(self.sems[self.esem[e]], 1)
        t = (self.esem[e], self.cnt[e])
        self._mark(t, reads, writes)
        return t

    def dma(self, q, out, in_, reads=(), writes=(), is_output=False, **kw):
        slot = self.ring[q][self.rpos[q]]
        self.rpos[q] = (self.rpos[q] + 1) % len(self.ring[q])
        deps = self._deps(reads, writes)
        if slot[1] > 0:
            deps.append((slot[0], slot[1]))
        self._wait(q, deps)
        ins = self.eng[q].dma_start(out=out, in_=in_, **kw)
        slot[1] += 16
        ins.then_inc(self.sems[slot[0]], 16)
        t = (slot[0], slot[1])
        self._mark(t, reads, writes)
        if is_output:
            self.out_tickets.append(t)
        return t

    def finish(self):
        deps = list(self.out_tickets)
        for e in self.eng:
            if self.cnt[e] > 0:
                deps.append((self.esem[e], self.cnt[e]))
        for q in self.ring:
            for slot in self.ring[q]:
                if slot[1] > 0:
                    deps.append((slot[0], slot[1]))
        self._wait("sp", deps)
        self.es.close()
        return self.nc


def groups_of(total, g):
    out = []
    s = 0
    while s < total:
        n = min(g, total - s)
        out.append((s, n))
        s += n
    return out


def build_mixer(mode, T, stop_after=None, debug=False):
    k = KB()
    NBLK = T // 128
    TT = T + 128
    NPB = 3 * NBLK
    G = min(512, T)
    full = mode == "B"
    og = groups_of(T, G)
    own_groups = [(128 + s, n) for (s, n) in og]
    tgroups = [(0, 128)] + own_groups

    d_xT = k.dram("xT", [128, 8, TT], F32, "ExternalInput")
    d_cv = k.dram("cvec", [128, 8], F32, "ExternalInput")
    NMOD = 48 if full else 16
    d_wada = k.dram("wada", [NMOD // 4, 128, 8, 512], F32, "ExternalInput")
    d_bada = k.dram("bada", [128, 48], F32, "ExternalInput")
    d_nmix = k.dram("nmix", [128, 8], F32, "ExternalInput")
    d_win = k.dram("win", [7, 128, 8, 512], F32, "ExternalInput")
    d_convw = k.dram("convw", [128, 4, 4], F32, "ExternalInput")
    d_lrus = k.dram("lrus", [128, 4, 4], F32, "ExternalInput")
    d_wlru = k.dram("wlru", [128, 2, 512], F32, "ExternalInput")
    d_qkg = k.dram("qkg", [128, 4], F32, "ExternalInput")
    d_flag = k.dram("flag", [128, 1], F32, "ExternalInput")
    d_id = k.dram("ident", [128, 128], F32, "ExternalInput")
    d_bd = k.dram("bdones", [128, 128], F32, "ExternalInput")
    if full:
        d_nffn = k.dram("nffn", [128, 8], F32, "ExternalInput")
        d_kprev = k.dram("kprev", [4, 128, NPB * 128], BF16, "ExternalInput")
        d_vprev = k.dram("vprev", [4, 128, NPB, 130], BF16, "ExternalInput")
        d_carry = k.dram("carry", [128, 4, 3, 2], F32, "ExternalInput")
        d_dbias = k.dram("dbias", [4, 128, 2, 2, 128], F32, "ExternalInput")
        d_b31 = k.dram("b31", [128, 4], F32, "ExternalInput")
        d_sbias = k.dram("sbias", [128, 8, 2, 128], F32, "ExternalInput")
        d_sink = k.dram("sinks", [128, 8], F32, "ExternalInput")
        d_dlam = k.dram("dlam", [128, 4, 64], F32, "ExternalInput")
        d_subln = k.dram("subln", [128, 128], F32, "ExternalInput")
        d_wgl = k.dram("wgl", [8, 128, 8, 384], F32, "ExternalInput")
        d_wbr = k.dram("wbr", [8, 128, 12, 128], F32, "ExternalInput")
        d_wout = k.dram("wout", [2, 128, 8, 512], F32, "ExternalInput")
        d_wr = k.dram("wr", [128, 8, 32], F32, "ExternalInput")
        d_br = k.dram("br", [128, 32], F32, "ExternalInput")
        d_lamc = k.dram("lamc", [128, 2], F32, "ExternalInput")
        if debug:
            o_dbg = k.dram("o_dbg", [128, 12, T], BF16, "ExternalOutput")
        o_xT = k.dram("o_xT", [128, 8, T], F32, "ExternalOutput")
        o_hT = k.dram("o_hT", [128, 8, T], BF16, "ExternalOutput")
        o_rw = k.dram("o_rw", [T, 32], F32, "ExternalOutput")
        o_gf = k.dram("o_gf", [128, 8], F32, "ExternalOutput")
    else:
        o_k = k.dram("o_k", [4, 128, T], BF16, "ExternalOutput")
        o_v = k.dram("o_v", [4, 128, NBLK, 130], BF16, "ExternalOutput")
        o_c = k.dram("o_c", [128, 4, 2], F32, "ExternalOutput")

    hT = k.sb("hT_s", [128, 8, TT], BF16); b_hT = Buf()
    cst = k.sb("cst", [128, 64], F32); b_cst = Buf()
    ident = k.sb("ident_s", [128, 128], F32); b_id = Buf()
    identb = k.sb("identb", [128, 128], BF16)
    bdones = k.sb("bdones_s", [128, 128], BF16); b_bd = Buf()
    ones = k.sb("ones_s", [128, 128], BF16); b_ones = Buf()
    mod = k.sb("mod", [128, 48], F32); b_mod = Buf()
    sm = k.sb("small", [128, 16], F32); b_sm = Buf()
    wst = k.sb("wst", [128, 8, 512], F32); b_wst = Buf()
    wbf = [k.sb("wbf%d" % i, [128, 8, 512], BF16) for i in range(3)]; b_wbf = [Buf(), Buf(), Buf()]
    sq = k.sb("sqs", [128, 8, G], BF16); b_sq = Buf()
    rstd = k.sb("rstd", [128, G], F32); b_rstd = Buf()
    tmpf = [k.sb("tmpf%d" % i, [128, G], F32) for i in range(2)]; b_tmpf = [Buf(), Buf()]
    tmpb = [k.sb("tmpb%d" % i, [128, G], BF16) for i in range(2)]; b_tmpb = [Buf(), Buf()]
    lsm = k.sb("lsm", [128, 16], F32); b_lsm = Buf()
    if full:
        obr = k.sb("obr", [128, 12, T], BF16); b_obr = Buf()
    pbank = [k.ps("pb%d" % i, [128, 512]) for i in range(8)]
    b_pb = [Buf() for _ in range(8)]

    k.dma("sp", ident[:], d_id, writes=[b_id])
    k.dma("sp", tmpf[0][:, 0:128], d_bd, writes=[b_tmpf[0]])
    k.op("dve", lambda e: e.tensor_copy(out=bdones[:], in_=tmpf[0][:, 0:128]), reads=[b_tmpf[0]], writes=[b_bd])
    k.op("dve", lambda e: e.memset(ones[:], 1.0), writes=[b_ones])
    k.op("dve", lambda e: e.tensor_copy(out=identb[:], in_=ident[:]), reads=[b_id], writes=[b_id])
    k.dma("sp", cst[:, 0:8], d_cv, writes=[b_cst])
    k.dma("sp", cst[:, 8:16], d_nmix, writes=[b_cst])
    k.dma("sp", cst[:, 24:40], d_convw.rearrange("p a b -> p (a b)"), writes=[b_cst])
    k.dma("sp", cst[:, 40:56], d_lrus.rearrange("p a b -> p (a b)"), writes=[b_cst])
    k.dma("sp", cst[:, 56:60], d_qkg, writes=[b_cst])
    k.dma("sp", cst[:, 60:61], d_flag, writes=[b_cst])
    k.dma("sp", mod[:], d_bada, writes=[b_mod])
    if full:
        k.dma("sp", cst[:, 16:24], d_nffn, writes=[b_cst])
    k.op("dve", lambda e: e.memset(cst[:, 61:62], EPS), writes=[b_cst])
    k.op("dve", lambda e: e.memset(cst[:, 62:63], 1.0), writes=[b_cst])
    convw = lambda c, tap: cst[:, 24 + c * 4 + tap: 25 + c * 4 + tap]
    lrus = lambda c, i: cst[:, 40 + c * 4 + i: 41 + c * 4 + i]
    epsc = cst[:, 61:62]
    onec = cst[:, 62:63]
    flag = cst[:, 60:61]

    k.op("act", lambda e: e.activation(out=cst[:, 0:8], in_=cst[:, 0:8], func=AF.Silu), reads=[b_cst], writes=[b_cst])
    for blk in range(NMOD // 4):
        k.dma("sp", wst[:], d_wada[blk], writes=[b_wst])
        pb = pbank[blk % 2]
        for fi in range(4):
            for kc in range(8):
                k.op("pe", lambda e, fi=fi, kc=kc: e.matmul(pb[:, fi:fi + 1], lhsT=wst[:, kc, fi * 128:(fi + 1) * 128],
                                                           rhs=cst[:, kc:kc + 1], start=(kc == 0), stop=(kc == 7)),
                     reads=[b_wst, b_cst], writes=[b_pb[blk % 2]])
        f0 = blk * 4
        k.op("dve", lambda e: e.tensor_tensor(out=mod[:, f0:f0 + 4], in0=pb[:, 0:4], in1=mod[:, f0:f0 + 4], op=ALU.add),
             reads=[b_pb[blk % 2], b_mod], writes=[b_mod])
    k.op("dve", lambda e: e.scalar_tensor_tensor(out=sm[:, 0:8], in0=mod[:, 8:16], scalar=1.0, in1=cst[:, 8:16],
                                                 op0=ALU.add, op1=ALU.mult), reads=[b_mod, b_cst], writes=[b_sm])
    if full:
        k.op("dve", lambda e: e.scalar_tensor_tensor(out=sm[:, 8:16], in0=mod[:, 32:40], scalar=1.0, in1=cst[:, 16:24],
                                                     op0=ALU.add, op1=ALU.mult), reads=[b_mod, b_cst], writes=[b_sm])

    def norm_mod(src, b_src, n, Acol, Bcol, dst_fn, b_dst, inplace_f32=False):
        for c in range(8):
            k.op("act", lambda e, c=c: e.activation(out=sq[:, c, 0:n], in_=src[:, c, 0:n], func=AF.Square),
                 reads=[b_src], writes=[b_sq])
        pb = pbank[2]
        for c in range(8):
            k.op("pe", lambda e, c=c: e.matmul(pb[:, 0:n], lhsT=ones[:], rhs=sq[:, c, 0:n], start=(c == 0), stop=(c == 7)),
                 reads=[b_sq, b_ones], writes=[b_pb[2]])
        k.op("act", lambda e: e.activation(out=rstd[:, 0:n], in_=pb[:, 0:n], func=AF.Sqrt, scale=1.0 / D, bias=epsc),
             reads=[b_pb[2], b_cst], writes=[b_rstd])
        k.op("dve", lambda e: e.reciprocal(out=rstd[:, 0:n], in_=rstd[:, 0:n]), reads=[b_rstd], writes=[b_rstd])
        for c in range(8):
            ti = c % 2
            k.op("dve", lambda e, c=c, ti=ti: e.scalar_tensor_tensor(out=tmpf[ti][:, 0:n], in0=src[:, c, 0:n],
                                                                    scalar=sm[:, Acol + c:Acol + c + 1], in1=rstd[:, 0:n],
                                                                    op0=ALU.mult, op1=ALU.mult),
                 reads=[b_src, b_sm, b_rstd], writes=[b_tmpf[ti]])
            if inplace_f32:
                k.op("act", lambda e, c=c, ti=ti: e.activation(out=src[:, c, 0:n], in_=tmpf[ti][:, 0:n], func=AF.Identity,
                                                               bias=mod[:, Bcol + c:Bcol + c + 1]),
                     reads=[b_tmpf[ti], b_mod], writes=[b_src])
                k.op("dve", lambda e, c=c: e.tensor_copy(out=dst_fn(c), in_=src[:, c, 0:n]), reads=[b_src], writes=[b_dst])
            else:
                k.op("act", lambda e, c=c, ti=ti: e.activation(out=dst_fn(c), in_=tmpf[ti][:, 0:n], func=AF.Identity,
                                                               bias=mod[:, Bcol + c:Bcol + c + 1]),
                     reads=[b_tmpf[ti], b_mod], writes=[b_dst])

    k.push()
    xg = k.sb("xg", [128, 8, 512], F32); b_xg = Buf()
    for (s, n) in tgroups:
        k.dma("sp", xg[:, :, 0:n], d_xT[:, :, s:s + n], writes=[b_xg])
        norm_mod(xg, b_xg, n, 0, 0, lambda c, s=s, n=n: hT[:, c, s:s + n], b_hT)
    k.pop()

    def load_w(slot, src, rows=8, cols=512):
        k.dma("sp", wst[:, 0:rows, 0:cols], src, writes=[b_wst])
        k.op("pool", lambda e: e.tensor_copy(out=wbf[slot][:, 0:rows, 0:cols], in_=wst[:, 0:rows, 0:cols]),
             reads=[b_wst], writes=[b_wbf[slot]])
        return wbf[slot], b_wbf[slot]

    pstate = {"i": 0}

    def next_pb():
        i = 4 + (pstate["i"] % 4)
        pstate["i"] += 1
        return pbank[i], b_pb[i]

    def linear_fm(w, b_w, chunks, src, b_src, KC, groups, evac):
        for (s, n) in groups:
            for nci in chunks:
                pb, bpb = next_pb()
                for kc in range(KC):
                    k.op("pe", lambda e, kc=kc, nci=nci: e.matmul(pb[:, 0:n], lhsT=w[:, kc, nci * 128:(nci + 1) * 128],
                                                                 rhs=src[:, kc, s:s + n], start=(kc == 0), stop=(kc == KC - 1)),
                         reads=[b_w, b_src], writes=[bpb])
                evac(nci, s, n, pb, bpb)

    def qknorm(dst_fn, b_dst, gcol):
        def ev(nci, s, n, pb, bpb):
            ti = nci % 2
            k.op("act", lambda e: e.activation(out=tmpb[ti][:, 0:n], in_=pb[:, 0:n], func=AF.Square),
                 reads=[bpb], writes=[b_tmpb[ti]])
            k.op("act", lambda e: e.activation(out=tmpf[ti][:, 0:n], in_=pb[:, 0:n], func=AF.Identity),
                 reads=[bpb], writes=[b_tmpf[ti]])
            p2 = pbank[3]
            k.op("pe", lambda e: e.matmul(p2[:, 0:n], lhsT=bdones[:], rhs=tmpb[ti][:, 0:n], start=True, stop=True),
                 reads=[b_bd, b_tmpb[ti]], writes=[b_pb[3]])
            k.op("act", lambda e: e.activation(out=rstd[:, 0:n], in_=p2[:, 0:n], func=AF.Sqrt, scale=1.0 / 64, bias=epsc),
                 reads=[b_pb[3], b_cst], writes=[b_rstd])
            k.op("dve", lambda e: e.reciprocal(out=rstd[:, 0:n], in_=rstd[:, 0:n]), reads=[b_rstd], writes=[b_rstd])
            k.op("dve", lambda e: e.scalar_tensor_tensor(out=dst_fn(nci, s, n), in0=tmpf[ti][:, 0:n],
                                                         scalar=cst[:, 56 + gcol:57 + gcol], in1=rstd[:, 0:n],
                                                         op0=ALU.mult, op1=ALU.mult),
                 reads=[b_tmpf[ti], b_cst, b_rstd], writes=[b_dst])
        return ev

    k.push()
    xr = k.sb("xr", [128, TT], F32); b_xr = Buf()
    xc = k.sb("xc", [128, T], F32); b_xc = Buf()
    xcb = k.sb("xcb", [128, 1, T], BF16); b_xcb = Buf()
    la = k.sb("la", [128, T], F32); b_la = Buf()
    bb = k.sb("bbuf", [128, T], F32); b_bb = Buf()
    hh = k.sb("hh", [128, T], F32); b_hh = Buf()
    wl = k.sb("wl", [128, 2, 512], BF16); b_wl = Buf()
    cout = k.sb("cout", [128, 4, 2], F32); b_cout = Buf()
    carry = k.sb("carry_s", [128, 4, 3, 2], F32); b_carry = Buf()
    w0, bw0 = load_w(0, d_win[0])
    w1, bw1 = load_w(1, d_win[1])
    k.dma("sp", wst[:, 0:2, :], d_wlru, writes=[b_wst])
    k.op("pool", lambda e: e.tensor_copy(out=wl[:], in_=wst[:, 0:2, :]), reads=[b_wst], writes=[b_wl])
    k.op("act", lambda e: e.activation(out=lsm[:, 0:4], in_=cst[:, 40:56].rearrange("p (c i) -> p c i", i=4)[:, :, 3],
                                       func=AF.Exp, scale=-1.0), reads=[b_cst], writes=[b_lsm])
    k.op("act", lambda e: e.activation(out=lsm[:, 0:4], in_=lsm[:, 0:4], func=AF.Ln, bias=onec), reads=[b_lsm, b_cst],
         writes=[b_lsm])
    k.op("dve", lambda e: e.tensor_scalar(out=lsm[:, 0:4], in0=lsm[:, 0:4], scalar1=-8.0, scalar2=None, op0=ALU.mult),
         reads=[b_lsm], writes=[b_lsm])
    k.op("dve", lambda e: e.memset(lsm[:, 8:12], 0.0), writes=[b_lsm])
    if full:
        k.dma("sp", carry[:], d_carry, writes=[b_carry])
        for kk in range(3):
            k.op("dve", lambda e, kk=kk: e.tensor_tensor(out=lsm[:, 8:12], in0=lsm[:, 8:12], in1=carry[:, :, kk, 0], op=ALU.mult),
                 reads=[b_lsm, b_carry], writes=[b_lsm])
            k.op("dve", lambda e, kk=kk: e.tensor_tensor(out=lsm[:, 8:12], in0=lsm[:, 8:12], in1=carry[:, :, kk, 1], op=ALU.add),
                 reads=[b_lsm, b_carry], writes=[b_lsm])
    C0 = 2.0 * math.sqrt(2.0 / math.pi)
    for c in range(4):
        def ev_xr(nci, s, n, pb, bpb):
            k.op("act", lambda e: e.activation(out=xr[:, s:s + n], in_=pb[:, 0:n], func=AF.Identity), reads=[bpb], writes=[b_xr])
        linear_fm(w0, bw0, [c], hT, b_hT, 8, tgroups, ev_xr)
        k.op("dve", lambda e: e.tensor_scalar(out=xr[:, 0:128], in0=xr[:, 0:128], scalar1=flag, scalar2=None, op0=ALU.mult),
             reads=[b_xr, b_cst], writes=[b_xr])
        k.op("dve", lambda e, c=c: e.tensor_scalar(out=xc[:], in0=xr[:, 125:125 + T], scalar1=convw(c, 0), scalar2=lrus(c, 0),
                                                   op0=ALU.mult, op1=ALU.add), reads=[b_xr, b_cst], writes=[b_xc])
        for tap in range(1, 4):
            k.op("dve", lambda e, c=c, tap=tap: e.scalar_tensor_tensor(out=xc[:], in0=xr[:, 125 + tap:125 + tap + T],
                                                                      scalar=convw(c, tap), in1=xc[:], op0=ALU.mult,
                                                                      op1=ALU.add), reads=[b_xr, b_cst, b_xc], writes=[b_xc])
        k.op("pool", lambda e: e.tensor_copy(out=xcb[:, 0, :], in_=xc[:]), reads=[b_xc], writes=[b_xcb])

        def ev_ga(nci, s, n, pb, bpb, c=c):
            k.op("act", lambda e: e.activation(out=la[:, s:s + n], in_=pb[:, 0:n], func=AF.Sigmoid, bias=lrus(c, 1)),
                 reads=[bpb, b_cst], writes=[b_la])

        def ev_gx(nci, s, n, pb, bpb, c=c):
            k.op("act", lambda e: e.activation(out=bb[:, s:s + n], in_=pb[:, 0:n], func=AF.Sigmoid, bias=lrus(c, 2)),
                 reads=[bpb, b_cst], writes=[b_bb])
        linear_fm(wl[:, 0:1, :], b_wl, [c], xcb, b_xcb, 1, og, ev_ga)
        linear_fm(wl[:, 1:2, :], b_wl, [c], xcb, b_xcb, 1, og, ev_gx)
        k.op("dve", lambda e, c=c: e.tensor_scalar(out=la[:], in0=la[:], scalar1=lsm[:, c:c + 1], scalar2=None, op0=ALU.mult),
             reads=[b_la, b_lsm], writes=[b_la])
        if not full:
            k.op("dve", lambda e, c=c: e.tensor_reduce(out=lsm[:, 12 + c:13 + c], in_=la[:], axis=AX.X, op=ALU.add),
                 reads=[b_la], writes=[b_lsm])
        k.op("act", lambda e: e.activation(out=hh[:], in_=la[:], func=AF.Exp, scale=2.0), reads=[b_la], writes=[b_hh])
        k.op("act", lambda e: e.activation(out=hh[:], in_=hh[:], func=AF.Sqrt, scale=-1.0, bias=onec), reads=[b_hh, b_cst],
             writes=[b_hh])
        k.op("act", lambda e: e.activation(out=la[:], in_=la[:], func=AF.Exp), reads=[b_la], writes=[b_la])
        k.op("dve", lambda e: e.tensor_tensor(out=bb[:], in0=bb[:], in1=xc[:], op=ALU.mult), reads=[b_bb, b_xc], writes=[b_bb])
        k.op("dve", lambda e: e.tensor_tensor(out=bb[:], in0=bb[:], in1=hh[:], op=ALU.mult), reads=[b_bb, b_hh], writes=[b_bb])
        k.op("dve", lambda e, c=c: e.tensor_tensor_scan(out=hh[:], data0=la[:], data1=bb[:], initial=lsm[:, 8 + c:9 + c],
                                                        op0=ALU.mult, op1=ALU.add),
             reads=[b_la, b_bb, b_lsm, b_hh], writes=[b_hh])
        if not full:
            k.op("dve", lambda e, c=c: e.tensor_copy(out=cout[:, c, 1:2], in_=hh[:, T - 1:T]), reads=[b_hh], writes=[b_cout])
        else:
            def ev_gr(nci, s, n, pb, bpb):
                ti = 0
                so = s - 128
                k.op("act", lambda e: e.activation(out=tmpf[ti][:, 0:n], in_=pb[:, 0:n], func=AF.Square), reads=[bpb],
                     writes=[b_tmpf[ti]])
                k.op("dve", lambda e: e.tensor_scalar(out=tmpf[ti][:, 0:n], in0=tmpf[ti][:, 0:n], scalar1=0.044715, scalar2=1.0,
                                                      op0=ALU.mult, op1=ALU.add), reads=[b_tmpf[ti]], writes=[b_tmpf[ti]])
                k.op("dve", lambda e: e.tensor_tensor(out=tmpf[ti][:, 0:n], in0=tmpf[ti][:, 0:n], in1=pb[:, 0:n], op=ALU.mult),
                     reads=[b_tmpf[ti], bpb], writes=[b_tmpf[ti]])
                k.op("act", lambda e: e.activation(out=tmpf[ti][:, 0:n], in_=tmpf[ti][:, 0:n], func=AF.Sigmoid, scale=C0),
                     reads=[b_tmpf[ti]], writes=[b_tmpf[ti]])
                k.op("dve", lambda e: e.tensor_tensor(out=tmpf[ti][:, 0:n], in0=tmpf[ti][:, 0:n], in1=pb[:, 0:n], op=ALU.mult),
                     reads=[b_tmpf[ti], bpb], writes=[b_tmpf[ti]])
                k.op("dve", lambda e: e.tensor_tensor(out=obr[:, nci, so:so + n], in0=tmpf[ti][:, 0:n], in1=hh[:, so:so + n],
                                                      op=ALU.mult), reads=[b_tmpf[ti], b_hh], writes=[b_obr])
            linear_fm(w1, bw1, [c], hT, b_hT, 8, own_groups, ev_gr)
    if not full:
        k.op("act", lambda e: e.activation(out=cout[:, :, 0], in_=lsm[:, 12:16], func=AF.Exp), reads=[b_lsm], writes=[b_cout])
        k.dma("sp", o_c, cout[:], reads=[b_cout], is_output=True)
    k.pop()

    if stop_after == "lru":
        return k.finish()
    k.push()
    kT = k.sb("kT", [128, T], BF16); b_kT = Buf()
    vA = k.sb("vA", [128, NBLK, 130], BF16); b_vA = Buf()
    wq, bwq = (load_w(0, d_win[2]) if full else (None, None))
    wk, bwk = load_w(1, d_win[3])
    wv, bwv = load_w(2, d_win[4])
    if full:
        qT = k.sb("qT", [128, T], BF16); b_qT = Buf()
        kprev = k.sb("kprev_s", [128, NPB * 128], BF16); b_kprev = Buf()
        vprev = k.sb("vprev_s", [128, NPB, 130], BF16); b_vprev = Buf()
        dbias = k.sb("dbias_s", [128, 2, 2, 128], F32); b_db = Buf()
        pT = [k.sb("pT%d" % i, [128, 2, 128], BF16) for i in range(2)]; b_pT = [Buf(), Buf()]
        sF = [k.sb("sF%d" % i, [128, 2, 128], F32) for i in range(2)]; b_sF = [Buf(), Buf()]
        acs = k.sb("acs", [128, 2, 130], F32); b_acs = Buf()
        otokb = k.sb("otokb", [128, 128], BF16); b_otokb = Buf()
        att = k.sb("att", [128, 32], F32); b_att = Buf()
        subln = k.sb("subln_s", [128, 128], F32); b_subln = Buf()
        dlam = k.sb("dlam_s", [128, 4, 64], F32); b_dlam = Buf()
        k.dma("sp", subln[:], d_subln, writes=[b_subln])
        k.dma("sp", dlam[:], d_dlam, writes=[b_dlam])
        k.dma("sp", att[:, 0:4], d_b31, writes=[b_att])
        k.dma("sp", att[:, 24:26], d_lamc, writes=[b_att])
        k.op("dve", lambda e: e.tensor_tensor(out=dlam[:, 0, :], in0=dlam[:, 0, :], in1=dlam[:, 1, :], op=ALU.mult),
             reads=[b_dlam], writes=[b_dlam])
        k.op("dve", lambda e: e.tensor_tensor(out=dlam[:, 2, :], in0=dlam[:, 2, :], in1=dlam[:, 3, :], op=ALU.mult),
             reads=[b_dlam], writes=[b_dlam])
        k.op("dve", lambda e: e.tensor_reduce(out=att[:, 14:15], in_=dlam[:, 0, :], axis=AX.X, op=ALU.add),
             reads=[b_dlam], writes=[b_att])
        k.op("dve", lambda e: e.tensor_reduce(out=att[:, 15:16], in_=dlam[:, 2, :], axis=AX.X, op=ALU.add),
             reads=[b_dlam], writes=[b_att])
        k.op("act", lambda e: e.activation(out=att[:, 14:16], in_=att[:, 14:16], func=AF.Exp), reads=[b_att], writes=[b_att])
        k.op("dve", lambda e: e.scalar_tensor_tensor(out=att[:, 13:14], in0=att[:, 15:16], scalar=att[:, 24:25], in1=att[:, 14:15],
                                                     op0=ALU.add, op1=ALU.subtract), reads=[b_att], writes=[b_att])
        k.op("dve", lambda e: e.tensor_scalar(out=subln[:], in0=subln[:], scalar1=att[:, 25:26], scalar2=None, op0=ALU.mult),
             reads=[b_subln, b_att], writes=[b_subln])
    for h in range(4):
        linear_fm(wk, bwk, [h], hT, b_hT, 8, own_groups, qknorm(lambda nci, s, n: kT[:, s - 128:s - 128 + n], b_kT, 1))
        k.op("dve", lambda e: e.memset(vA[:, :, 128:130], 1.0), writes=[b_vA])
        for blk in range(NBLK):
            pb, bpb = next_pb()
            for kc in range(8):
                k.op("pe", lambda e, kc=kc, blk=blk: e.matmul(pb[:, 0:128], lhsT=hT[:, kc, 128 + blk * 128:256 + blk * 128],
                                                             rhs=wv[:, kc, h * 128:(h + 1) * 128], start=(kc == 0), stop=(kc == 7)),
                     reads=[b_hT, bwv], writes=[bpb])
            k.op("act", lambda e, blk=blk: e.activation(out=vA[:, blk, 0:128], in_=pb[:, 0:128], func=AF.Identity),
                 reads=[bpb], writes=[b_vA])
        if not full:
            k.dma("sp", o_k[h], kT[:], reads=[b_kT], is_output=True)
            k.dma("sp", o_v[h], vA[:], reads=[b_vA], is_output=True)
            continue
        linear_fm(wq, bwq, [h], hT, b_hT, 8, own_groups, qknorm(lambda nci, s, n: qT[:, s - 128:s - 128 + n], b_qT, 0))
        k.dma("sp", kprev[:], d_kprev[h], writes=[b_kprev])
        k.dma("sp", vprev[:], d_vprev[h], writes=[b_vprev])
        k.dma("sp", dbias[:], d_dbias[h], writes=[b_db])
        for i in range(NBLK):
            kbl = [("p", j) for j in range(NPB)] + [("o", j) for j in range(i + 1)]
            acc = [pbank[0], pbank[1]]
            for idx, (kind, j) in enumerate(kbl):
                si = idx % 2
                pS = pbank[2 + si]
                if kind == "p":
                    ksrc = kprev[:, j * 128:(j + 1) * 128]
                    vsrc = vprev[:, j, 0:129]
                    kb_, vb_ = b_kprev, b_vprev
                    near = 1 if (i == 0 and j == NPB - 1) else None
                else:
                    ksrc = kT[:, j * 128:(j + 1) * 128]
                    vsrc = vA[:, j, 0:129]
                    kb_, vb_ = b_kT, b_vA
                    near = 0 if j == i else (1 if j == i - 1 else None)
                pSm = [pbank[2 + si], pbank[4 + si]]
                bSm = [b_pb[2 + si], b_pb[4 + si]]
                for m in range(2):
                    k.op("pe", lambda e, m=m, ksrc=ksrc: e.matmul(pSm[m][:, 0:128], lhsT=ksrc[m * 64:(m + 1) * 64, :],
                                                                 rhs=qT[m * 64:(m + 1) * 64, i * 128:(i + 1) * 128],
                                                                 start=True, stop=True),
                         reads=[kb_, b_qT], writes=[bSm[m]])
                for m in range(2):
                    if near is None:
                        k.op("act", lambda e, m=m, si=si: e.activation(out=pT[si][:, m, :], in_=pSm[m][:, 0:128], func=AF.Exp,
                                                                       scale=0.125, bias=att[:, h:h + 1]),
                             reads=[bSm[m], b_att], writes=[b_pT[si]])
                    else:
                        k.op("dve", lambda e, m=m, si=si, near=near: e.scalar_tensor_tensor(
                            out=sF[si][:, m, :], in0=pSm[m][:, 0:128], scalar=0.125, in1=dbias[:, near, m, :],
                            op0=ALU.mult, op1=ALU.add), reads=[bSm[m], b_db], writes=[b_sF[si]])
                        k.op("act", lambda e, m=m, si=si: e.activation(out=pT[si][:, m, :], in_=sF[si][:, m, :], func=AF.Exp),
                             reads=[b_sF[si]], writes=[b_pT[si]])
                for m in range(2):
                    k.op("pe", lambda e, m=m, si=si, vsrc=vsrc, idx=idx, L=len(kbl): e.matmul(
                        acc[m][:, 0:129], lhsT=pT[si][:, m, :], rhs=vsrc, start=(idx == 0), stop=(idx == L - 1)),
                        reads=[b_pT[si], vb_], writes=[b_pb[m]])
            for m in range(2):
                k.op("act", lambda e, m=m: e.activation(out=acs[:, m, 0:129], in_=acc[m][:, 0:129], func=AF.Identity),
                     reads=[b_pb[m]], writes=[b_acs])
            k.op("dve", lambda e: e.reciprocal(out=att[:, 16:18], in_=acs[:, :, 128]), reads=[b_acs], writes=[b_att])
            k.op("dve", lambda e: e.tensor_tensor(out=att[:, 17:18], in0=att[:, 17:18], in1=att[:, 13:14], op=ALU.mult),
                 reads=[b_att], writes=[b_att])
            k.op("dve", lambda e: e.tensor_scalar(out=acs[:, 0, 0:128], in0=acs[:, 0, 0:128], scalar1=att[:, 16:17], scalar2=None,
                                                  op0=ALU.mult), reads=[b_acs, b_att], writes=[b_acs])
            k.op("dve", lambda e: e.scalar_tensor_tensor(out=acs[:, 0, 0:128], in0=acs[:, 1, 0:128], scalar=att[:, 17:18],
                                                         in1=acs[:, 0, 0:128], op0=ALU.mult, op1=ALU.add),
                 reads=[b_acs, b_att], writes=[b_acs])
            k.op("dve", lambda e: e.tensor_tensor(out=acs[:, 1, 0:128], in0=acs[:, 0, 0:128], in1=acs[:, 0, 0:128], op=ALU.mult),
                 reads=[b_acs], writes=[b_acs])
            k.op("dve", lambda e: e.tensor_reduce(out=att[:, 18:19], in_=acs[:, 1, 0:128], axis=AX.X, op=ALU.add),
                 reads=[b_acs], writes=[b_att])
            k.op("act", lambda e: e.activation(out=att[:, 18:19], in_=att[:, 18:19], func=AF.Sqrt, scale=1.0 / 128, bias=epsc),
                 reads=[b_att, b_cst], writes=[b_att])
            k.op("dve", lambda e: e.reciprocal(out=att[:, 18:19], in_=att[:, 18:19]), reads=[b_att], writes=[b_att])
            k.op("dve", lambda e: e.scalar_tensor_tensor(out=otokb[:], in0=acs[:, 0, 0:128], scalar=att[:, 18:19], in1=subln[:],
                                                         op0=ALU.mult, op1=ALU.mult),
                 reads=[b_acs, b_att, b_subln], writes=[b_otokb])
            pt, bpt = next_pb()
            k.op("pe", lambda e: e.matmul(pt[:, 0:128], lhsT=otokb[:], rhs=identb[:], start=True, stop=True),
                 reads=[b_otokb, b_id], writes=[bpt])
            k.op("act", lambda e, i=i: e.activation(out=obr[:, 4 + h, i * 128:(i + 1) * 128], in_=pt[:, 0:128], func=AF.Identity),
                 reads=[bpt], writes=[b_obr])
    k.pop()
    if not full or stop_after == "diff":
        return k.finish()

    k.push()
    sqT = k.sb("sqT", [128, 4, T], BF16); b_sqT = Buf()
    skT = k.sb("skT", [128, TT], BF16); b_skT = Buf()
    svA = k.sb("svA", [128, NBLK + 1, 2, 66], BF16); b_svA = Buf()
    sbias = k.sb("sbias_s", [128, 8, 2, 128], F32); b_sb = Buf()
    pT = [k.sb("spT%d" % i, [128, 2, 128], BF16) for i in range(2)]; b_pT = [Buf(), Buf()]
    sF = [k.sb("ssF%d" % i, [128, 2, 128], F32) for i in range(2)]; b_sF = [Buf(), Buf()]
    otokb = k.sb("sotokb", [128, 512], BF16); b_otokb = Buf()
    att = k.sb("satt", [128, 32], F32); b_att = Buf()
    k.dma("sp", sbias[:], d_sbias, writes=[b_sb])
    k.dma("sp", att[:, 4:12], d_sink, writes=[b_att])
    k.op("act", lambda e: e.activation(out=att[:, 4:12], in_=att[:, 4:12], func=AF.Exp), reads=[b_att], writes=[b_att])
    w, bw = load_w(0, d_win[5])
    linear_fm(w, bw, [0, 1, 2, 3], hT, b_hT, 8, own_groups,
              qknorm(lambda nci, s, n: sqT[:, nci, s - 128:s - 128 + n], b_sqT, 2))
    w, bw = load_w(1, d_win[6])
    linear_fm(w, bw, [0], hT, b_hT, 8, tgroups, qknorm(lambda nci, s, n: skT[:, s:s + n], b_skT, 3))
    k.op("dve", lambda e: e.memset(svA[:, :, :, 64:66], 1.0), writes=[b_svA])
    for blk in range(NBLK + 1):
        pb, bpb = next_pb()
        for kc in range(8):
            k.op("pe", lambda e, kc=kc, blk=blk: e.matmul(pb[:, 0:128], lhsT=hT[:, kc, blk * 128:(blk + 1) * 128],
                                                         rhs=w[:, kc, 128:256], start=(kc == 0), stop=(kc == 7)),
                 reads=[b_hT, bw], writes=[bpb])
        k.op("act", lambda e, blk=blk: e.activation(out=svA[:, blk, :, 0:64], in_=pb[:, 0:128].rearrange("p (h d) -> p h d", h=2),
                                                    func=AF.Identity), reads=[bpb], writes=[b_svA])
    k.op("dve", lambda e: e.tensor_scalar(out=svA[:, 0, :, :], in0=svA[:, 0, :, :], scalar1=flag, scalar2=None, op0=ALU.mult),
         reads=[b_svA, b_cst], writes=[b_svA])
    for i in range(NBLK):
        for hd in range(8):
            kv = hd // 4
            c = hd % 4
            si = hd % 2
            pS = pbank[2 + si]
            bS = b_pb[2 + si]
            for bi in range(2):
                k.op("pe", lambda e, bi=bi, kv=kv, c=c, i=i, pS=pS: e.matmul(
                    pS[:, bi * 128:(bi + 1) * 128], lhsT=skT[kv * 64:(kv + 1) * 64, (i + bi) * 128:(i + bi + 1) * 128],
                    rhs=sqT[kv * 64:(kv + 1) * 64, c, i * 128:(i + 1) * 128], start=True, stop=True),
                    reads=[b_skT, b_sqT], writes=[bS])
            pv = pS[:, 0:256].rearrange("p (m q) -> p m q", m=2)
            k.op("dve", lambda e, pv=pv, si=si, hd=hd: e.scalar_tensor_tensor(out=sF[si][:], in0=pv, scalar=0.125,
                                                                             in1=sbias[:, hd], op0=ALU.mult, op1=ALU.add),
                 reads=[bS, b_sb], writes=[b_sF[si]])
            k.op("act", lambda e, si=si: e.activation(out=pT[si][:], in_=sF[si][:], func=AF.Exp), reads=[b_sF[si]],
                 writes=[b_pT[si]])
            pa = pbank[si]
            ba_ = b_pb[si]
            for bi in range(2):
                k.op("pe", lambda e, bi=bi, si=si, kv=kv, i=i, pa=pa: e.matmul(pa[:, 0:65], lhsT=pT[si][:, bi, :],
                                                                              rhs=svA[:, i + bi, kv, 0:65],
                                                                              start=(bi == 0), stop=(bi == 1)),
                     reads=[b_pT[si], b_svA], writes=[ba_])
            k.op("dve", lambda e, hd=hd, pa=pa: e.tensor_tensor(out=att[:, 20 + hd:21 + hd], in0=pa[:, 64:65],
                                                               in1=att[:, 4 + hd:5 + hd], op=ALU.add),
                 reads=[ba_, b_att], writes=[b_att])
            k.op("dve", lambda e, hd=hd: e.reciprocal(out=att[:, 20 + hd:21 + hd], in_=att[:, 20 + hd:21 + hd]), reads=[b_att],
                 writes=[b_att])
            k.op("dve", lambda e, hd=hd, pa=pa: e.tensor_scalar(out=otokb[:, hd * 64:(hd + 1) * 64], in0=pa[:, 0:64],
                                                               scalar1=att[:, 20 + hd:21 + hd], scalar2=None, op0=ALU.mult),
                 reads=[ba_, b_att], writes=[b_otokb])
        for c in range(4):
            pt, bpt = next_pb()
            k.op("pe", lambda e, c=c: e.matmul(pt[:, 0:128], lhsT=otokb[:, c * 128:(c + 1) * 128], rhs=identb[:],
                                               start=True, stop=True), reads=[b_otokb, b_id], writes=[bpt])
            k.op("act", lambda e, c=c, i=i: e.activation(out=obr[:, 8 + c, i * 128:(i + 1) * 128], in_=pt[:, 0:128],
                                                         func=AF.Identity), reads=[bpt], writes=[b_obr])
    k.pop()

    if debug:
        k.dma("sp", o_dbg, obr[:], reads=[b_obr], is_output=True)
    if stop_after == "swa":
        return k.finish()
    k.push()
    mg = k.sb("mg", [128, 8, G], BF16); b_mg = Buf()
    xg = k.sb("xg2", [128, 8, G], F32); b_xg = Buf()
    wo = k.sb("wo", [128, 8, 1024], BF16); b_wo = Buf()
    wr = k.sb("wr_s", [128, 8, 32], F32); b_wr = Buf()
    brt = k.sb("br_s", [128, 32], F32); b_br = Buf()
    lg = k.sb("lg", [128, 32], F32); b_lg = Buf()
    t8 = k.sb("t8", [128, 8], F32); b_t8 = Buf()
    rwt = k.sb("rwt", [128, 32], F32); b_rwt = Buf()
    k.dma("sp", wr[:], d_wr, writes=[b_wr])
    k.dma("sp", brt[:], d_br, writes=[b_br])
    k.dma("sp", o_gf, mod[:, 40:48], reads=[b_mod], is_output=True)
    for half in range(2):
        k.dma("sp", wst[:], d_wout[half], writes=[b_wst])
        k.op("pool", lambda e, half=half: e.tensor_copy(out=wo[:, :, half * 512:(half + 1) * 512], in_=wst[:]),
             reads=[b_wst], writes=[b_wo])
    for (s, n) in og:
        k.dma("sp", xg[:, :, 0:n], d_xT[:, :, 128 + s:128 + s + n], writes=[b_xg])
        for f in range(8):
            k.dma("sp", wst[:, :, 0:384], d_wgl[f], writes=[b_wst])
            k.dma("sp", wst[:, :, 384:512], d_wbr[f][:, 0:8, :], writes=[b_wst])
            k.op("pool", lambda e: e.tensor_copy(out=wbf[0][:], in_=wst[:]), reads=[b_wst], writes=[b_wbf[0]])
            k.dma("sp", wst[:, 0:4, 0:128], d_wbr[f][:, 8:12, :], writes=[b_wst])
            k.op("pool", lambda e: e.tensor_copy(out=wbf[1][:, 0:4, 0:128], in_=wst[:, 0:4, 0:128]), reads=[b_wst],
                 writes=[b_wbf[1]])
            wg, bwg = wbf[0], b_wbf[0]
            w2_, bw2_ = wbf[1], b_wbf[1]
            for nb in range(3):
                pg, bpg = next_pb()
                for kc in range(8):
                    k.op("pe", lambda e, kc=kc, nb=nb, pg=pg: e.matmul(pg[:, 0:n], lhsT=wg[:, kc, nb * 128:(nb + 1) * 128],
                                                                      rhs=hT[:, kc, 128 + s:128 + s + n],
                                                                      start=(kc == 0), stop=(kc == 7)),
                         reads=[bwg, b_hT], writes=[bpg])
                ti = nb % 2
                k.op("act", lambda e, ti=ti, pg=pg: e.activation(out=tmpf[ti][:, 0:n], in_=pg[:, 0:n], func=AF.Sigmoid),
                     reads=[bpg], writes=[b_tmpf[ti]])
                pp, bpp = next_pb()
                for kc in range(4):
                    if nb < 2:
                        lw, lb = wg[:, nb * 4 + kc, 384:512], bwg
                    else:
                        lw, lb = w2_[:, kc, 0:128], bw2_
                    k.op("pe", lambda e, kc=kc, lw=lw, nb=nb, pp=pp: e.matmul(pp[:, 0:n], lhsT=lw, rhs=obr[:, nb * 4 + kc, s:s + n],
                                                                             start=(kc == 0), stop=(kc == 3)),
                         reads=[lb, b_obr], writes=[bpp])
                if nb == 0:
                    k.op("dve", lambda e, ti=ti, pp=pp: e.tensor_tensor(out=rstd[:, 0:n], in0=pp[:, 0:n], in1=tmpf[ti][:, 0:n],
                                                                       op=ALU.mult), reads=[bpp, b_tmpf[ti]], writes=[b_rstd])
                else:
                    k.op("dve", lambda e, ti=ti, pp=pp: e.tensor_tensor(out=tmpf[ti][:, 0:n], in0=pp[:, 0:n],
                                                                       in1=tmpf[ti][:, 0:n], op=ALU.mult),
                         reads=[bpp, b_tmpf[ti]], writes=[b_tmpf[ti]])
                    if nb == 1:
                        k.op("dve", lambda e, ti=ti: e.tensor_tensor(out=rstd[:, 0:n], in0=rstd[:, 0:n], in1=tmpf[ti][:, 0:n],
                                                                    op=ALU.add), reads=[b_rstd, b_tmpf[ti]], writes=[b_rstd])
                    else:
                        k.op("dve", lambda e, ti=ti, f=f: e.tensor_tensor(out=mg[:, f, 0:n], in0=rstd[:, 0:n],
                                                                         in1=tmpf[ti][:, 0:n], op=ALU.add),
                             reads=[b_rstd, b_tmpf[ti]], writes=[b_mg])

        def ev_out(nci, s_, n_, pb, bpb):
            k.op("dve", lambda e: e.scalar_tensor_tensor(out=xg[:, nci, 0:n_], in0=pb[:, 0:n_], scalar=mod[:, 16 + nci:17 + nci],
                                                         in1=xg[:, nci, 0:n_], op0=ALU.mult, op1=ALU.add),
                 reads=[bpb, b_mod, b_xg], writes=[b_xg])
        linear_fm(wo, b_wo, list(range(8)), mg, b_mg, 8, [(0, n)], ev_out)
        k.dma("sp", o_xT[:, :, s:s + n], xg[:, :, 0:n], reads=[b_xg], is_output=True)
        norm_mod(xg, b_xg, n, 8, 24, lambda c, s=s, n=n: hT[:, c, 128 + s:128 + s + n], b_hT, inplace_f32=True)
        for tb in range(n // 128):
            pl, bpl = next_pb()
            for kc in range(8):
                k.op("pe", lambda e, kc=kc, tb=tb, pl=pl: e.matmul(pl[:, 0:32], lhsT=xg[:, kc, tb * 128:(tb + 1) * 128],
                                                                  rhs=wr[:, kc, :], start=(kc == 0), stop=(kc == 7)),
                     reads=[b_xg, b_wr], writes=[bpl])
            k.op("dve", lambda e, pl=pl: e.tensor_tensor(out=lg[:], in0=pl[:, 0:32], in1=brt[:], op=ALU.add), reads=[bpl, b_br],
                 writes=[b_lg])
            k.op("dve", lambda e: e.max(out=t8[:], in_=lg[:]), reads=[b_lg], writes=[b_t8])
            k.op("dve", lambda e: e.tensor_scalar(out=rwt[:], in0=lg[:], scalar1=t8[:, 3:4], scalar2=None, op0=ALU.is_ge),
                 reads=[b_lg, b_t8], writes=[b_rwt])
            k.op("dve", lambda e: e.tensor_scalar(out=lg[:], in0=lg[:], scalar1=t8[:, 0:1], scalar2=None, op0=ALU.subtract),
                 reads=[b_lg, b_t8], writes=[b_lg])
            k.op("act", lambda e: e.activation(out=lg[:], in_=lg[:], func=AF.Exp), reads=[b_lg], writes=[b_lg])
            k.op("dve", lambda e: e.tensor_tensor(out=rwt[:], in0=rwt[:], in1=lg[:], op=ALU.mult), reads=[b_rwt, b_lg],
                 writes=[b_rwt])
            k.op("dve", lambda e: e.tensor_reduce(out=t8[:, 4:5], in_=rwt[:], axis=AX.X, op=ALU.add), reads=[b_rwt],
                 writes=[b_t8])
            k.op("dve", lambda e: e.reciprocal(out=t8[:, 4:5], in_=t8[:, 4:5]), reads=[b_t8], writes=[b_t8])
            k.op("dve", lambda e: e.tensor_scalar(out=rwt[:], in0=rwt[:], scalar1=t8[:, 4:5], scalar2=None, op0=ALU.mult),
                 reads=[b_rwt, b_t8], writes=[b_rwt])
            r0 = s + tb * 128
            k.dma("sp", o_rw[r0:r0 + 128, :], rwt[:], reads=[b_rwt], is_output=True)
    for (s, n) in groups_of(T, 512):
        k.dma("sp", o_hT[:, :, s:s + n], hT[:, :, 128 + s:128 + s + n], reads=[b_hT], is_output=True)
    k.pop()
    return k.finish()

def build_moe(NT):
    k = KB()
    G = 512
    NG = NT // G
    d_hT = k.dram("hT", [128, 8, NT], BF16, "ExternalInput")
    d_rw = k.dram("rw", [4, NT], F32, "ExternalInput")
    d_w1 = k.dram("w1", [4, 4, 128, 8, 512], F32, "ExternalInput")
    d_w2 = k.dram("w2", [4, 2, 128, 8, 512], F32, "ExternalInput")
    d_b1 = k.dram("b1", [128, 4, 16], F32, "ExternalInput")
    d_b2 = k.dram("b2", [128, 4, 8], F32, "ExternalInput")
    d_sel = k.dram("sel", [4, 4, 128], F32, "ExternalInput")
    o_y = k.dram("o_y", [128, 8, NT], BF16, "ExternalOutput")
    scr = k.dram("yscr", [128, 8, NT], F32, "Internal")

    w1 = k.sb("w1s", [128, 8, 2048], BF16); b_w1 = Buf()
    w2 = k.sb("w2s", [128, 8, 1024], BF16); b_w2 = Buf()
    wst = k.sb("wst", [128, 8, 512], F32); b_wst = Buf()
    hg = [k.sb("hg%d" % i, [128, 8, G], BF16) for i in range(2)]; b_hg = [Buf(), Buf()]
    act = k.sb("act", [128, 8, G], BF16); b_act = Buf()
    ysb = k.sb("ysb", [128, 8, G], F32); b_ysb = Buf()
    ypv = k.sb("ypv", [128, 8, G], F32); b_ypv = Buf()
    y16 = k.sb("y16", [128, 8, G], BF16); b_y16 = Buf()
    wbc = k.sb("wbc", [128, G], F32); b_wbc = Buf()
    rwg = [k.sb("rws%d" % i, [4, G], F32) for i in range(2)]; b_rwg = [Buf(), Buf()]
    sel = k.sb("sels", [4, 4, 128], F32); b_sel = Buf()
    b1 = k.sb("b1s", [128, 4, 16], F32); b_b1 = Buf()
    b2 = k.sb("b2s", [128, 4, 8], F32); b_b2 = Buf()
    tg = [k.sb("tg%d" % i, [128, G], F32) for i in range(2)]; b_tg = [Buf(), Buf()]
    ts_ = [k.sb("ts%d" % i, [128, G], F32) for i in range(2)]; b_ts = [Buf(), Buf()]
    tl = [k.sb("tl%d" % i, [128, G], F32) for i in range(2)]; b_tl = [Buf(), Buf()]
    pbank = [k.ps("pb%d" % i, [128, 512]) for i in range(8)]
    b_pb = [Buf() for _ in range(8)]
    b_y = [Buf() for _ in range(NG)]

    k.dma("sp", sel[:], d_sel, writes=[b_sel])
    k.dma("sp", b1[:], d_b1, writes=[b_b1])
    k.dma("sp", b2[:], d_b2, writes=[b_b2])
    k.op("dve", lambda e: e.tensor_scalar(out=b1[:, :, 8:16], in0=b1[:, :, 8:16], scalar1=1.0, scalar2=None, op0=ALU.add),
         reads=[b_b1], writes=[b_b1])
    pi = [0]

    def npb(lo, hi):
        i = lo + (pi[0] % (hi - lo))
        pi[0] += 1
        return pbank[i], b_pb[i]

    for ex in range(4):
        for blk in range(4):
            k.dma("sp", wst[:], d_w1[ex, blk], writes=[b_wst])
            k.op("pool", lambda e, blk=blk: e.tensor_copy(out=w1[:, :, blk * 512:(blk + 1) * 512], in_=wst[:]),
                 reads=[b_wst], writes=[b_w1])
        for blk in range(2):
            k.dma("sp", wst[:], d_w2[ex, blk], writes=[b_wst])
            k.op("pool", lambda e, blk=blk: e.tensor_copy(out=w2[:, :, blk * 512:(blk + 1) * 512], in_=wst[:]),
                 reads=[b_wst], writes=[b_w2])
        for g in range(NG):
            gi = g % 2
            s = g * G
            k.dma("sp", hg[gi][:], d_hT[:, :, s:s + G], writes=[b_hg[gi]])
            k.dma("sp", rwg[gi][:], d_rw[:, s:s + G], writes=[b_rwg[gi]])
            if ex > 0:
                k.dma("sp", ypv[:], scr[:, :, s:s + G], reads=[b_y[g]], writes=[b_ypv])
            pw = pbank[7]
            k.op("pe", lambda e: e.matmul(pw[:, :], lhsT=sel[:, ex, :], rhs=rwg[gi][:], start=True, stop=True),
                 reads=[b_sel, b_rwg[gi]], writes=[b_pb[7]])
            k.op("act", lambda e: e.activation(out=wbc[:], in_=pw[:, :], func=AF.Identity), reads=[b_pb[7]], writes=[b_wbc])
            for ci in range(8):
                ti = ci % 2
                pg, bpg = npb(0, 3)
                for kc in range(8):
                    k.op("pe", lambda e, kc=kc: e.matmul(pg[:, :], lhsT=w1[:, kc, ci * 128:(ci + 1) * 128], rhs=hg[gi][:, kc, :],
                                                         start=(kc == 0), stop=(kc == 7)), reads=[b_w1, b_hg[gi]], writes=[bpg])
                pl, bpl = npb(0, 3)
                for kc in range(8):
                    k.op("pe", lambda e, kc=kc: e.matmul(pl[:, :], lhsT=w1[:, kc, 1024 + ci * 128:1024 + (ci + 1) * 128],
                                                         rhs=hg[gi][:, kc, :], start=(kc == 0), stop=(kc == 7)),
                         reads=[b_w1, b_hg[gi]], writes=[bpl])
                k.op("dve", lambda e: e.tensor_scalar(out=tg[ti][:], in0=pg[:, :], scalar1=b1[:, ex, ci:ci + 1], scalar2=7.0,
                                                      op0=ALU.add, op1=ALU.min), reads=[bpg, b_b1], writes=[b_tg[ti]])
                k.op("act", lambda e: e.activation(out=ts_[ti][:], in_=tg[ti][:], func=AF.Sigmoid, scale=1.702),
                     reads=[b_tg[ti]], writes=[b_ts[ti]])
                k.op("dve", lambda e: e.tensor_scalar(out=tl[ti][:], in0=pl[:, :], scalar1=b1[:, ex, 8 + ci:9 + ci], scalar2=8.0,
                                                      op0=ALU.add, op1=ALU.min), reads=[bpl, b_b1], writes=[b_tl[ti]])
                k.op("dve", lambda e: e.tensor_tensor(out=tg[ti][:], in0=tg[ti][:], in1=ts_[ti][:], op=ALU.mult),
                     reads=[b_tg[ti], b_ts[ti]], writes=[b_tg[ti]])
                k.op("dve", lambda e: e.scalar_tensor_tensor(out=tl[ti][:], in0=tl[ti][:], scalar=-6.0, in1=tg[ti][:],
                                                             op0=ALU.max, op1=ALU.mult), reads=[b_tl[ti], b_tg[ti]],
                     writes=[b_tl[ti]])
                k.op("dve", lambda e: e.tensor_tensor(out=act[:, ci, :], in0=tl[ti][:], in1=wbc[:], op=ALU.mult),
                     reads=[b_tl[ti], b_wbc], writes=[b_act])
            for f in range(8):
                py, bpy = npb(3, 7)
                for kc in range(8):
                    k.op("pe", lambda e, kc=kc: e.matmul(py[:, :], lhsT=w2[:, kc, f * 128:(f + 1) * 128], rhs=act[:, kc, :],
                                                         start=(kc == 0), stop=(kc == 7)), reads=[b_w2, b_act], writes=[bpy])
                k.op("dve", lambda e: e.scalar_tensor_tensor(out=ysb[:, f, :], in0=wbc[:], scalar=b2[:, ex, f:f + 1], in1=py[:, :],
                                                             op0=ALU.mult, op1=ALU.add), reads=[b_wbc, b_b2, bpy], writes=[b_ysb])
                if ex > 0:
                    k.op("dve", lambda e: e.tensor_tensor(out=ysb[:, f, :], in0=ysb[:, f, :], in1=ypv[:, f, :], op=ALU.add),
                         reads=[b_ysb, b_ypv], writes=[b_ysb])
            if ex < 3:
                k.dma("sp", scr[:, :, s:s + G], ysb[:], reads=[b_ysb], writes=[b_y[g]])
            else:
                k.op("pool", lambda e: e.tensor_copy(out=y16[:], in_=ysb[:]), reads=[b_ysb], writes=[b_y16])
                k.dma("sp", o_y[:, :, s:s + G], y16[:], reads=[b_y16], is_output=True)
    return k.finish()


def build_resid(T):
    k = KB()
    d_y = k.dram("yp", [8, 128, 8, T], BF16, "ExternalInput")
    d_x = k.dram("xT", [128, 8, T], F32, "ExternalInput")
    d_g = k.dram("gf", [128, 8], F32, "ExternalInput")
    o_x = k.dram("o_x", [128, 8, T], F32, "ExternalOutput")
    G = min(512, T)
    acc = [k.sb("acc%d" % i, [128, 8, G], F32) for i in range(2)]; b_acc = [Buf(), Buf()]
    yb = [k.sb("yb%d" % i, [128, 8, G], BF16) for i in range(3)]; b_yb = [Buf(), Buf(), Buf()]
    xb = [k.sb("xb%d" % i, [128, 8, G], F32) for i in range(2)]; b_xb = [Buf(), Buf()]
    gf = k.sb("gfs", [128, 8], F32); b_gf = Buf()
    k.dma("sp", gf[:], d_g, writes=[b_gf])
    n_y = 0
    for gi, (s, n) in enumerate(groups_of(T, G)):
        a = gi % 2
        k.dma("sp", xb[a][:, :, 0:n], d_x[:, :, s:s + n], writes=[b_xb[a]])
        k.dma("sp", yb[2][:, :, 0:n], d_y[0, :, :, s:s + n], writes=[b_yb[2]])
        k.op("dve", lambda e: e.tensor_copy(out=acc[a][:, :, 0:n], in_=yb[2][:, :, 0:n]), reads=[b_yb[2]], writes=[b_acc[a]])
        for r in range(1, 8):
            yi = n_y % 2
            n_y += 1
            k.dma("sp", yb[yi][:, :, 0:n], d_y[r, :, :, s:s + n], writes=[b_yb[yi]])
            k.op("dve", lambda e: e.tensor_tensor(out=acc[a][:, :, 0:n], in0=acc[a][:, :, 0:n], in1=yb[yi][:, :, 0:n], op=ALU.add),
                 reads=[b_acc[a], b_yb[yi]], writes=[b_acc[a]])
        for c in range(8):
            k.op("dve", lambda e, c=c: e.scalar_tensor_tensor(out=xb[a][:, c, 0:n], in0=acc[a][:, c, 0:n], scalar=gf[:, c:c + 1],
                                                             in1=xb[a][:, c, 0:n], op0=ALU.mult, op1=ALU.add),
                 reads=[b_acc[a], b_gf, b_xb[a]], writes=[b_xb[a]])
        k.dma("sp", o_x[:, :, s:s + n], xb[a][:, :, 0:n], reads=[b_xb[a]], is_output=True)
    return k.finish()

def _tile_w(W):
    K_, N_ = W.shape
    return np.ascontiguousarray(W.reshape(K_ // 128, 128, N_).transpose(1, 0, 2))


def _vec(v, nch):
    return np.ascontiguousarray(np.asarray(v).reshape(nch, 128).T)


def _t5_bucket(dist):
    dist = np.asarray(dist)
    lr = np.log(np.maximum(dist, 1).astype(np.float32) / np.float32(16)) / np.float32(math.log(128 / 16))
    large = 16 + (lr * np.float32(16)).astype(np.int32)
    return np.where(dist < 16, dist, np.minimum(large, 31))


def _run(nc, maps):
    res = run_bass_kernel_spmd(nc, maps, core_ids=list(range(len(maps))))
    return res.results


_DEBUG = {}


def kernel(x, c, w_ada, b_ada, norm_mix, norm_ffn, w_in, conv_w, conv_b, lru_wa, lru_ba, lru_wx, lru_bx, lru_lambda,
           diff_qnorm, diff_knorm, diff_lambda, diff_subln, swa_qnorm, swa_knorm, swa_sinks, rel_bias, w_branch, w_out,
           w_router, b_router, w1, b1, w2, b2):
    f32 = np.float32
    x = np.asarray(x, f32)
    Bn, S, _ = x.shape
    NC = 8
    CPB = NC // Bn
    T = S // CPB
    NBLK = T // 128
    NT = Bn * S
    depth = w_ada.shape[0]
    bf = ml_dtypes.bfloat16
    ident = np.eye(128, dtype=f32)
    bdones = np.zeros((128, 128), f32)
    bdones[:64, :64] = 1.0
    bdones[64:, 64:] = 1.0
    rel_bias = np.asarray(rel_bias, f32)
    qq = np.arange(128)[None, :]
    kk = np.arange(128)[:, None]
    dbias = np.zeros((4, 128, 2, 2, 128), f32)
    d0 = qq - kk
    d1 = 128 + qq - kk
    for h in range(4):
        diag = np.where(d0 >= 0, rel_bias[_t5_bucket(np.maximum(d0, 0)), h], f32(NEG))
        prev = rel_bias[_t5_bucket(d1), h]
        for m in range(2):
            dbias[h, :, 0, m, :] = diag
            dbias[h, :, 1, m, :] = prev
    b31 = np.ascontiguousarray(np.broadcast_to(rel_bias[31, 0:4][None, :], (128, 4))).astype(f32)
    sbias = np.zeros((128, 8, 2, 128), f32)
    for hd in range(8):
        sbias[:, hd, 0, :] = np.where(d1 < 128, rel_bias[_t5_bucket(np.clip(d1, 0, 127)), 4 + hd], f32(NEG))
        sbias[:, hd, 1, :] = np.where(d0 >= 0, rel_bias[_t5_bucket(np.clip(d0, 0, 127)), 4 + hd], f32(NEG))
    sel = np.zeros((4, 4, 128), f32)
    for e in range(4):
        sel[e, e, :] = 1.0
    perm = np.concatenate([np.concatenate([np.arange(cc * 64, (cc + 1) * 64), np.arange((cc + 4) * 64, (cc + 5) * 64)])
                           for cc in range(4)])

    xcur = x.reshape(NT, 1024)
    for l in range(depth):
        lam_init = 0.8 - 0.6 * math.exp(-0.3 * l)
        W = np.asarray(w_in[l], f32)
        blk6 = np.concatenate([W[:, 3072:3328], np.zeros((1024, 256), f32)], axis=1)
        win = np.stack([_tile_w(W[:, 0:512]), _tile_w(W[:, 512:1024]), _tile_w(W[:, 1024:1536]), _tile_w(W[:, 1536:2048]),
                        _tile_w(W[:, 2048:2560]), _tile_w(W[:, 2560:3072][:, perm]), _tile_w(blk6)])
        wada_full = np.stack([_tile_w(np.asarray(w_ada[l], f32)[:, i * 512:(i + 1) * 512]) for i in range(12)])
        bada = _vec(b_ada[l], 48)
        convw = np.ascontiguousarray(np.asarray(conv_w[l], f32).reshape(4, 4, 128).transpose(2, 1, 0))
        lrus = np.stack([_vec(conv_b[l], 4), _vec(lru_ba[l], 4), _vec(lru_bx[l], 4), _vec(lru_lambda[l], 4)], axis=2)
        wlru = np.zeros((128, 2, 512), f32)
        for wi, Wl in enumerate((lru_wa[l], lru_wx[l])):
            Wl = np.asarray(Wl, f32)
            for cc in range(4):
                for hb in range(2):
                    wlru[hb * 64:(hb + 1) * 64, wi, cc * 128 + hb * 64:cc * 128 + (hb + 1) * 64] = Wl[2 * cc + hb]
        qkg = np.stack([np.tile(np.asarray(g[l], f32), 2) for g in (diff_qnorm, diff_knorm, swa_qnorm, swa_knorm)], axis=1)
        common = {"wada": None, "bada": bada, "nmix": _vec(norm_mix[l], 8), "win": win, "convw": convw,
                  "lrus": np.ascontiguousarray(lrus), "wlru": wlru, "qkg": np.ascontiguousarray(qkg), "ident": ident,
                  "bdones": bdones}

        def xT_of(core):
            b, j = divmod(core, CPB)
            start = b * S + j * T
            xs = np.zeros((T + 128, 1024), f32)
            xs[128:] = xcur[start:start + T]
            if j > 0:
                xs[:128] = xcur[start - 128:start]
            return np.ascontiguousarray(xs.T.reshape(8, 128, T + 128).transpose(1, 0, 2))

        def base_map(core, full):
            b, j = divmod(core, CPB)
            m = dict(common)
            m["wada"] = wada_full if full else np.ascontiguousarray(wada_full[:4])
            m["xT"] = xT_of(core)
            m["cvec"] = _vec(np.asarray(c, f32)[b], 8)
            m["flag"] = np.full((128, 1), 1.0 if j > 0 else 0.0, f32)
            return m

        ncA = build_mixer("A", T)
        resA = _run(ncA, [base_map(core, False) for core in range(NC)])
        Wg = W[:, 3328:6400]
        wgl = np.stack([_tile_w(np.concatenate([Wg[:, n * 1024 + f * 128:n * 1024 + (f + 1) * 128] for n in range(3)], axis=1))
                        for f in range(8)])
        Wb = np.asarray(w_branch[l], f32)
        wbr = np.stack([np.concatenate([_tile_w(Wb[n][:, f * 128:(f + 1) * 128]) for n in range(3)], axis=1) for f in range(8)])
        Wo = np.asarray(w_out[l], f32)
        wout = np.stack([_tile_w(Wo[:, 0:512]), _tile_w(Wo[:, 512:1024])])
        extraB = {"nffn": _vec(norm_ffn[l], 8), "dbias": dbias, "b31": b31, "sbias": sbias,
                  "sinks": np.ascontiguousarray(np.broadcast_to(np.asarray(swa_sinks[l], f32)[None, :], (128, 8))),
                  "dlam": np.ascontiguousarray(np.broadcast_to(np.asarray(diff_lambda[l], f32)[None], (128, 4, 64))),
                  "subln": np.ascontiguousarray(np.broadcast_to(np.asarray(diff_subln[l], f32)[None, :], (128, 128))),
                  "lamc": np.ascontiguousarray(np.broadcast_to(np.array([[-lam_init, 1.0 - lam_init]], f32), (128, 2))),
                  "wgl": wgl, "wbr": wbr, "wout": wout, "wr": _tile_w(np.asarray(w_router[l], f32)),
                  "br": np.ascontiguousarray(np.broadcast_to(np.asarray(b_router[l], f32)[None, :], (128, 32)))}
        mapsB = []
        for core in range(NC):
            b, j = divmod(core, CPB)
            m = base_map(core, True)
            m.update(extraB)
            kprev = np.zeros((4, 128, 3 * T), bf)
            vprev = np.zeros((4, 128, 3 * NBLK, 130), bf)
            carry = np.zeros((128, 4, 3, 2), f32)
            carry[:, :, :, 0] = 1.0
            for q3 in range(3):
                jj = j - 3 + q3
                if jj >= 0:
                    r = resA[b * CPB + jj]
                    kprev[:, :, q3 * T:(q3 + 1) * T] = r["o_k"]
                    vprev[:, :, q3 * NBLK:(q3 + 1) * NBLK, :] = r["o_v"]
                    carry[:, :, q3, :] = r["o_c"]
            m["kprev"], m["vprev"], m["carry"] = kprev, vprev, carry
            mapsB.append(m)
        dbg = bool(_DEBUG.get("on"))
        ncB = build_mixer("B", T, debug=dbg)
        resB = _run(ncB, mapsB)
        if dbg:
            _DEBUG.setdefault("A", []).append(resA)
            _DEBUG.setdefault("B", []).append(resB)
        hT_all = np.ascontiguousarray(np.concatenate([r["o_hT"] for r in resB], axis=2))
        rw_all = np.concatenate([r["o_rw"] for r in resB], axis=0)
        mapsE = []
        for ec in range(NC):
            es = range(4 * ec, 4 * ec + 4)
            W1 = [np.asarray(w1[l, e], f32) for e in es]
            w1t = np.stack([np.stack([_tile_w(Wx[:, 0::2][:, 0:512]), _tile_w(Wx[:, 0::2][:, 512:1024]),
                                      _tile_w(Wx[:, 1::2][:, 0:512]), _tile_w(Wx[:, 1::2][:, 512:1024])]) for Wx in W1])
            W2 = [np.asarray(w2[l, e], f32) for e in es]
            w2t = np.stack([np.stack([_tile_w(Wx[:, 0:512]), _tile_w(Wx[:, 512:1024])]) for Wx in W2])
            b1t = np.stack([np.concatenate([_vec(np.asarray(b1[l, e], f32)[0::2], 8), _vec(np.asarray(b1[l, e], f32)[1::2], 8)],
                                           axis=1) for e in es], axis=1)
            b2t = np.stack([_vec(b2[l, e], 8) for e in es], axis=1)
            mapsE.append({"hT": hT_all, "rw": np.ascontiguousarray(rw_all[:, 4 * ec:4 * ec + 4].T), "w1": w1t, "w2": w2t,
                          "b1": np.ascontiguousarray(b1t), "b2": np.ascontiguousarray(b2t), "sel": sel})
        resE = _run(build_moe(NT), mapsE)
        if dbg:
            _DEBUG.setdefault("E", []).append(resE)
        mapsR = []
        for core in range(NC):
            yp = np.stack([resE[r]["o_y"][:, :, core * T:(core + 1) * T] for r in range(NC)])
            mapsR.append({"yp": np.ascontiguousarray(yp), "xT": resB[core]["o_xT"], "gf": resB[core]["o_gf"]})
        resR = _run(build_resid(T), mapsR)
        if _DEBUG.get("depth") == l + 1:
            depth_stop = True
        else:
            depth_stop = False
        xcur = np.concatenate([np.ascontiguousarray(r["o_x"].transpose(2, 1, 0)).reshape(T, 1024) for r in resR], axis=0)
        if depth_stop:
            break
    return np.ascontiguousarray(xcur.reshape(Bn, S, 1024).astype(f32))
```

```python
import contextlib
import math
import numpy as np
import ml_dtypes
import concourse.bass as bass
import concourse.mybir as mybir
from concourse.bass_utils import run_bass_kernel_spmd

F32 = mybir.dt.float32
BF16 = mybir.dt.bfloat16
AF = mybir.ActivationFunctionType
ALU = mybir.AluOpType
AX = mybir.AxisListType

D = 1024
NEXP = 32
DFF = 1024
EPS = 1e-6
NEG = -30000.0


class Buf:
    __slots__ = ("w", "r")

    def __init__(self):
        self.w = None
        self.r = {}


class KB:
    def __init__(self):
        self.nc = bass.Bass("TRN2", target_bir_lowering=False)
        self.es = contextlib.ExitStack()
        nc = self.nc
        self.eng = {"pe": nc.tensor, "act": nc.scalar, "dve": nc.vector, "pool": nc.gpsimd, "sp": nc.sync}
        self.sems = []
        self.esem = {}
        self.cnt = {}
        for e in self.eng:
            self.esem[e] = self._newsem("e_" + e)
            self.cnt[e] = 0
        self.seen = {e: {} for e in self.eng}
        self.ring = {}
        self.rpos = {}
        for q, n in (("sp", 12), ("act", 4), ("pool", 8)):
            self.ring[q] = [[self._newsem("d_%s%d" % (q, i)), 0] for i in range(n)]
            self.rpos[q] = 0
        self.out_tickets = []
        self.stack = [self.es]

    def push(self):
        self.stack.append(contextlib.ExitStack())

    def pop(self):
        self.barrier()
        self.stack.pop().close()

    def barrier(self):
        deps = []
        for e in self.eng:
            if self.cnt[e] > 0:
                deps.append((self.esem[e], self.cnt[e]))
        for q in self.ring:
            for slot in self.ring[q]:
                if slot[1] > 0:
                    deps.append((slot[0], slot[1]))
        for e in self.eng:
            self._wait(e, deps)

    def _newsem(self, name):
        s = self.es.enter_context(self.nc.semaphore(name))
        self.sems.append(s)
        return len(self.sems) - 1

    def sb(self, name, shape, dt):
        return self.stack[-1].enter_context(self.nc.sbuf_tensor(name, list(shape), dt))

    def ps(self, name, shape, dt=F32):
        return self.es.enter_context(self.nc.psum_tensor(name, list(shape), dt))

    def dram(self, name, shape, dt, kind):
        return self.nc.dram_tensor(name, list(shape), dt, kind=kind).ap()

    def _wait(self, e, deps):
        h = self.eng[e]
        best = {}
        for (s, v) in deps:
            if best.get(s, 0) < v:
                best[s] = v
        for s, v in best.items():
            if s == self.esem[e] and e in ("pe", "sp"):
                continue
            if self.seen[e].get(s, 0) < v:
                h.wait_ge(self.sems[s], v)
                self.seen[e][s] = v

    def _deps(self, reads, writes):
        deps = []
        for b in reads:
            if b.w is not None:
                deps.append(b.w)
        for b in writes:
            if b.w is not None:
                deps.append(b.w)
            deps.extend(b.r.items())
        return deps

    def _mark(self, t, reads, writes):
        for b in reads:
            if b.r.get(t[0], 0) < t[1]:
                b.r[t[0]] = t[1]
        for b in writes:
            b.w = t
            b.r = {}

    def op(self, e, fn, reads=(), writes=()):
        self._wait(e, self._deps(reads, writes))
        ins = fn(self.eng[e])
        self.cnt[e] += 1
        ins.then_inc(self.sems[self.esem[e]], 1)
        t = (self.esem[e], self.cnt[e])
        self._mark(t, reads, writes)
        return t

    def dma(self, q, out, in_, reads=(), writes=(), is_output=False, **kw):
        slot = self.ring[q][self.rpos[q]]
        self.rpos[q] = (self.rpos[q] + 1) % len(self.ring[q])
        deps = self._deps(reads, writes)
        if slot[1] > 0:
            deps.append((slot[0], slot[1]))
        self._wait(q, deps)
        ins = self.eng[q].dma_start(out=out, in_=in_, **kw)
        slot[1] += 16
        ins.then_inc(self.sems[slot[0]], 16)
        t = (slot[0], slot[1])
        self._mark(t, reads, writes)
        if is_output:
            self.out_tickets.append(t)
        return t

    def finish(self):
        deps = list(self.out_tickets)
        for e in self.eng:
            if self.cnt[e] > 0:
                deps.append((self.esem[e], self.cnt[e]))
        for q in self.ring:
            for slot in self.ring[q]:
                if slot[1] > 0:
                    deps.append((slot[0], slot[1]))
        self._wait("sp", deps)
        self.es.close()
        return self.nc


def groups_of(total, g):
    out = []
    s = 0
    while s < total:
        n = min(g, total - s)
        out.append((s, n))
        s += n
    return out


def build_mixer(mode, T, stop_after=None, debug=False):
    k = KB()
    NBLK = T // 128
    TT = T + 128
    NPB = 3 * NBLK
    G = min(512, T)
    full = mode == "B"
    og = groups_of(T, G)
    own_groups = [(128 + s, n) for (s, n) in og]
    tgroups = [(0, 128)] + own_groups

    d_xT = k.dram("xT", [128, 8, TT], F32, "ExternalInput")
    d_cv = k.dram("cvec", [128, 8], F32, "ExternalInput")
    NMOD = 48 if full else 16
    d_wada = k.dram("wada", [NMOD // 4, 128, 8, 512], F32, "ExternalInput")
    d_bada = k.dram("bada", [128, 48], F32, "ExternalInput")
    d_nmix = k.dram("nmix", [128, 8], F32, "ExternalInput")
    d_win = k.dram("win", [7, 128, 8, 512], F32, "ExternalInput")
    d_convw = k.dram("convw", [128, 4, 4], F32, "ExternalInput")
    d_lrus = k.dram("lrus", [128, 4, 4], F32, "ExternalInput")
    d_wlru = k.dram("wlru", [128, 2, 512], F32, "ExternalInput")
    d_qkg = k.dram("qkg", [128, 4], F32, "ExternalInput")
    d_flag = k.dram("flag", [128, 1], F32, "ExternalInput")
    d_id = k.dram("ident", [128, 128], F32, "ExternalInput")
    d_bd = k.dram("bdones", [128, 128], F32, "ExternalInput")
    if full:
        d_nffn = k.dram("nffn", [128, 8], F32, "ExternalInput")
        d_kprev = k.dram("kprev", [4, 128, NPB * 128], BF16, "ExternalInput")
        d_vprev = k.dram("vprev", [4, 128, NPB, 130], BF16, "ExternalInput")
        d_carry = k.dram("carry", [128, 4, 3, 2], F32, "ExternalInput")
        d_dbias = k.dram("dbias", [4, 128, 2, 2, 128], F32, "ExternalInput")
        d_b31 = k.dram("b31", [128, 4], F32, "ExternalInput")
        d_sbias = k.dram("sbias", [128, 8, 2, 128], F32, "ExternalInput")
        d_sink = k.dram("sinks", [128, 8], F32, "ExternalInput")
        d_dlam = k.dram("dlam", [128, 4, 64], F32, "ExternalInput")
        d_subln = k.dram("subln", [128, 128], F32, "ExternalInput")
        d_wgl = k.dram("wgl", [8, 128, 8, 384], F32, "ExternalInput")
        d_wbr = k.dram("wbr", [8, 128, 12, 128], F32, "ExternalInput")
        d_wout = k.dram("wout", [2, 128, 8, 512], F32, "ExternalInput")
        d_wr = k.dram("wr", [128, 8, 32], F32, "ExternalInput")
        d_br = k.dram("br", [128, 32], F32, "ExternalInput")
        d_lamc = k.dram("lamc", [128, 2], F32, "ExternalInput")
        if debug:
            o_dbg = k.dram("o_dbg", [128, 12, T], BF16, "ExternalOutput")
        o_xT = k.dram("o_xT", [128, 8, T], F32, "ExternalOutput")
        o_hT = k.dram("o_hT", [128, 8, T], BF16, "ExternalOutput")
        o_rw = k.dram("o_rw", [T, 32], F32, "ExternalOutput")
        o_gf = k.dram("o_gf", [128, 8], F32, "ExternalOutput")
    else:
        o_k = k.dram("o_k", [4, 128, T], BF16, "ExternalOutput")
        o_v = k.dram("o_v", [4, 128, NBLK, 130], BF16, "ExternalOutput")
        o_c = k.dram("o_c", [128, 4, 2], F32, "ExternalOutput")

    hT = k.sb("hT_s", [128, 8, TT], BF16); b_hT = Buf()
    cst = k.sb("cst", [128, 64], F32); b_cst = Buf()
    ident = k.sb("ident_s", [128, 128], F32); b_id = Buf()
    identb = k.sb("identb", [128, 128], BF16)
    bdones = k.sb("bdones_s", [128, 128], BF16); b_bd = Buf()
    ones = k.sb("ones_s", [128, 128], BF16); b_ones = Buf()
    mod = k.sb("mod", [128, 48], F32); b_mod = Buf()
    sm = k.sb("small", [128, 16], F32); b_sm = Buf()
    wst = k.sb("wst", [128, 8, 512], F32); b_wst = Buf()
    wbf = [k.sb("wbf%d" % i, [128, 8, 512], BF16) for i in range(3)]; b_wbf = [Buf(), Buf(), Buf()]
    sq = k.sb("sqs", [128, 8, G], BF16); b_sq = Buf()
    rstd = k.sb("rstd", [128, G], F32); b_rstd = Buf()
    tmpf = [k.sb("tmpf%d" % i, [128, G], F32) for i in range(2)]; b_tmpf = [Buf(), Buf()]
    tmpb = [k.sb("tmpb%d" % i, [128, G], BF16) for i in range(2)]; b_tmpb = [Buf(), Buf()]
    lsm = k.sb("lsm", [128, 16], F32); b_lsm = Buf()
    if full:
        obr = k.sb("obr", [128, 12, T], BF16); b_obr = Buf()
    pbank = [k.ps("pb%d" % i, [128, 512]) for i in range(8)]
    b_pb = [Buf() for _ in range(8)]

    k.dma("sp", ident[:], d_id, writes=[b_id])
    k.dma("sp", tmpf[0][:, 0:128], d_bd, writes=[b_tmpf[0]])
    k.op("dve", lambda e: e.tensor_copy(out=bdones[:], in_=tmpf[0][:, 0:128]), reads=[b_tmpf[0]], writes=[b_bd])
    k.op("dve", lambda e: e.memset(ones[:], 1.0), writes=[b_ones])
    k.op("dve", lambda e: e.tensor_copy(out=identb[:], in_=ident[:]), reads=[b_id], writes=[b_id])
    k.dma("sp", cst[:, 0:8], d_cv, writes=[b_cst])
    k.dma("sp", cst[:, 8:16], d_nmix, writes=[b_cst])
    k.dma("sp", cst[:, 24:40], d_convw.rearrange("p a b -> p (a b)"), writes=[b_cst])
    k.dma("sp", cst[:, 40:56], d_lrus.rearrange("p a b -> p (a b)"), writes=[b_cst])
    k.dma("sp", cst[:, 56:60], d_qkg, writes=[b_cst])
    k.dma("sp", cst[:, 60:61], d_flag, writes=[b_cst])
    k.dma("sp", mod[:], d_bada, writes=[b_mod])
    if full:
        k.dma("sp", cst[:, 16:24], d_nffn, writes=[b_cst])
    k.op("dve", lambda e: e.memset(cst[:, 61:62], EPS), writes=[b_cst])
    k.op("dve", lambda e: e.memset(cst[:, 62:63], 1.0), writes=[b_cst])
    convw = lambda c, tap: cst[:, 24 + c * 4 + tap: 25 + c * 4 + tap]
    lrus = lambda c, i: cst[:, 40 + c * 4 + i: 41 + c * 4 + i]
    epsc = cst[:, 61:62]
    onec = cst[:, 62:63]
    flag = cst[:, 60:61]

    k.op("act", lambda e: e.activation(out=cst[:, 0:8], in_=cst[:, 0:8], func=AF.Silu), reads=[b_cst], writes=[b_cst])
    for blk in range(NMOD // 4):
        k.dma("sp", wst[:], d_wada[blk], writes=[b_wst])
        pb = pbank[blk % 2]
        for fi in range(4):
            for kc in range(8):
                k.op("pe", lambda e, fi=fi, kc=kc: e.matmul(pb[:, fi:fi + 1], lhsT=wst[:, kc, fi * 128:(fi + 1) * 128],
                                                           rhs=cst[:, kc:kc + 1], start=(kc == 0), stop=(kc == 7)),
                     reads=[b_wst, b_cst], writes=[b_pb[blk % 2]])
        f0 = blk * 4
        k.op("dve", lambda e: e.tensor_tensor(out=mod[:, f0:f0 + 4], in0=pb[:, 0:4], in1=mod[:, f0:f0 + 4], op=ALU.add),
             reads=[b_pb[blk % 2], b_mod], writes=[b_mod])
    k.op("dve", lambda e: e.scalar_tensor_tensor(out=sm[:, 0:8], in0=mod[:, 8:16], scalar=1.0, in1=cst[:, 8:16],
                                                 op0=ALU.add, op1=ALU.mult), reads=[b_mod, b_cst], writes=[b_sm])
    if full:
        k.op("dve", lambda e: e.scalar_tensor_tensor(out=sm[:, 8:16], in0=mod[:, 32:40], scalar=1.0, in1=cst[:, 16:24],
                                                     op0=ALU.add, op1=ALU.mult), reads=[b_mod, b_cst], writes=[b_sm])

    def norm_mod(src, b_src, n, Acol, Bcol, dst_fn, b_dst, inplace_f32=False):
        for c in range(8):
            k.op("act", lambda e, c=c: e.activation(out=sq[:, c, 0:n], in_=src[:, c, 0:n], func=AF.Square),
                 reads=[b_src], writes=[b_sq])
        pb = pbank[2]
        for c in range(8):
            k.op("pe", lambda e, c=c: e.matmul(pb[:, 0:n], lhsT=ones[:], rhs=sq[:, c, 0:n], start=(c == 0), stop=(c == 7)),
                 reads=[b_sq, b_ones], writes=[b_pb[2]])
        k.op("act", lambda e: e.activation(out=rstd[:, 0:n], in_=pb[:, 0:n], func=AF.Sqrt, scale=1.0 / D, bias=epsc),
             reads=[b_pb[2], b_cst], writes=[b_rstd])
        k.op("dve", lambda e: e.reciprocal(out=rstd[:, 0:n], in_=rstd[:, 0:n]), reads=[b_rstd], writes=[b_rstd])
        for c in range(8):
            ti = c % 2
            k.op("dve", lambda e, c=c, ti=ti: e.scalar_tensor_tensor(out=tmpf[ti][:, 0:n], in0=src[:, c, 0:n],
                                                                    scalar=sm[:, Acol + c:Acol + c + 1], in1=rstd[:, 0:n],
                                                                    op0=ALU.mult, op1=ALU.mult),
                 reads=[b_src, b_sm, b_rstd], writes=[b_tmpf[ti]])
            if inplace_f32:
                k.op("act", lambda e, c=c, ti=ti: e.activation(out=src[:, c, 0:n], in_=tmpf[ti][:, 0:n], func=AF.Identity,
                                                               bias=mod[:, Bcol + c:Bcol + c + 1]),
                     reads=[b_tmpf[ti], b_mod], writes=[b_src])
                k.op("dve", lambda e, c=c: e.tensor_copy(out=dst_fn(c), in_=src[:, c, 0:n]), reads=[b_src], writes=[b_dst])
            else:
                k.op("act", lambda e, c=c, ti=ti: e.activation(out=dst_fn(c), in_=tmpf[ti][:, 0:n], func=AF.Identity,
                                                               bias=mod[:, Bcol + c:Bcol + c + 1]),
                     reads=[b_tmpf[ti], b_mod], writes=[b_dst])

    k.push()
    xg = k.sb("xg", [128, 8, 512], F32); b_xg = Buf()
    for (s, n) in tgroups:
        k.dma("sp", xg[:, :, 0:n], d_xT[:, :, s:s + n], writes=[b_xg])
        norm_mod(xg, b_xg, n, 0, 0, lambda c, s=s, n=n: hT[:, c, s:s + n], b_hT)
    k.pop()

    def load_w(slot, src, rows=8, cols=512):
        k.dma("sp", wst[:, 0:rows, 0:cols], src, writes=[b_wst])
        k.op("pool", lambda e: e.tensor_copy(out=wbf[slot][:, 0:rows, 0:cols], in_=wst[:, 0:rows, 0:cols]),
             reads=[b_wst], writes=[b_wbf[slot]])
        return wbf[slot], b_wbf[slot]

    pstate = {"i": 0}

    def next_pb():
        i = 4 + (pstate["i"] % 4)
        pstate["i"] += 1
        return pbank[i], b_pb[i]

    def linear_fm(w, b_w, chunks, src, b_src, KC, groups, evac):
        for (s, n) in groups:
            for nci in chunks:
                pb, bpb = next_pb()
                for kc in range(KC):
                    k.op("pe", lambda e, kc=kc, nci=nci: e.matmul(pb[:, 0:n], lhsT=w[:, kc, nci * 128:(nci + 1) * 128],
                                                                 rhs=src[:, kc, s:s + n], start=(kc == 0), stop=(kc == KC - 1)),
                         reads=[b_w, b_src], writes=[bpb])
                evac(nci, s, n, pb, bpb)

    def qknorm(dst_fn, b_dst, gcol):
        def ev(nci, s, n, pb, bpb):
            ti = nci % 2
            k.op("act", lambda e: e.activation(out=tmpb[ti][:, 0:n], in_=pb[:, 0:n], func=AF.Square),
                 reads=[bpb], writes=[b_tmpb[ti]])
            k.op("act", lambda e: e.activation(out=tmpf[ti][:, 0:n], in_=pb[:, 0:n], func=AF.Identity),
                 reads=[bpb], writes=[b_tmpf[ti]])
            p2 = pbank[3]
            k.op("pe", lambda e: e.matmul(p2[:, 0:n], lhsT=bdones[:], rhs=tmpb[ti][:, 0:n], start=True, stop=True),
                 reads=[b_bd, b_tmpb[ti]], writes=[b_pb[3]])
            k.op("act", lambda e: e.activation(out=rstd[:, 0:n], in_=p2[:, 0:n], func=AF.Sqrt, scale=1.0 / 64, bias=epsc),
                 reads=[b_pb[3], b_cst], writes=[b_rstd])
            k.op("dve", lambda e: e.reciprocal(out=rstd[:, 0:n], in_=rstd[:, 0:n]), reads=[b_rstd], writes=[b_rstd])
            k.op("dve", lambda e: e.scalar_tensor_tensor(out=dst_fn(nci, s, n), in0=tmpf[ti][:, 0:n],
                                                         scalar=cst[:, 56 + gcol:57 + gcol], in1=rstd[:, 0:n],
                                                         op0=ALU.mult, op1=ALU.mult),
                 reads=[b_tmpf[ti], b_cst, b_rstd], writes=[b_dst])
        return ev

    k.push()
    xr = k.sb("xr", [128, TT], F32); b_xr = Buf()
    xc = k.sb("xc", [128, T], F32); b_xc = Buf()
    xcb = k.sb("xcb", [128, 1, T], BF16); b_xcb = Buf()
    la = k.sb("la", [128, T], F32); b_la = Buf()
    bb = k.sb("bbuf", [128, T], F32); b_bb = Buf()
    hh = k.sb("hh", [128, T], F32); b_hh = Buf()
    wl = k.sb("wl", [128, 2, 512], BF16); b_wl = Buf()
    cout = k.sb("cout", [128, 4, 2], F32); b_cout = Buf()
    carry = k.sb("carry_s", [128, 4, 3, 2], F32); b_carry = Buf()
    w0, bw0 = load_w(0, d_win[0])
    w1, bw1 = load_w(1, d_win[1])
    k.dma("sp", wst[:, 0:2, :], d_wlru, writes=[b_wst])
    k.op("pool", lambda e: e.tensor_copy(out=wl[:], in_=wst[:, 0:2, :]), reads=[b_wst], writes=[b_wl])
    k.op("act", lambda e: e.activation(out=lsm[:, 0:4], in_=cst[:, 40:56].rearrange("p (c i) -> p c i", i=4)[:, :, 3],
                                       func=AF.Exp, scale=-1.0), reads=[b_cst], writes=[b_lsm])
    k.op("act", lambda e: e.activation(out=lsm[:, 0:4], in_=lsm[:, 0:4], func=AF.Ln, bias=onec), reads=[b_lsm, b_cst],
         writes=[b_lsm])
    k.op("dve", lambda e: e.tensor_scalar(out=lsm[:, 0:4], in0=lsm[:, 0:4], scalar1=-8.0, scalar2=None, op0=ALU.mult),
         reads=[b_lsm], writes=[b_lsm])
    k.op("dve", lambda e: e.memset(lsm[:, 8:12], 0.0), writes=[b_lsm])
    if full:
        k.dma("sp", carry[:], d_carry, writes=[b_carry])
        for kk in range(3):
            k.op("dve", lambda e, kk=kk: e.tensor_tensor(out=lsm[:, 8:12], in0=lsm[:, 8:12], in1=carry[:, :, kk, 0], op=ALU.mult),
                 reads=[b_lsm, b_carry], writes=[b_lsm])
            k.op("dve", lambda e, kk=kk: e.tensor_tensor(out=lsm[:, 8:12], in0=lsm[:, 8:12], in1=carry[:, :, kk, 1], op=ALU.add),
                 reads=[b_lsm, b_carry], writes=[b_lsm])
    C0 = 2.0 * math.sqrt(2.0 / math.pi)
    for c in range(4):
        def ev_xr(nci, s, n, pb, bpb):
            k.op("act", lambda e: e.activation(out=xr[:, s:s + n], in_=pb[:, 0:n], func=AF.Identity), reads=[bpb], writes=[b_xr])
        linear_fm(w0, bw0, [c], hT, b_hT, 8, tgroups, ev_xr)
        k.op("dve", lambda e: e.tensor_scalar(out=xr[:, 0:128], in0=xr[:, 0:128], scalar1=flag, scalar2=None, op0=ALU.mult),
             reads=[b_xr, b_cst], writes=[b_xr])
        k.op("dve", lambda e, c=c: e.tensor_scalar(out=xc[:], in0=xr[:, 125:125 + T], scalar1=convw(c, 0), scalar2=lrus(c, 0),
                                                   op0=ALU.mult, op1=ALU.add), reads=[b_xr, b_cst], writes=[b_xc])
        for tap in range(1, 4):
            k.op("dve", lambda e, c=c, tap=tap: e.scalar_tensor_tensor(out=xc[:], in0=xr[:, 125 + tap:125 + tap + T],
                                                                      scalar=convw(c, tap), in1=xc[:], op0=ALU.mult,
                                                                      op1=ALU.add), reads=[b_xr, b_cst, b_xc], writes=[b_xc])
        k.op("pool", lambda e: e.tensor_copy(out=xcb[:, 0, :], in_=xc[:]), reads=[b_xc], writes=[b_xcb])

        def ev_ga(nci, s, n, pb, bpb, c=c):
            k.op("act", lambda e: e.activation(out=la[:, s:s + n], in_=pb[:, 0:n], func=AF.Sigmoid, bias=lrus(c, 1)),
                 reads=[bpb, b_cst], writes=[b_la])

        def ev_gx(nci, s, n, pb, bpb, c=c):
            k.op("act", lambda e: e.activation(out=bb[:, s:s + n], in_=pb[:, 0:n], func=AF.Sigmoid, bias=lrus(c, 2)),
                 reads=[bpb, b_cst], writes=[b_bb])
        linear_fm(wl[:, 0:1, :], b_wl, [c], xcb, b_xcb, 1, og, ev_ga)
        linear_fm(wl[:, 1:2, :], b_wl, [c], xcb, b_xcb, 1, og, ev_gx)
        k.op("dve", lambda e, c=c: e.tensor_scalar(out=la[:], in0=la[:], scalar1=lsm[:, c:c + 1], scalar2=None, op0=ALU.mult),
             reads=[b_la, b_lsm], writes=[b_la])
        if not full:
            k.op("dve", lambda e, c=c: e.tensor_reduce(out=lsm[:, 12 + c:13 + c], in_=la[:], axis=AX.X, op=ALU.add),
                 reads=[b_la], writes=[b_lsm])
        k.op("act", lambda e: e.activation(out=hh[:], in_=la[:], func=AF.Exp, scale=2.0), reads=[b_la], writes=[b_hh])
        k.op("act", lambda e: e.activation(out=hh[:], in_=hh[:], func=AF.Sqrt, scale=-1.0, bias=onec), reads=[b_hh, b_cst],
             writes=[b_hh])
        k.op("act", lambda e: e.activation(out=la[:], in_=la[:], func=AF.Exp), reads=[b_la], writes=[b_la])
        k.op("dve", lambda e: e.tensor_tensor(out=bb[:], in0=bb[:], in1=xc[:], op=ALU.mult), reads=[b_bb, b_xc], writes=[b_bb])
        k.op("dve", lambda e: e.tensor_tensor(out=bb[:], in0=bb[:], in1=hh[:], op=ALU.mult), reads=[b_bb, b_hh], writes=[b_bb])
        k.op("dve", lambda e, c=c: e.tensor_tensor_scan(out=hh[:], data0=la[:], data1=bb[:], initial=lsm[:, 8 + c:9 + c],
                                                        op0=ALU.mult, op1=ALU.add),
             reads=[b_la, b_bb, b_lsm, b_hh], writes=[b_hh])
        if not full:
            k.op("dve", lambda e, c=c: e.tensor_copy(out=cout[:, c, 1:2], in_=hh[:, T - 1:T]), reads=[b_hh], writes=[b_cout])
        else:
            def ev_gr(nci, s, n, pb, bpb):
                ti = 0
                so = s - 128
                k.op("act", lambda e: e.activation(out=tmpf[ti][:, 0:n], in_=pb[:, 0:n], func=AF.Square), reads=[bpb],
                     writes=[b_tmpf[ti]])
                k.op("dve", lambda e: e.tensor_scalar(out=tmpf[ti][:, 0:n], in0=tmpf[ti][:, 0:n], scalar1=0.044715, scalar2=1.0,
                                                      op0=ALU.mult, op1=ALU.add), reads=[b_tmpf[ti]], writes=[b_tmpf[ti]])
                k.op("dve", lambda e: e.tensor_tensor(out=tmpf[ti][:, 0:n], in0=tmpf[ti][:, 0:n], in1=pb[:, 0:n], op=ALU.mult),
                     reads=[b_tmpf[ti], bpb], writes=[b_tmpf[ti]])
                k.op("act", lambda e: e.activation(out=tmpf[ti][:, 0:n], in_=tmpf[ti][:, 0:n], func=AF.Sigmoid, scale=C0),
                     reads=[b_tmpf[ti]], writes=[b_tmpf[ti]])
                k.op("dve", lambda e: e.tensor_tensor(out=tmpf[ti][:, 0:n], in0=tmpf[ti][:, 0:n], in1=pb[:, 0:n], op=ALU.mult),
                     reads=[b_tmpf[ti], bpb], writes=[b_tmpf[ti]])
                k.op("dve", lambda e: e.tensor_tensor(out=obr[:, nci, so:so + n], in0=tmpf[ti][:, 0:n], in1=hh[:, so:so + n],
                                                      op=ALU.mult), reads=[b_tmpf[ti], b_hh], writes=[b_obr])
            linear_fm(w1, bw1, [c], hT, b_hT, 8, own_groups, ev_gr)
    if not full:
        k.op("act", lambda e: e.activation(out=cout[:, :, 0], in_=lsm[:, 12:16], func=AF.Exp), reads=[b_lsm], writes=[b_cout])
        k.dma("sp", o_c, cout[:], reads=[b_cout], is_output=True)
    k.pop()

    if stop_after == "lru":
        return k.finish()
    k.push()
    kT = k.sb("kT", [128, T], BF16); b_kT = Buf()
    vA = k.sb("vA", [128, NBLK, 130], BF16); b_vA = Buf()
    wq, bwq = (load_w(0, d_win[2]) if full else (None, None))
    wk, bwk = load_w(1, d_win[3])
    wv, bwv = load_w(2, d_win[4])
    if full:
        qT = k.sb("qT", [128, T], BF16); b_qT = Buf()
        kprev = k.sb("kprev_s", [128, NPB * 128], BF16); b_kprev = Buf()
        vprev = k.sb("vprev_s", [128, NPB, 130], BF16); b_vprev = Buf()
        dbias = k.sb("dbias_s", [128, 2, 2, 128], F32); b_db = Buf()
        pT = [k.sb("pT%d" % i, [128, 2, 128], BF16) for i in range(2)]; b_pT = [Buf(), Buf()]
        sF = [k.sb("sF%d" % i, [128, 2, 128], F32) for i in range(2)]; b_sF = [Buf(), Buf()]
        acs = k.sb("acs", [128, 2, 130], F32); b_acs = Buf()
        otokb = k.sb("otokb", [128, 128], BF16); b_otokb = Buf()
        att = k.sb("att", [128, 32], F32); b_att = Buf()
        subln = k.sb("subln_s", [128, 128], F32); b_subln = Buf()
        dlam = k.sb("dlam_s", [128, 4, 64], F32); b_dlam = Buf()
        k.dma("sp", subln[:], d_subln, writes=[b_subln])
        k.dma("sp", dlam[:], d_dlam, writes=[b_dlam])
        k.dma("sp", att[:, 0:4], d_b31, writes=[b_att])
        k.dma("sp", att[:, 24:26], d_lamc, writes=[b_att])
        k.op("dve", lambda e: e.tensor_tensor(out=dlam[:, 0, :], in0=dlam[:, 0, :], in1=dlam[:, 1, :], op=ALU.mult),
             reads=[b_dlam], writes=[b_dlam])
        k.op("dve", lambda e: e.tensor_tensor(out=dlam[:, 2, :], in0=dlam[:, 2, :], in1=dlam[:, 3, :], op=ALU.mult),
             reads=[b_dlam], writes=[b_dlam])
        k.op("dve", lambda e: e.tensor_reduce(out=att[:, 14:15], in_=dlam[:, 0, :], axis=AX.X, op=ALU.add),
             reads=[b_dlam], writes=[b_att])
        k.op("dve", lambda e: e.tensor_reduce(out=att[:, 15:16], in_=dlam[:, 2, :], axis=AX.X, op=ALU.add),
             reads=[b_dlam], writes=[b_att])
        k.op("act", lambda e: e.activation(out=att[:, 14:16], in_=att[:, 14:16], func=AF.Exp), reads=[b_att], writes=[b_att])
        k.op("dve", lambda e: e.scalar_tensor_tensor(out=att[:, 13:14], in0=att[:, 15:16], scalar=att[:, 24:25], in1=att[:, 14:15],
                                                     op0=ALU.add, op1=ALU.subtract), reads=[b_att], writes=[b_att])
        k.op("dve", lambda e: e.tensor_scalar(out=subln[:], in0=subln[:], scalar1=att[:, 25:26], scalar2=None, op0=ALU.mult),
             reads=[b_subln, b_att], writes=[b_subln])
    for h in range(4):
        linear_fm(wk, bwk, [h], hT, b_hT, 8, own_groups, qknorm(lambda nci, s, n: kT[:, s - 128:s - 128 + n], b_kT, 1))
        k.op("dve", lambda e: e.memset(vA[:, :, 128:130], 1.0), writes=[b_vA])
        for blk in range(NBLK):
            pb, bpb = next_pb()
            for kc in range(8):
                k.op("pe", lambda e, kc=kc, blk=blk: e.matmul(pb[:, 0:128], lhsT=hT[:, kc, 128 + blk * 128:256 + blk * 128],
                                                             rhs=wv[:, kc, h * 128:(h + 1) * 128], start=(kc == 0), stop=(kc == 7)),
                     reads=[b_hT, bwv], writes=[bpb])
            k.op("act", lambda e, blk=blk: e.activation(out=vA[:, blk, 0:128], in_=pb[:, 0:128], func=AF.Identity),
                 reads=[bpb], writes=[b_vA])
        if not full:
            k.dma("sp", o_k[h], kT[:], reads=[b_kT], is_output=True)
            k.dma("sp", o_v[h], vA[:], reads=[b_vA], is_output=True)
            continue
        linear_fm(wq, bwq, [h], hT, b_hT, 8, own_groups, qknorm(lambda nci, s, n: qT[:, s - 128:s - 128 + n], b_qT, 0))
        k.dma("sp", kprev[:], d_kprev[h], writes=[b_kprev])
        k.dma("sp", vprev[:], d_vprev[h], writes=[b_vprev])
        k.dma("sp", dbias[:], d_dbias[h], writes=[b_db])
        for i in range(NBLK):
            kbl = [("p", j) for j in range(NPB)] + [("o", j) for j in range(i + 1)]
            acc = [pbank[0], pbank[1]]
            L = len(kbl)

            def d_info(idx):
                kind, j = kbl[idx]
                si = idx % 2
                if kind == "p":
                    return (si, kprev[:, j * 128:(j + 1) * 128], vprev[:, j, 0:129], b_kprev, b_vprev,
                            (1 if (i == 0 and j == NPB - 1) else None))
                return (si, kT[:, j * 128:(j + 1) * 128], vA[:, j, 0:129], b_kT, b_vA,
                        (0 if j == i else (1 if j == i - 1 else None)))

            def d_qk(idx):
                si, ksrc, vsrc, kb_, vb_, near = d_info(idx)
                pSm = [pbank[2 + si], pbank[4 + si]]
                bSm = [b_pb[2 + si], b_pb[4 + si]]
                for m in range(2):
                    k.op("pe", lambda e, m=m: e.matmul(pSm[m][:, 0:128], lhsT=ksrc[m * 64:(m + 1) * 64, :],
                                                       rhs=qT[m * 64:(m + 1) * 64, i * 128:(i + 1) * 128],
                                                       start=True, stop=True),
                         reads=[kb_, b_qT], writes=[bSm[m]])

            def d_exp(idx):
                si, ksrc, vsrc, kb_, vb_, near = d_info(idx)
                pSm = [pbank[2 + si], pbank[4 + si]]
                bSm = [b_pb[2 + si], b_pb[4 + si]]
                for m in range(2):
                    if near is None:
                        k.op("act", lambda e, m=m: e.activation(out=pT[si][:, m, :], in_=pSm[m][:, 0:128], func=AF.Exp,
                                                                scale=0.125, bias=att[:, h:h + 1]),
                             reads=[bSm[m], b_att], writes=[b_pT[si]])
                    else:
                        k.op("dve", lambda e, m=m: e.scalar_tensor_tensor(
                            out=sF[si][:, m, :], in0=pSm[m][:, 0:128], scalar=0.125, in1=dbias[:, near, m, :],
                            op0=ALU.mult, op1=ALU.add), reads=[bSm[m], b_db], writes=[b_sF[si]])
                        k.op("act", lambda e, m=m: e.activation(out=pT[si][:, m, :], in_=sF[si][:, m, :], func=AF.Exp),
                             reads=[b_sF[si]], writes=[b_pT[si]])

            def d_pv(idx):
                si, ksrc, vsrc, kb_, vb_, near = d_info(idx)
                for m in range(2):
                    k.op("pe", lambda e, m=m: e.matmul(acc[m][:, 0:129], lhsT=pT[si][:, m, :], rhs=vsrc,
                                                       start=(idx == 0), stop=(idx == L - 1)),
                         reads=[b_pT[si], vb_], writes=[b_pb[m]])

            d_qk(0)
            for idx in range(L):
                if idx + 1 < L:
                    d_qk(idx + 1)
                d_exp(idx)
                d_pv(idx)
            for m in range(2):
                k.op("act", lambda e, m=m: e.activation(out=acs[:, m, 0:129], in_=acc[m][:, 0:129], func=AF.Identity),
                     reads=[b_pb[m]], writes=[b_acs])
            k.op("dve", lambda e: e.reciprocal(out=att[:, 16:18], in_=acs[:, :, 128]), reads=[b_acs], writes=[b_att])
            k.op("dve", lambda e: e.tensor_tensor(out=att[:, 17:18], in0=att[:, 17:18], in1=att[:, 13:14], op=ALU.mult),
                 reads=[b_att], writes=[b_att])
            k.op("dve", lambda e: e.tensor_scalar(out=acs[:, 0, 0:128], in0=acs[:, 0, 0:128], scalar1=att[:, 16:17], scalar2=None,
                                                  op0=ALU.mult), reads=[b_acs, b_att], writes=[b_acs])
            k.op("dve", lambda e: e.scalar_tensor_tensor(out=acs[:, 0, 0:128], in0=acs[:, 1, 0:128], scalar=att[:, 17:18],
                                                         in1=acs[:, 0, 0:128], op0=ALU.mult, op1=ALU.add),
                 reads=[b_acs, b_att], writes=[b_acs])
            k.op("dve", lambda e: e.tensor_tensor(out=acs[:, 1, 0:128], in0=acs[:, 0, 0:128], in1=acs[:, 0, 0:128], op=ALU.mult),
                 reads=[b_acs], writes=[b_acs])
            k.op("dve", lambda e: e.tensor_reduce(out=att[:, 18:19], in_=acs[:, 1, 0:128], axis=AX.X, op=ALU.add),
                 reads=[b_acs], writes=[b_att])
            k.op("act", lambda e: e.activation(out=att[:, 18:19], in_=att[:, 18:19], func=AF.Sqrt, scale=1.0 / 128, bias=epsc),
                 reads=[b_att, b_cst], writes=[b_att])
            k.op("dve", lambda e: e.reciprocal(out=att[:, 18:19], in_=att[:, 18:19]), reads=[b_att], writes=[b_att])
            k.op("dve", lambda e: e.scalar_tensor_tensor(out=otokb[:], in0=acs[:, 0, 0:128], scalar=att[:, 18:19], in1=subln[:],
                                                         op0=ALU.mult, op1=ALU.mult),
                 reads=[b_acs, b_att, b_subln], writes=[b_otokb])
            pt, bpt = next_pb()
            k.op("pe", lambda e: e.matmul(pt[:, 0:128], lhsT=otokb[:], rhs=identb[:], start=True, stop=True),
                 reads=[b_otokb, b_id], writes=[bpt])
            k.op("act", lambda e, i=i: e.activation(out=obr[:, 4 + h, i * 128:(i + 1) * 128], in_=pt[:, 0:128], func=AF.Identity),
                 reads=[bpt], writes=[b_obr])
    k.pop()
    if not full or stop_after == "diff":
        return k.finish()

    k.push()
    sqT = k.sb("sqT", [128, 4, T], BF16); b_sqT = Buf()
    skT = k.sb("skT", [128, TT], BF16); b_skT = Buf()
    svA = k.sb("svA", [128, NBLK + 1, 2, 66], BF16); b_svA = Buf()
    sbias = k.sb("sbias_s", [128, 8, 2, 128], F32); b_sb = Buf()
    pT = [k.sb("spT%d" % i, [128, 2, 128], BF16) for i in range(2)]; b_pT = [Buf(), Buf()]
    sF = [k.sb("ssF%d" % i, [128, 2, 128], F32) for i in range(2)]; b_sF = [Buf(), Buf()]
    otokb = k.sb("sotokb", [128, 512], BF16); b_otokb = Buf()
    att = k.sb("satt", [128, 32], F32); b_att = Buf()
    k.dma("sp", sbias[:], d_sbias, writes=[b_sb])
    k.dma("sp", att[:, 4:12], d_sink, writes=[b_att])
    k.op("act", lambda e: e.activation(out=att[:, 4:12], in_=att[:, 4:12], func=AF.Exp), reads=[b_att], writes=[b_att])
    w, bw = load_w(0, d_win[5])
    linear_fm(w, bw, [0, 1, 2, 3], hT, b_hT, 8, own_groups,
              qknorm(lambda nci, s, n: sqT[:, nci, s - 128:s - 128 + n], b_sqT, 2))
    w, bw = load_w(1, d_win[6])
    linear_fm(w, bw, [0], hT, b_hT, 8, tgroups, qknorm(lambda nci, s, n: skT[:, s:s + n], b_skT, 3))
    k.op("dve", lambda e: e.memset(svA[:, :, :, 64:66], 1.0), writes=[b_svA])
    for blk in range(NBLK + 1):
        pb, bpb = next_pb()
        for kc in range(8):
            k.op("pe", lambda e, kc=kc, blk=blk: e.matmul(pb[:, 0:128], lhsT=hT[:, kc, blk * 128:(blk + 1) * 128],
                                                         rhs=w[:, kc, 128:256], start=(kc == 0), stop=(kc == 7)),
                 reads=[b_hT, bw], writes=[bpb])
        k.op("act", lambda e, blk=blk: e.activation(out=svA[:, blk, :, 0:64], in_=pb[:, 0:128].rearrange("p (h d) -> p h d", h=2),
                                                    func=AF.Identity), reads=[bpb], writes=[b_svA])
    k.op("dve", lambda e: e.tensor_scalar(out=svA[:, 0, :, :], in0=svA[:, 0, :, :], scalar1=flag, scalar2=None, op0=ALU.mult),
         reads=[b_svA, b_cst], writes=[b_svA])
    steps = [(i, hd) for i in range(NBLK) for hd in range(8)]

    def s_qk(st):
        i, hd = steps[st]
        kv, c, si = hd // 4, hd % 4, st % 2
        pS, bS = pbank[2 + si], b_pb[2 + si]
        for bi in range(2):
            k.op("pe", lambda e, bi=bi: e.matmul(
                pS[:, bi * 128:(bi + 1) * 128], lhsT=skT[kv * 64:(kv + 1) * 64, (i + bi) * 128:(i + bi + 1) * 128],
                rhs=sqT[kv * 64:(kv + 1) * 64, c, i * 128:(i + 1) * 128], start=True, stop=True),
                reads=[b_skT, b_sqT], writes=[bS])

    def s_soft(st):
        i, hd = steps[st]
        si = st % 2
        pS, bS = pbank[2 + si], b_pb[2 + si]
        pv = pS[:, 0:256].rearrange("p (m q) -> p m q", m=2)
        k.op("dve", lambda e: e.scalar_tensor_tensor(out=sF[si][:], in0=pv, scalar=0.125, in1=sbias[:, hd], op0=ALU.mult,
                                                     op1=ALU.add), reads=[bS, b_sb], writes=[b_sF[si]])
        k.op("act", lambda e: e.activation(out=pT[si][:], in_=sF[si][:], func=AF.Exp), reads=[b_sF[si]], writes=[b_pT[si]])

    def s_pv(st):
        i, hd = steps[st]
        kv, si = hd // 4, st % 2
        pa, ba_ = pbank[si], b_pb[si]
        for bi in range(2):
            k.op("pe", lambda e, bi=bi: e.matmul(pa[:, 0:65], lhsT=pT[si][:, bi, :], rhs=svA[:, i + bi, kv, 0:65],
                                                 start=(bi == 0), stop=(bi == 1)), reads=[b_pT[si], b_svA], writes=[ba_])
        k.op("dve", lambda e: e.tensor_tensor(out=att[:, 20 + hd:21 + hd], in0=pa[:, 64:65], in1=att[:, 4 + hd:5 + hd],
                                              op=ALU.add), reads=[ba_, b_att], writes=[b_att])
        k.op("dve", lambda e: e.reciprocal(out=att[:, 20 + hd:21 + hd], in_=att[:, 20 + hd:21 + hd]), reads=[b_att],
             writes=[b_att])
        k.op("dve", lambda e: e.tensor_scalar(out=otokb[:, hd * 64:(hd + 1) * 64], in0=pa[:, 0:64],
                                              scalar1=att[:, 20 + hd:21 + hd], scalar2=None, op0=ALU.mult),
             reads=[ba_, b_att], writes=[b_otokb])
        if hd == 7:
            for c in range(4):
                pt, bpt = next_pb()
                k.op("pe", lambda e, c=c: e.matmul(pt[:, 0:128], lhsT=otokb[:, c * 128:(c + 1) * 128], rhs=identb[:],
                                                   start=True, stop=True), reads=[b_otokb, b_id], writes=[bpt])
                k.op("act", lambda e, c=c: e.activation(out=obr[:, 8 + c, i * 128:(i + 1) * 128], in_=pt[:, 0:128],
                                                        func=AF.Identity), reads=[bpt], writes=[b_obr])

    s_qk(0)
    for st in range(len(steps)):
        if st + 1 < len(steps):
            s_qk(st + 1)
        s_soft(st)
        s_pv(st)
    k.pop()

    if debug:
        k.dma("sp", o_dbg, obr[:], reads=[b_obr], is_output=True)
    if stop_after == "swa":
        return k.finish()
    k.push()
    mg = k.sb("mg", [128, 8, G], BF16); b_mg = Buf()
    xg = k.sb("xg2", [128, 8, G], F32); b_xg = Buf()
    wo = k.sb("wo", [128, 8, 1024], BF16); b_wo = Buf()
    wr = k.sb("wr_s", [128, 8, 32], F32); b_wr = Buf()
    brt = k.sb("br_s", [128, 32], F32); b_br = Buf()
    lg = k.sb("lg", [128, 32], F32); b_lg = Buf()
    t8 = k.sb("t8", [128, 8], F32); b_t8 = Buf()
    rwt = k.sb("rwt", [128, 32], F32); b_rwt = Buf()
    k.dma("sp", wr[:], d_wr, writes=[b_wr])
    k.dma("sp", brt[:], d_br, writes=[b_br])
    k.dma("sp", o_gf, mod[:, 40:48], reads=[b_mod], is_output=True)
    for half in range(2):
        k.dma("sp", wst[:], d_wout[half], writes=[b_wst])
        k.op("pool", lambda e, half=half: e.tensor_copy(out=wo[:, :, half * 512:(half + 1) * 512], in_=wst[:]),
             reads=[b_wst], writes=[b_wo])
    for (s, n) in og:
        k.dma("sp", xg[:, :, 0:n], d_xT[:, :, 128 + s:128 + s + n], writes=[b_xg])
        for f in range(8):
            k.dma("sp", wst[:, :, 0:384], d_wgl[f], writes=[b_wst])
            k.dma("sp", wst[:, :, 384:512], d_wbr[f][:, 0:8, :], writes=[b_wst])
            k.op("pool", lambda e: e.tensor_copy(out=wbf[0][:], in_=wst[:]), reads=[b_wst], writes=[b_wbf[0]])
            k.dma("sp", wst[:, 0:4, 0:128], d_wbr[f][:, 8:12, :], writes=[b_wst])
            k.op("pool", lambda e: e.tensor_copy(out=wbf[1][:, 0:4, 0:128], in_=wst[:, 0:4, 0:128]), reads=[b_wst],
                 writes=[b_wbf[1]])
            wg, bwg = wbf[0], b_wbf[0]
            w2_, bw2_ = wbf[1], b_wbf[1]
            for nb in range(3):
                pg, bpg = next_pb()
                for kc in range(8):
                    k.op("pe", lambda e, kc=kc, nb=nb, pg=pg: e.matmul(pg[:, 0:n], lhsT=wg[:, kc, nb * 128:(nb + 1) * 128],
                                                                      rhs=hT[:, kc, 128 + s:128 + s + n],
                                                                      start=(kc == 0), stop=(kc == 7)),
                         reads=[bwg, b_hT], writes=[bpg])
                ti = nb % 2
                k.op("act", lambda e, ti=ti, pg=pg: e.activation(out=tmpf[ti][:, 0:n], in_=pg[:, 0:n], func=AF.Sigmoid),
                     reads=[bpg], writes=[b_tmpf[ti]])
                pp, bpp = next_pb()
                for kc in range(4):
                    if nb < 2:
                        lw, lb = wg[:, nb * 4 + kc, 384:512], bwg
                    else:
                        lw, lb = w2_[:, kc, 0:128], bw2_
                    k.op("pe", lambda e, kc=kc, lw=lw, nb=nb, pp=pp: e.matmul(pp[:, 0:n], lhsT=lw, rhs=obr[:, nb * 4 + kc, s:s + n],
                                                                             start=(kc == 0), stop=(kc == 3)),
                         reads=[lb, b_obr], writes=[bpp])
                if nb == 0:
                    k.op("dve", lambda e, ti=ti, pp=pp: e.tensor_tensor(out=rstd[:, 0:n], in0=pp[:, 0:n], in1=tmpf[ti][:, 0:n],
                                                                       op=ALU.mult), reads=[bpp, b_tmpf[ti]], writes=[b_rstd])
                else:
                    k.op("dve", lambda e, ti=ti, pp=pp: e.tensor_tensor(out=tmpf[ti][:, 0:n], in0=pp[:, 0:n],
                                                                       in1=tmpf[ti][:, 0:n], op=ALU.mult),
                         reads=[bpp, b_tmpf[ti]], writes=[b_tmpf[ti]])
                    if nb == 1:
                        k.op("dve", lambda e, ti=ti: e.tensor_tensor(out=rstd[:, 0:n], in0=rstd[:, 0:n], in1=tmpf[ti][:, 0:n],
                                                                    op=ALU.add), reads=[b_rstd, b_tmpf[ti]], writes=[b_rstd])
                    else:
                        k.op("dve", lambda e, ti=ti, f=f: e.tensor_tensor(out=mg[:, f, 0:n], in0=rstd[:, 0:n],
                                                                         in1=tmpf[ti][:, 0:n], op=ALU.add),
                             reads=[b_rstd, b_tmpf[ti]], writes=[b_mg])

        def ev_out(nci, s_, n_, pb, bpb):
            k.op("dve", lambda e: e.scalar_tensor_tensor(out=xg[:, nci, 0:n_], in0=pb[:, 0:n_], scalar=mod[:, 16 + nci:17 + nci],
                                                         in1=xg[:, nci, 0:n_], op0=ALU.mult, op1=ALU.add),
                 reads=[bpb, b_mod, b_xg], writes=[b_xg])
        linear_fm(wo, b_wo, list(range(8)), mg, b_mg, 8, [(0, n)], ev_out)
        k.dma("sp", o_xT[:, :, s:s + n], xg[:, :, 0:n], reads=[b_xg], is_output=True)
        norm_mod(xg, b_xg, n, 8, 24, lambda c, s=s, n=n: hT[:, c, 128 + s:128 + s + n], b_hT, inplace_f32=True)
        for tb in range(n // 128):
            pl, bpl = next_pb()
            for kc in range(8):
                k.op("pe", lambda e, kc=kc, tb=tb, pl=pl: e.matmul(pl[:, 0:32], lhsT=xg[:, kc, tb * 128:(tb + 1) * 128],
                                                                  rhs=wr[:, kc, :], start=(kc == 0), stop=(kc == 7)),
                     reads=[b_xg, b_wr], writes=[bpl])
            k.op("dve", lambda e, pl=pl: e.tensor_tensor(out=lg[:], in0=pl[:, 0:32], in1=brt[:], op=ALU.add), reads=[bpl, b_br],
                 writes=[b_lg])
            k.op("dve", lambda e: e.max(out=t8[:], in_=lg[:]), reads=[b_lg], writes=[b_t8])
            k.op("dve", lambda e: e.tensor_scalar(out=rwt[:], in0=lg[:], scalar1=t8[:, 3:4], scalar2=None, op0=ALU.is_ge),
                 reads=[b_lg, b_t8], writes=[b_rwt])
            k.op("dve", lambda e: e.tensor_scalar(out=lg[:], in0=lg[:], scalar1=t8[:, 0:1], scalar2=None, op0=ALU.subtract),
                 reads=[b_lg, b_t8], writes=[b_lg])
            k.op("act", lambda e: e.activation(out=lg[:], in_=lg[:], func=AF.Exp), reads=[b_lg], writes=[b_lg])
            k.op("dve", lambda e: e.tensor_tensor(out=rwt[:], in0=rwt[:], in1=lg[:], op=ALU.mult), reads=[b_rwt, b_lg],
                 writes=[b_rwt])
            k.op("dve", lambda e: e.tensor_reduce(out=t8[:, 4:5], in_=rwt[:], axis=AX.X, op=ALU.add), reads=[b_rwt],
                 writes=[b_t8])
            k.op("dve", lambda e: e.reciprocal(out=t8[:, 4:5], in_=t8[:, 4:5]), reads=[b_t8], writes=[b_t8])
            k.op("dve", lambda e: e.tensor_scalar(out=rwt[:], in0=rwt[:], scalar1=t8[:, 4:5], scalar2=None, op0=ALU.mult),
                 reads=[b_rwt, b_t8], writes=[b_rwt])
            r0 = s + tb * 128
            k.dma("sp", o_rw[r0:r0 + 128, :], rwt[:], reads=[b_rwt], is_output=True)
    for (s, n) in groups_of(T, 512):
        k.dma("sp", o_hT[:, :, s:s + n], hT[:, :, 128 + s:128 + s + n], reads=[b_hT], is_output=True)
    k.pop()
    return k.finish()

def build_moe(NT):
    k = KB()
    G = 512
    NG = NT // G
    d_hT = k.dram("hT", [128, 8, NT], BF16, "ExternalInput")
    d_rw = k.dram("rw", [4, NT], F32, "ExternalInput")
    d_w1 = k.dram("w1", [4, 4, 128, 8, 512], F32, "ExternalInput")
    d_w2 = k.dram("w2", [4, 2, 128, 8, 512], F32, "ExternalInput")
    d_b1 = k.dram("b1", [128, 4, 16], F32, "ExternalInput")
    d_b2 = k.dram("b2", [128, 4, 8], F32, "ExternalInput")
    d_sel = k.dram("sel", [4, 4, 128], F32, "ExternalInput")
    o_y = k.dram("o_y", [128, 8, NT], BF16, "ExternalOutput")
    scr = k.dram("yscr", [128, 8, NT], F32, "Internal")

    ALPHA = 1.702
    C7 = ALPHA * 7.0 / (1.0 + math.exp(-ALPHA * 7.0))
    w1 = k.sb("w1s", [128, 8, 2048], BF16); b_w1 = Buf()
    w2 = k.sb("w2s", [128, 8, 1024], BF16); b_w2 = Buf()
    wst = k.sb("wst", [128, 8, 512], F32); b_wst = Buf()
    hg = [k.sb("hg%d" % i, [128, 8, G], BF16) for i in range(2)]; b_hg = [Buf(), Buf()]
    act = [k.sb("act%d" % i, [128, 8, G], BF16) for i in range(2)]; b_act = [Buf(), Buf()]
    ysb = k.sb("ysb", [128, 8, G], F32); b_ysb = Buf()
    ypv = k.sb("ypv", [128, 8, G], F32); b_ypv = Buf()
    y16 = k.sb("y16", [128, 8, G], BF16); b_y16 = Buf()
    wbc = [k.sb("wbc%d" % i, [128, G], F32) for i in range(2)]; b_wbc = [Buf(), Buf()]
    rwg = [k.sb("rws%d" % i, [4, G], F32) for i in range(2)]; b_rwg = [Buf(), Buf()]
    sel = k.sb("sels", [4, 4, 128], F32); b_sel = Buf()
    b1 = k.sb("b1s", [128, 4, 16], F32); b_b1 = Buf()
    b2 = k.sb("b2s", [128, 4, 8], F32); b_b2 = Buf()
    s1 = [k.sb("s1_%d" % i, [128, G], BF16) for i in range(2)]; b_s1 = [Buf(), Buf()]
    s1c = [k.sb("s1c%d" % i, [128, G], BF16) for i in range(2)]; b_s1c = [Buf(), Buf()]
    tl = [k.sb("tl%d" % i, [128, G], BF16) for i in range(2)]; b_tl = [Buf(), Buf()]
    uu = [k.sb("uu%d" % i, [128, G], BF16) for i in range(2)]; b_uu = [Buf(), Buf()]
    pbank = [k.ps("pb%d" % i, [128, 512]) for i in range(8)]
    b_pb = [Buf() for _ in range(8)]
    b_y = [Buf() for _ in range(NG)]

    k.dma("sp", sel[:], d_sel, writes=[b_sel])
    k.dma("sp", b1[:], d_b1, writes=[b_b1])
    k.dma("sp", b2[:], d_b2, writes=[b_b2])
    k.op("dve", lambda e: e.tensor_scalar(out=b1[:, :, 0:8], in0=b1[:, :, 0:8], scalar1=ALPHA, scalar2=None, op0=ALU.mult),
         reads=[b_b1], writes=[b_b1])
    k.op("dve", lambda e: e.tensor_scalar(out=b1[:, :, 8:16], in0=b1[:, :, 8:16], scalar1=1.0, scalar2=None, op0=ALU.add),
         reads=[b_b1], writes=[b_b1])
    k.op("dve", lambda e: e.tensor_scalar(out=b2[:], in0=b2[:], scalar1=ALPHA, scalar2=None, op0=ALU.mult),
         reads=[b_b2], writes=[b_b2])
    pi = [0]

    def npb(lo, hi):
        i = lo + (pi[0] % (hi - lo))
        pi[0] += 1
        return pbank[i], b_pb[i]

    def stage1(ex, g):
        gi = g % 2
        s = g * G
        k.dma("sp", hg[gi][:], d_hT[:, :, s:s + G], writes=[b_hg[gi]])
        k.dma("sp", rwg[gi][:], d_rw[:, s:s + G], writes=[b_rwg[gi]])
        pw = pbank[7]
        k.op("pe", lambda e: e.matmul(pw[:, :], lhsT=sel[:, ex, :], rhs=rwg[gi][:], start=True, stop=True),
             reads=[b_sel, b_rwg[gi]], writes=[b_pb[7]])
        k.op("act", lambda e: e.activation(out=wbc[gi][:], in_=pw[:, :], func=AF.Identity, scale=1.0 / ALPHA),
             reads=[b_pb[7]], writes=[b_wbc[gi]])
        for ci in range(8):
            ti = ci % 2
            pg, bpg = npb(0, 3)
            for kc in range(8):
                k.op("pe", lambda e, kc=kc: e.matmul(pg[:, :], lhsT=w1[:, kc, ci * 128:(ci + 1) * 128], rhs=hg[gi][:, kc, :],
                                                     start=(kc == 0), stop=(kc == 7)), reads=[b_w1, b_hg[gi]], writes=[bpg])
            pl, bpl = npb(0, 3)
            for kc in range(8):
                k.op("pe", lambda e, kc=kc: e.matmul(pl[:, :], lhsT=w1[:, kc, 1024 + ci * 128:1024 + (ci + 1) * 128],
                                                     rhs=hg[gi][:, kc, :], start=(kc == 0), stop=(kc == 7)),
                     reads=[b_w1, b_hg[gi]], writes=[bpl])
            k.op("act", lambda e: e.activation(out=s1[ti][:], in_=pg[:, :], func=AF.Silu, scale=ALPHA, bias=b1[:, ex, ci:ci + 1]),
                 reads=[bpg, b_b1], writes=[b_s1[ti]])
            k.op("pool", lambda e: e.tensor_scalar(out=s1c[ti][:], in0=s1[ti][:], scalar1=C7, scalar2=-1.0e30, op0=ALU.min,
                                                   op1=ALU.max), reads=[b_s1[ti]], writes=[b_s1c[ti]])
            k.op("dve", lambda e: e.tensor_scalar(out=tl[ti][:], in0=pl[:, :], scalar1=b1[:, ex, 8 + ci:9 + ci], scalar2=8.0,
                                                  op0=ALU.add, op1=ALU.min), reads=[bpl, b_b1], writes=[b_tl[ti]])
            k.op("dve", lambda e: e.scalar_tensor_tensor(out=uu[ti][:], in0=tl[ti][:], scalar=-6.0, in1=s1c[ti][:],
                                                         op0=ALU.max, op1=ALU.mult), reads=[b_tl[ti], b_s1c[ti]],
                 writes=[b_uu[ti]])
            k.op("pool", lambda e: e.tensor_tensor(out=act[gi][:, ci, :], in0=uu[ti][:], in1=wbc[gi][:], op=ALU.mult),
                 reads=[b_uu[ti], b_wbc[gi]], writes=[b_act[gi]])

    def stage2(ex, g):
        gi = g % 2
        s = g * G
        if ex > 0:
            k.dma("sp", ypv[:], scr[:, :, s:s + G], reads=[b_y[g]], writes=[b_ypv])
        for f in range(8):
            py, bpy = npb(3, 7)
            for kc in range(8):
                k.op("pe", lambda e, kc=kc: e.matmul(py[:, :], lhsT=w2[:, kc, f * 128:(f + 1) * 128], rhs=act[gi][:, kc, :],
                                                     start=(kc == 0), stop=(kc == 7)), reads=[b_w2, b_act[gi]], writes=[bpy])
            k.op("dve", lambda e: e.scalar_tensor_tensor(out=ysb[:, f, :], in0=wbc[gi][:], scalar=b2[:, ex, f:f + 1], in1=py[:, :],
                                                         op0=ALU.mult, op1=ALU.add), reads=[b_wbc[gi], b_b2, bpy], writes=[b_ysb])
            if ex > 0:
                k.op("pool", lambda e: e.tensor_tensor(out=ysb[:, f, :], in0=ysb[:, f, :], in1=ypv[:, f, :], op=ALU.add),
                     reads=[b_ysb, b_ypv], writes=[b_ysb])
        if ex < 3:
            k.dma("sp", scr[:, :, s:s + G], ysb[:], reads=[b_ysb], writes=[b_y[g]])
        else:
            k.op("act", lambda e: e.activation(out=y16[:], in_=ysb[:], func=AF.Identity), reads=[b_ysb], writes=[b_y16])
            k.dma("sp", o_y[:, :, s:s + G], y16[:], reads=[b_y16], is_output=True)

    for ex in range(4):
        for blk in range(4):
            k.dma("sp", wst[:], d_w1[ex, blk], writes=[b_wst])
            k.op("pool", lambda e, blk=blk: e.tensor_copy(out=w1[:, :, blk * 512:(blk + 1) * 512], in_=wst[:]),
                 reads=[b_wst], writes=[b_w1])
        for blk in range(2):
            k.dma("sp", wst[:], d_w2[ex, blk], writes=[b_wst])
            k.op("pool", lambda e, blk=blk: e.tensor_copy(out=w2[:, :, blk * 512:(blk + 1) * 512], in_=wst[:]),
                 reads=[b_wst], writes=[b_w2])
        stage1(ex, 0)
        for g in range(NG):
            if g + 1 < NG:
                stage1(ex, g + 1)
            stage2(ex, g)
    return k.finish()


def build_resid(T):
    k = KB()
    d_y = k.dram("yp", [8, 128, 8, T], BF16, "ExternalInput")
    d_x = k.dram("xT", [128, 8, T], F32, "ExternalInput")
    d_g = k.dram("gf", [128, 8], F32, "ExternalInput")
    o_x = k.dram("o_x", [128, 8, T], F32, "ExternalOutput")
    G = min(512, T)
    acc = [k.sb("acc%d" % i, [128, 8, G], F32) for i in range(2)]; b_acc = [Buf(), Buf()]
    yb = [k.sb("yb%d" % i, [128, 8, G], BF16) for i in range(3)]; b_yb = [Buf(), Buf(), Buf()]
    xb = [k.sb("xb%d" % i, [128, 8, G], F32) for i in range(2)]; b_xb = [Buf(), Buf()]
    gf = k.sb("gfs", [128, 8], F32); b_gf = Buf()
    k.dma("sp", gf[:], d_g, writes=[b_gf])
    n_y = 0
    for gi, (s, n) in enumerate(groups_of(T, G)):
        a = gi % 2
        k.dma("sp", xb[a][:, :, 0:n], d_x[:, :, s:s + n], writes=[b_xb[a]])
        k.dma("sp", yb[2][:, :, 0:n], d_y[0, :, :, s:s + n], writes=[b_yb[2]])
        k.op("dve", lambda e: e.tensor_copy(out=acc[a][:, :, 0:n], in_=yb[2][:, :, 0:n]), reads=[b_yb[2]], writes=[b_acc[a]])
        for r in range(1, 8):
            yi = n_y % 2
            n_y += 1
            k.dma("sp", yb[yi][:, :, 0:n], d_y[r, :, :, s:s + n], writes=[b_yb[yi]])
            k.op("dve", lambda e: e.tensor_tensor(out=acc[a][:, :, 0:n], in0=acc[a][:, :, 0:n], in1=yb[yi][:, :, 0:n], op=ALU.add),
                 reads=[b_acc[a], b_yb[yi]], writes=[b_acc[a]])
        for c in range(8):
            k.op("dve", lambda e, c=c: e.scalar_tensor_tensor(out=xb[a][:, c, 0:n], in0=acc[a][:, c, 0:n], scalar=gf[:, c:c + 1],
                                                             in1=xb[a][:, c, 0:n], op0=ALU.mult, op1=ALU.add),
                 reads=[b_acc[a], b_gf, b_xb[a]], writes=[b_xb[a]])
        k.dma("sp", o_x[:, :, s:s + n], xb[a][:, :, 0:n], reads=[b_xb[a]], is_output=True)
    return k.finish()

def _tile_w(W):
    K_, N_ = W.shape
    return np.ascontiguousarray(W.reshape(K_ // 128, 128, N_).transpose(1, 0, 2))


def _vec(v, nch):
    return np.ascontiguousarray(np.asarray(v).reshape(nch, 128).T)


def _t5_bucket(dist):
    dist = np.asarray(dist)
    lr = np.log(np.maximum(dist, 1).astype(np.float32) / np.float32(16)) / np.float32(math.log(128 / 16))
    large = 16 + (lr * np.float32(16)).astype(np.int32)
    return np.where(dist < 16, dist, np.minimum(large, 31))


def _run(nc, maps):
    res = run_bass_kernel_spmd(nc, maps, core_ids=list(range(len(maps))))
    return res.results


_DEBUG = {}


def kernel(x, c, w_ada, b_ada, norm_mix, norm_ffn, w_in, conv_w, conv_b, lru_wa, lru_ba, lru_wx, lru_bx, lru_lambda,
           diff_qnorm, diff_knorm, diff_lambda, diff_subln, swa_qnorm, swa_knorm, swa_sinks, rel_bias, w_branch, w_out,
           w_router, b_router, w1, b1, w2, b2):
    f32 = np.float32
    x = np.asarray(x, f32)
    Bn, S, _ = x.shape
    NC = 8
    CPB = NC // Bn
    T = S // CPB
    NBLK = T // 128
    NT = Bn * S
    depth = w_ada.shape[0]
    bf = ml_dtypes.bfloat16
    ident = np.eye(128, dtype=f32)
    bdones = np.zeros((128, 128), f32)
    bdones[:64, :64] = 1.0
    bdones[64:, 64:] = 1.0
    rel_bias = np.asarray(rel_bias, f32)
    qq = np.arange(128)[None, :]
    kk = np.arange(128)[:, None]
    dbias = np.zeros((4, 128, 2, 2, 128), f32)
    d0 = qq - kk
    d1 = 128 + qq - kk
    for h in range(4):
        diag = np.where(d0 >= 0, rel_bias[_t5_bucket(np.maximum(d0, 0)), h], f32(NEG))
        prev = rel_bias[_t5_bucket(d1), h]
        for m in range(2):
            dbias[h, :, 0, m, :] = diag
            dbias[h, :, 1, m, :] = prev
    b31 = np.ascontiguousarray(np.broadcast_to(rel_bias[31, 0:4][None, :], (128, 4))).astype(f32)
    sbias = np.zeros((128, 8, 2, 128), f32)
    for hd in range(8):
        sbias[:, hd, 0, :] = np.where(d1 < 128, rel_bias[_t5_bucket(np.clip(d1, 0, 127)), 4 + hd], f32(NEG))
        sbias[:, hd, 1, :] = np.where(d0 >= 0, rel_bias[_t5_bucket(np.clip(d0, 0, 127)), 4 + hd], f32(NEG))
    sel = np.zeros((4, 4, 128), f32)
    for e in range(4):
        sel[e, e, :] = 1.0
    perm = np.concatenate([np.concatenate([np.arange(cc * 64, (cc + 1) * 64), np.arange((cc + 4) * 64, (cc + 5) * 64)])
                           for cc in range(4)])

    xcur = x.reshape(NT, 1024)
    for l in range(depth):
        lam_init = 0.8 - 0.6 * math.exp(-0.3 * l)
        W = np.asarray(w_in[l], f32)
        blk6 = np.concatenate([W[:, 3072:3328], np.zeros((1024, 256), f32)], axis=1)
        win = np.stack([_tile_w(W[:, 0:512]), _tile_w(W[:, 512:1024]), _tile_w(W[:, 1024:1536]), _tile_w(W[:, 1536:2048]),
                        _tile_w(W[:, 2048:2560]), _tile_w(W[:, 2560:3072][:, perm]), _tile_w(blk6)])
        wada_full = np.stack([_tile_w(np.asarray(w_ada[l], f32)[:, i * 512:(i + 1) * 512]) for i in range(12)])
        bada = _vec(b_ada[l], 48)
        convw = np.ascontiguousarray(np.asarray(conv_w[l], f32).reshape(4, 4, 128).transpose(2, 1, 0))
        lrus = np.stack([_vec(conv_b[l], 4), _vec(lru_ba[l], 4), _vec(lru_bx[l], 4), _vec(lru_lambda[l], 4)], axis=2)
        wlru = np.zeros((128, 2, 512), f32)
        for wi, Wl in enumerate((lru_wa[l], lru_wx[l])):
            Wl = np.asarray(Wl, f32)
            for cc in range(4):
                for hb in range(2):
                    wlru[hb * 64:(hb + 1) * 64, wi, cc * 128 + hb * 64:cc * 128 + (hb + 1) * 64] = Wl[2 * cc + hb]
        qkg = np.stack([np.tile(np.asarray(g[l], f32), 2) for g in (diff_qnorm, diff_knorm, swa_qnorm, swa_knorm)], axis=1)
        common = {"wada": None, "bada": bada, "nmix": _vec(norm_mix[l], 8), "win": win, "convw": convw,
                  "lrus": np.ascontiguousarray(lrus), "wlru": wlru, "qkg": np.ascontiguousarray(qkg), "ident": ident,
                  "bdones": bdones}

        def xT_of(core):
            b, j = divmod(core, CPB)
            start = b * S + j * T
            xs = np.zeros((T + 128, 1024), f32)
            xs[128:] = xcur[start:start + T]
            if j > 0:
                xs[:128] = xcur[start - 128:start]
            return np.ascontiguousarray(xs.T.reshape(8, 128, T + 128).transpose(1, 0, 2))

        def base_map(core, full):
            b, j = divmod(core, CPB)
            m = dict(common)
            m["wada"] = wada_full if full else np.ascontiguousarray(wada_full[:4])
            m["xT"] = xT_of(core)
            m["cvec"] = _vec(np.asarray(c, f32)[b], 8)
            m["flag"] = np.full((128, 1), 1.0 if j > 0 else 0.0, f32)
            return m

        ncA = build_mixer("A", T)
        resA = _run(ncA, [base_map(core, False) for core in range(NC)])
        Wg = W[:, 3328:6400]
        wgl = np.stack([_tile_w(np.concatenate([Wg[:, n * 1024 + f * 128:n * 1024 + (f + 1) * 128] for n in range(3)], axis=1))
                        for f in range(8)])
        Wb = np.asarray(w_branch[l], f32)
        wbr = np.stack([np.concatenate([_tile_w(Wb[n][:, f * 128:(f + 1) * 128]) for n in range(3)], axis=1) for f in range(8)])
        Wo = np.asarray(w_out[l], f32)
        wout = np.stack([_tile_w(Wo[:, 0:512]), _tile_w(Wo[:, 512:1024])])
        extraB = {"nffn": _vec(norm_ffn[l], 8), "dbias": dbias, "b31": b31, "sbias": sbias,
                  "sinks": np.ascontiguousarray(np.broadcast_to(np.asarray(swa_sinks[l], f32)[None, :], (128, 8))),
                  "dlam": np.ascontiguousarray(np.broadcast_to(np.asarray(diff_lambda[l], f32)[None], (128, 4, 64))),
                  "subln": np.ascontiguousarray(np.broadcast_to(np.asarray(diff_subln[l], f32)[None, :], (128, 128))),
                  "lamc": np.ascontiguousarray(np.broadcast_to(np.array([[-lam_init, 1.0 - lam_init]], f32), (128, 2))),
                  "wgl": wgl, "wbr": wbr, "wout": wout, "wr": _tile_w(np.asarray(w_router[l], f32)),
                  "br": np.ascontiguousarray(np.broadcast_to(np.asarray(b_router[l], f32)[None, :], (128, 32)))}
        mapsB = []
        for core in range(NC):
            b, j = divmod(core, CPB)
            m = base_map(core, True)
            m.update(extraB)
            kprev = np.zeros((4, 128, 3 * T), bf)
            vprev = np.zeros((4, 128, 3 * NBLK, 130), bf)
            carry = np.zeros((128, 4, 3, 2), f32)
            carry[:, :, :, 0] = 1.0
            for q3 in range(3):
                jj = j - 3 + q3
                if jj >= 0:
                    r = resA[b * CPB + jj]
                    kprev[:, :, q3 * T:(q3 + 1) * T] = r["o_k"]
                    vprev[:, :, q3 * NBLK:(q3 + 1) * NBLK, :] = r["o_v"]
                    carry[:, :, q3, :] = r["o_c"]
            m["kprev"], m["vprev"], m["carry"] = kprev, vprev, carry
            mapsB.append(m)
        dbg = bool(_DEBUG.get("on"))
        ncB = build_mixer("B", T, debug=dbg)
        resB = _run(ncB, mapsB)
        if dbg:
            _DEBUG.setdefault("A", []).append(resA)
            _DEBUG.setdefault("B", []).append(resB)
        hT_all = np.ascontiguousarray(np.concatenate([r["o_hT"] for r in resB], axis=2))
        rw_all = np.concatenate([r["o_rw"] for r in resB], axis=0)
        mapsE = []
        for ec in range(NC):
            es = range(4 * ec, 4 * ec + 4)
            W1 = [np.asarray(w1[l, e], f32) for e in es]
            w1t = np.stack([np.stack([_tile_w(Wx[:, 0::2][:, 0:512]), _tile_w(Wx[:, 0::2][:, 512:1024]),
                                      _tile_w(Wx[:, 1::2][:, 0:512]), _tile_w(Wx[:, 1::2][:, 512:1024])]) for Wx in W1])
            W2 = [np.asarray(w2[l, e], f32) for e in es]
            w2t = np.stack([np.stack([_tile_w(Wx[:, 0:512]), _tile_w(Wx[:, 512:1024])]) for Wx in W2])
            b1t = np.stack([np.concatenate([_vec(np.asarray(b1[l, e], f32)[0::2], 8), _vec(np.asarray(b1[l, e], f32)[1::2], 8)],
                                           axis=1) for e in es], axis=1)
            b2t = np.stack([_vec(b2[l, e], 8) for e in es], axis=1)
            mapsE.append({"hT": hT_all, "rw": np.ascontiguousarray(rw_all[:, 4 * ec:4 * ec + 4].T), "w1": w1t, "w2": w2t,
                          "b1": np.ascontiguousarray(b1t), "b2": np.ascontiguousarray(b2t), "sel": sel})
        resE = _run(build_moe(NT), mapsE)
        if dbg:
            _DEBUG.setdefault("E", []).append(resE)
        mapsR = []
        for core in range(NC):
            yp = np.stack([resE[r]["o_y"][:, :, core * T:(core + 1) * T] for r in range(NC)])
            mapsR.append({"yp": np.ascontiguousarray(yp), "xT": resB[core]["o_xT"], "gf": resB[core]["o_gf"]})
        resR = _run(build_resid(T), mapsR)
        if _DEBUG.get("depth") == l + 1:
            depth_stop = True
        else:
            depth_stop = False
        xcur = np.concatenate([np.ascontiguousarray(r["o_x"].transpose(2, 1, 0)).reshape(T, 1024) for r in resR], axis=0)
        if depth_stop:
            break
    return np.ascontiguousarray(xcur.reshape(Bn, S, 1024).astype(f32))
```

```python
import contextlib
import math
import numpy as np
import ml_dtypes
import concourse.bass as bass
import concourse.mybir as mybir
from concourse.bass_utils import run_bass_kernel_spmd

F32 = mybir.dt.float32
BF16 = mybir.dt.bfloat16
AF = mybir.ActivationFunctionType
ALU = mybir.AluOpType
AX = mybir.AxisListType

D = 1024
NEXP = 32
DFF = 1024
EPS = 1e-6
NEG = -30000.0


class Buf:
    __slots__ = ("w", "r")

    def __init__(self):
        self.w = None
        self.r = {}


class KB:
    def __init__(self):
        self.nc = bass.Bass("TRN2", target_bir_lowering=False)
        self.es = contextlib.ExitStack()
        nc = self.nc
        self.eng = {"pe": nc.tensor, "act": nc.scalar, "dve": nc.vector, "pool": nc.gpsimd, "sp": nc.sync}
        self.sems = []
        self.esem = {}
        self.cnt = {}
        for e in self.eng:
            self.esem[e] = self._newsem("e_" + e)
            self.cnt[e] = 0
        self.seen = {e: {} for e in self.eng}
        self.ring = {}
        self.rpos = {}
        for q, n in (("sp", 12), ("act", 4), ("pool", 8)):
            self.ring[q] = [[self._newsem("d_%s%d" % (q, i)), 0] for i in range(n)]
            self.rpos[q] = 0
        self.out_tickets = []
        self.stack = [self.es]

    def push(self):
        self.stack.append(contextlib.ExitStack())

    def pop(self):
        self.barrier()
        self.stack.pop().close()

    def barrier(self):
        deps = []
        for e in self.eng:
            if self.cnt[e] > 0:
                deps.append((self.esem[e], self.cnt[e]))
        for q in self.ring:
            for slot in self.ring[q]:
                if slot[1] > 0:
                    deps.append((slot[0], slot[1]))
        for e in self.eng:
            self._wait(e, deps)

    def _newsem(self, name):
        s = self.es.enter_context(self.nc.semaphore(name))
        self.sems.append(s)
        return len(self.sems) - 1

    def sb(self, name, shape, dt):
        return self.stack[-1].enter_context(self.nc.sbuf_tensor(name, list(shape), dt))

    def ps(self, name, shape, dt=F32):
        return self.es.enter_context(self.nc.psum_tensor(name, list(shape), dt))

    def dram(self, name, shape, dt, kind):
        return self.nc.dram_tensor(name, list(shape), dt, kind=kind).ap()

    def _wait(self, e, deps):
        h = self.eng[e]
        best = {}
        for (s, v) in deps:
            if best.get(s, 0) < v:
                best[s] = v
        for s, v in best.items():
            if s == self.esem[e] and e in ("pe", "sp"):
                continue
            if self.seen[e].get(s, 0) < v:
                h.wait_ge(self.sems[s], v)
                self.seen[e][s] = v

    def _deps(self, reads, writes):
        deps = []
        for b in reads:
            if b.w is not None:
                deps.append(b.w)
        for b in writes:
            if b.w is not None:
                deps.append(b.w)
            deps.extend(b.r.items())
        return deps

    def _mark(self, t, reads, writes):
        for b in reads:
            if b.r.get(t[0], 0) < t[1]:
                b.r[t[0]] = t[1]
        for b in writes:
            b.w = t
            b.r = {}

    def op(self, e, fn, reads=(), writes=()):
        self._wait(e, self._deps(reads, writes))
        ins = fn(self.eng[e])
        self.cnt[e] += 1
        ins.then_inc(self.sems[self.esem[e]], 1)
        t = (self.esem[e], self.cnt[e])
        self._mark(t, reads, writes)
        return t

    def dma(self, q, out, in_, reads=(), writes=(), is_output=False, **kw):
        slot = self.ring[q][self.rpos[q]]
        self.rpos[q] = (self.rpos[q] + 1) % len(self.ring[q])
        deps = self._deps(reads, writes)
        if slot[1] > 0:
            deps.append((slot[0], slot[1]))
        self._wait(q, deps)
        ins = self.eng[q].dma_start(out=out, in_=in_, **kw)
        slot[1] += 16
        ins.then_inc(self.sems[slot[0]], 16)
        t = (slot[0], slot[1])
        self._mark(t, reads, writes)
        if is_output:
            self.out_tickets.append(t)
        return t

    def finish(self):
        deps = list(self.out_tickets)
        for e in self.eng:
            if self.cnt[e] > 0:
                deps.append((self.esem[e], self.cnt[e]))
        for q in self.ring:
            for slot in self.ring[q]:
                if slot[1] > 0:
                    deps.append((slot[0], slot[1]))
        self._wait("sp", deps)
        self.es.close()
        return self.nc


def groups_of(total, g):
    out = []
    s = 0
    while s < total:
        n = min(g, total - s)
        out.append((s, n))
        s += n
    return out


def build_mixer(mode, T, stop_after=None, debug=False):
    k = KB()
    NBLK = T // 128
    TT = T + 128
    NPB = 3 * NBLK
    G = min(512, T)
    full = mode == "B"
    og = groups_of(T, G)
    own_groups = [(128 + s, n) for (s, n) in og]
    tgroups = [(0, 128)] + own_groups

    d_xT = k.dram("xT", [128, 8, TT], F32, "ExternalInput")
    d_cv = k.dram("cvec", [128, 8], F32, "ExternalInput")
    NMOD = 48 if full else 16
    d_wada = k.dram("wada", [NMOD // 4, 128, 8, 512], F32, "ExternalInput")
    d_bada = k.dram("bada", [128, 48], F32, "ExternalInput")
    d_nmix = k.dram("nmix", [128, 8], F32, "ExternalInput")
    d_win = k.dram("win", [7, 128, 8, 512], F32, "ExternalInput")
    d_convw = k.dram("convw", [128, 4, 4], F32, "ExternalInput")
    d_lrus = k.dram("lrus", [128, 4, 4], F32, "ExternalInput")
    d_wlru = k.dram("wlru", [128, 2, 512], F32, "ExternalInput")
    d_qkg = k.dram("qkg", [128, 4], F32, "ExternalInput")
    d_flag = k.dram("flag", [128, 1], F32, "ExternalInput")
    d_id = k.dram("ident", [128, 128], F32, "ExternalInput")
    d_bd = k.dram("bdones", [128, 128], F32, "ExternalInput")
    if full:
        d_nffn = k.dram("nffn", [128, 8], F32, "ExternalInput")
        d_kprev = k.dram("kprev", [4, 128, NPB * 128], BF16, "ExternalInput")
        d_vprev = k.dram("vprev", [4, 128, NPB, 130], BF16, "ExternalInput")
        d_carry = k.dram("carry", [128, 4, 3, 2], F32, "ExternalInput")
        d_dbias = k.dram("dbias", [4, 128, 2, 2, 128], F32, "ExternalInput")
        d_b31 = k.dram("b31", [128, 4], F32, "ExternalInput")
        d_sbias = k.dram("sbias", [128, 8, 2, 128], F32, "ExternalInput")
        d_sink = k.dram("sinks", [128, 8], F32, "ExternalInput")
        d_dlam = k.dram("dlam", [128, 4, 64], F32, "ExternalInput")
        d_subln = k.dram("subln", [128, 128], F32, "ExternalInput")
        d_wgl = k.dram("wgl", [8, 128, 8, 384], F32, "ExternalInput")
        d_wbr = k.dram("wbr", [8, 128, 12, 128], F32, "ExternalInput")
        d_wout = k.dram("wout", [2, 128, 8, 512], F32, "ExternalInput")
        d_wr = k.dram("wr", [128, 8, 32], F32, "ExternalInput")
        d_br = k.dram("br", [128, 32], F32, "ExternalInput")
        d_lamc = k.dram("lamc", [128, 2], F32, "ExternalInput")
        if debug:
            o_dbg = k.dram("o_dbg", [128, 12, T], BF16, "ExternalOutput")
        o_xT = k.dram("o_xT", [128, 8, T], F32, "ExternalOutput")
        o_hT = k.dram("o_hT", [128, 8, T], BF16, "ExternalOutput")
        o_rw = k.dram("o_rw", [T, 32], F32, "ExternalOutput")
        o_gf = k.dram("o_gf", [128, 8], F32, "ExternalOutput")
    else:
        o_k = k.dram("o_k", [4, 128, T], BF16, "ExternalOutput")
        o_v = k.dram("o_v", [4, 128, NBLK, 130], BF16, "ExternalOutput")
        o_c = k.dram("o_c", [128, 4, 2], F32, "ExternalOutput")

    hT = k.sb("hT_s", [128, 8, TT], BF16); b_hT = Buf()
    cst = k.sb("cst", [128, 64], F32); b_cst = Buf()
    ident = k.sb("ident_s", [128, 128], F32); b_id = Buf()
    identb = k.sb("identb", [128, 128], BF16)
    bdones = k.sb("bdones_s", [128, 128], BF16); b_bd = Buf()
    ones = k.sb("ones_s", [128, 128], BF16); b_ones = Buf()
    mod = k.sb("mod", [128, 48], F32); b_mod = Buf()
    sm = k.sb("small", [128, 16], F32); b_sm = Buf()
    wst = k.sb("wst", [128, 8, 512], F32); b_wst = Buf()
    wbf = [k.sb("wbf%d" % i, [128, 8, 512], BF16) for i in range(3)]; b_wbf = [Buf(), Buf(), Buf()]
    sq = k.sb("sqs", [128, 8, G], BF16); b_sq = Buf()
    rstd = k.sb("rstd", [128, G], F32); b_rstd = Buf()
    tmpf = [k.sb("tmpf%d" % i, [128, G], F32) for i in range(2)]; b_tmpf = [Buf(), Buf()]
    tmpb = [k.sb("tmpb%d" % i, [128, G], BF16) for i in range(2)]; b_tmpb = [Buf(), Buf()]
    lsm = k.sb("lsm", [128, 16], F32); b_lsm = Buf()
    if full:
        obr = k.sb("obr", [128, 12, T], BF16); b_obr = Buf()
    pbank = [k.ps("pb%d" % i, [128, 512]) for i in range(8)]
    b_pb = [Buf() for _ in range(8)]

    k.dma("sp", ident[:], d_id, writes=[b_id])
    k.dma("sp", tmpf[0][:, 0:128], d_bd, writes=[b_tmpf[0]])
    k.op("dve", lambda e: e.tensor_copy(out=bdones[:], in_=tmpf[0][:, 0:128]), reads=[b_tmpf[0]], writes=[b_bd])
    k.op("dve", lambda e: e.memset(ones[:], 1.0), writes=[b_ones])
    k.op("dve", lambda e: e.tensor_copy(out=identb[:], in_=ident[:]), reads=[b_id], writes=[b_id])
    k.dma("sp", cst[:, 0:8], d_cv, writes=[b_cst])
    k.dma("sp", cst[:, 8:16], d_nmix, writes=[b_cst])
    k.dma("sp", cst[:, 24:40], d_convw.rearrange("p a b -> p (a b)"), writes=[b_cst])
    k.dma("sp", cst[:, 40:56], d_lrus.rearrange("p a b -> p (a b)"), writes=[b_cst])
    k.dma("sp", cst[:, 56:60], d_qkg, writes=[b_cst])
    k.dma("sp", cst[:, 60:61], d_flag, writes=[b_cst])
    k.dma("sp", mod[:], d_bada, writes=[b_mod])
    if full:
        k.dma("sp", cst[:, 16:24], d_nffn, writes=[b_cst])
    k.op("dve", lambda e: e.memset(cst[:, 61:62], EPS), writes=[b_cst])
    k.op("dve", lambda e: e.memset(cst[:, 62:63], 1.0), writes=[b_cst])
    convw = lambda c, tap: cst[:, 24 + c * 4 + tap: 25 + c * 4 + tap]
    lrus = lambda c, i: cst[:, 40 + c * 4 + i: 41 + c * 4 + i]
    epsc = cst[:, 61:62]
    onec = cst[:, 62:63]
    flag = cst[:, 60:61]

    k.op("act", lambda e: e.activation(out=cst[:, 0:8], in_=cst[:, 0:8], func=AF.Silu), reads=[b_cst], writes=[b_cst])
    for blk in range(NMOD // 4):
        k.dma("sp", wst[:], d_wada[blk], writes=[b_wst])
        pb = pbank[blk % 2]
        for fi in range(4):
            for kc in range(8):
                k.op("pe", lambda e, fi=fi, kc=kc: e.matmul(pb[:, fi:fi + 1], lhsT=wst[:, kc, fi * 128:(fi + 1) * 128],
                                                           rhs=cst[:, kc:kc + 1], start=(kc == 0), stop=(kc == 7)),
                     reads=[b_wst, b_cst], writes=[b_pb[blk % 2]])
        f0 = blk * 4
        k.op("dve", lambda e: e.tensor_tensor(out=mod[:, f0:f0 + 4], in0=pb[:, 0:4], in1=mod[:, f0:f0 + 4], op=ALU.add),
             reads=[b_pb[blk % 2], b_mod], writes=[b_mod])
    k.op("dve", lambda e: e.scalar_tensor_tensor(out=sm[:, 0:8], in0=mod[:, 8:16], scalar=1.0, in1=cst[:, 8:16],
                                                 op0=ALU.add, op1=ALU.mult), reads=[b_mod, b_cst], writes=[b_sm])
    if full:
        k.op("dve", lambda e: e.scalar_tensor_tensor(out=sm[:, 8:16], in0=mod[:, 32:40], scalar=1.0, in1=cst[:, 16:24],
                                                     op0=ALU.add, op1=ALU.mult), reads=[b_mod, b_cst], writes=[b_sm])

    def norm_mod(src, b_src, n, Acol, Bcol, dst_fn, b_dst, inplace_f32=False):
        for c in range(8):
            k.op("act", lambda e, c=c: e.activation(out=sq[:, c, 0:n], in_=src[:, c, 0:n], func=AF.Square),
                 reads=[b_src], writes=[b_sq])
        pb = pbank[2]
        for c in range(8):
            k.op("pe", lambda e, c=c: e.matmul(pb[:, 0:n], lhsT=ones[:], rhs=sq[:, c, 0:n], start=(c == 0), stop=(c == 7)),
                 reads=[b_sq, b_ones], writes=[b_pb[2]])
        k.op("act", lambda e: e.activation(out=rstd[:, 0:n], in_=pb[:, 0:n], func=AF.Sqrt, scale=1.0 / D, bias=epsc),
             reads=[b_pb[2], b_cst], writes=[b_rstd])
        k.op("dve", lambda e: e.reciprocal(out=rstd[:, 0:n], in_=rstd[:, 0:n]), reads=[b_rstd], writes=[b_rstd])
        for c in range(8):
            ti = c % 2
            k.op("dve", lambda e, c=c, ti=ti: e.scalar_tensor_tensor(out=tmpf[ti][:, 0:n], in0=src[:, c, 0:n],
                                                                    scalar=sm[:, Acol + c:Acol + c + 1], in1=rstd[:, 0:n],
                                                                    op0=ALU.mult, op1=ALU.mult),
                 reads=[b_src, b_sm, b_rstd], writes=[b_tmpf[ti]])
            if inplace_f32:
                k.op("act", lambda e, c=c, ti=ti: e.activation(out=src[:, c, 0:n], in_=tmpf[ti][:, 0:n], func=AF.Identity,
                                                               bias=mod[:, Bcol + c:Bcol + c + 1]),
                     reads=[b_tmpf[ti], b_mod], writes=[b_src])
                k.op("dve", lambda e, c=c: e.tensor_copy(out=dst_fn(c), in_=src[:, c, 0:n]), reads=[b_src], writes=[b_dst])
            else:
                k.op("act", lambda e, c=c, ti=ti: e.activation(out=dst_fn(c), in_=tmpf[ti][:, 0:n], func=AF.Identity,
                                                               bias=mod[:, Bcol + c:Bcol + c + 1]),
                     reads=[b_tmpf[ti], b_mod], writes=[b_dst])

    k.push()
    xg = k.sb("xg", [128, 8, 512], F32); b_xg = Buf()
    for (s, n) in tgroups:
        k.dma("sp", xg[:, :, 0:n], d_xT[:, :, s:s + n], writes=[b_xg])
        norm_mod(xg, b_xg, n, 0, 0, lambda c, s=s, n=n: hT[:, c, s:s + n], b_hT)
    k.pop()

    def load_w(slot, src, rows=8, cols=512):
        k.dma("sp", wst[:, 0:rows, 0:cols], src, writes=[b_wst])
        k.op("pool", lambda e: e.tensor_copy(out=wbf[slot][:, 0:rows, 0:cols], in_=wst[:, 0:rows, 0:cols]),
             reads=[b_wst], writes=[b_wbf[slot]])
        return wbf[slot], b_wbf[slot]

    pstate = {"i": 0}

    def next_pb():
        i = 4 + (pstate["i"] % 4)
        pstate["i"] += 1
        return pbank[i], b_pb[i]

    def linear_fm(w, b_w, chunks, src, b_src, KC, groups, evac):
        for (s, n) in groups:
            for nci in chunks:
                pb, bpb = next_pb()
                for kc in range(KC):
                    k.op("pe", lambda e, kc=kc, nci=nci: e.matmul(pb[:, 0:n], lhsT=w[:, kc, nci * 128:(nci + 1) * 128],
                                                                 rhs=src[:, kc, s:s + n], start=(kc == 0), stop=(kc == KC - 1)),
                         reads=[b_w, b_src], writes=[bpb])
                evac(nci, s, n, pb, bpb)

    def qknorm(dst_fn, b_dst, gcol):
        def ev(nci, s, n, pb, bpb):
            ti = nci % 2
            k.op("act", lambda e: e.activation(out=tmpb[ti][:, 0:n], in_=pb[:, 0:n], func=AF.Square),
                 reads=[bpb], writes=[b_tmpb[ti]])
            k.op("act", lambda e: e.activation(out=tmpf[ti][:, 0:n], in_=pb[:, 0:n], func=AF.Identity),
                 reads=[bpb], writes=[b_tmpf[ti]])
            p2 = pbank[3]
            k.op("pe", lambda e: e.matmul(p2[:, 0:n], lhsT=bdones[:], rhs=tmpb[ti][:, 0:n], start=True, stop=True),
                 reads=[b_bd, b_tmpb[ti]], writes=[b_pb[3]])
            k.op("act", lambda e: e.activation(out=rstd[:, 0:n], in_=p2[:, 0:n], func=AF.Sqrt, scale=1.0 / 64, bias=epsc),
                 reads=[b_pb[3], b_cst], writes=[b_rstd])
            k.op("dve", lambda e: e.reciprocal(out=rstd[:, 0:n], in_=rstd[:, 0:n]), reads=[b_rstd], writes=[b_rstd])
            k.op("dve", lambda e: e.scalar_tensor_tensor(out=dst_fn(nci, s, n), in0=tmpf[ti][:, 0:n],
                                                         scalar=cst[:, 56 + gcol:57 + gcol], in1=rstd[:, 0:n],
                                                         op0=ALU.mult, op1=ALU.mult),
                 reads=[b_tmpf[ti], b_cst, b_rstd], writes=[b_dst])
        return ev

    k.push()
    xr = k.sb("xr", [128, TT], F32); b_xr = Buf()
    xc = k.sb("xc", [128, T], F32); b_xc = Buf()
    xcb = k.sb("xcb", [128, 1, T], BF16); b_xcb = Buf()
    la = k.sb("la", [128, T], F32); b_la = Buf()
    bb = k.sb("bbuf", [128, T], F32); b_bb = Buf()
    hh = k.sb("hh", [128, T], F32); b_hh = Buf()
    wl = k.sb("wl", [128, 2, 512], BF16); b_wl = Buf()
    cout = k.sb("cout", [128, 4, 2], F32); b_cout = Buf()
    carry = k.sb("carry_s", [128, 4, 3, 2], F32); b_carry = Buf()
    w0, bw0 = load_w(0, d_win[0])
    w1, bw1 = load_w(1, d_win[1])
    k.dma("sp", wst[:, 0:2, :], d_wlru, writes=[b_wst])
    k.op("pool", lambda e: e.tensor_copy(out=wl[:], in_=wst[:, 0:2, :]), reads=[b_wst], writes=[b_wl])
    k.op("act", lambda e: e.activation(out=lsm[:, 0:4], in_=cst[:, 40:56].rearrange("p (c i) -> p c i", i=4)[:, :, 3],
                                       func=AF.Exp, scale=-1.0), reads=[b_cst], writes=[b_lsm])
    k.op("act", lambda e: e.activation(out=lsm[:, 0:4], in_=lsm[:, 0:4], func=AF.Ln, bias=onec), reads=[b_lsm, b_cst],
         writes=[b_lsm])
    k.op("dve", lambda e: e.tensor_scalar(out=lsm[:, 0:4], in0=lsm[:, 0:4], scalar1=-8.0, scalar2=None, op0=ALU.mult),
         reads=[b_lsm], writes=[b_lsm])
    k.op("dve", lambda e: e.memset(lsm[:, 8:12], 0.0), writes=[b_lsm])
    if full:
        k.dma("sp", carry[:], d_carry, writes=[b_carry])
        for kk in range(3):
            k.op("dve", lambda e, kk=kk: e.tensor_tensor(out=lsm[:, 8:12], in0=lsm[:, 8:12], in1=carry[:, :, kk, 0], op=ALU.mult),
                 reads=[b_lsm, b_carry], writes=[b_lsm])
            k.op("dve", lambda e, kk=kk: e.tensor_tensor(out=lsm[:, 8:12], in0=lsm[:, 8:12], in1=carry[:, :, kk, 1], op=ALU.add),
                 reads=[b_lsm, b_carry], writes=[b_lsm])
    C0 = 2.0 * math.sqrt(2.0 / math.pi)
    for c in range(4):
        def ev_xr(nci, s, n, pb, bpb):
            k.op("act", lambda e: e.activation(out=xr[:, s:s + n], in_=pb[:, 0:n], func=AF.Identity), reads=[bpb], writes=[b_xr])
        linear_fm(w0, bw0, [c], hT, b_hT, 8, tgroups, ev_xr)
        k.op("dve", lambda e: e.tensor_scalar(out=xr[:, 0:128], in0=xr[:, 0:128], scalar1=flag, scalar2=None, op0=ALU.mult),
             reads=[b_xr, b_cst], writes=[b_xr])
        k.op("dve", lambda e, c=c: e.tensor_scalar(out=xc[:], in0=xr[:, 125:125 + T], scalar1=convw(c, 0), scalar2=lrus(c, 0),
                                                   op0=ALU.mult, op1=ALU.add), reads=[b_xr, b_cst], writes=[b_xc])
        for tap in range(1, 4):
            k.op("dve", lambda e, c=c, tap=tap: e.scalar_tensor_tensor(out=xc[:], in0=xr[:, 125 + tap:125 + tap + T],
                                                                      scalar=convw(c, tap), in1=xc[:], op0=ALU.mult,
                                                                      op1=ALU.add), reads=[b_xr, b_cst, b_xc], writes=[b_xc])
        k.op("pool", lambda e: e.tensor_copy(out=xcb[:, 0, :], in_=xc[:]), reads=[b_xc], writes=[b_xcb])

        def ev_ga(nci, s, n, pb, bpb, c=c):
            k.op("act", lambda e: e.activation(out=la[:, s:s + n], in_=pb[:, 0:n], func=AF.Sigmoid, bias=lrus(c, 1)),
                 reads=[bpb, b_cst], writes=[b_la])

        def ev_gx(nci, s, n, pb, bpb, c=c):
            k.op("act", lambda e: e.activation(out=bb[:, s:s + n], in_=pb[:, 0:n], func=AF.Sigmoid, bias=lrus(c, 2)),
                 reads=[bpb, b_cst], writes=[b_bb])
        linear_fm(wl[:, 0:1, :], b_wl, [c], xcb, b_xcb, 1, og, ev_ga)
        linear_fm(wl[:, 1:2, :], b_wl, [c], xcb, b_xcb, 1, og, ev_gx)
        k.op("dve", lambda e, c=c: e.tensor_scalar(out=la[:], in0=la[:], scalar1=lsm[:, c:c + 1], scalar2=None, op0=ALU.mult),
             reads=[b_la, b_lsm], writes=[b_la])
        if not full:
            k.op("dve", lambda e, c=c: e.tensor_reduce(out=lsm[:, 12 + c:13 + c], in_=la[:], axis=AX.X, op=ALU.add),
                 reads=[b_la], writes=[b_lsm])
        k.op("act", lambda e: e.activation(out=hh[:], in_=la[:], func=AF.Exp, scale=2.0), reads=[b_la], writes=[b_hh])
        k.op("act", lambda e: e.activation(out=hh[:], in_=hh[:], func=AF.Sqrt, scale=-1.0, bias=onec), reads=[b_hh, b_cst],
             writes=[b_hh])
        k.op("act", lambda e: e.activation(out=la[:], in_=la[:], func=AF.Exp), reads=[b_la], writes=[b_la])
        k.op("dve", lambda e: e.tensor_tensor(out=bb[:], in0=bb[:], in1=xc[:], op=ALU.mult), reads=[b_bb, b_xc], writes=[b_bb])
        k.op("dve", lambda e: e.tensor_tensor(out=bb[:], in0=bb[:], in1=hh[:], op=ALU.mult), reads=[b_bb, b_hh], writes=[b_bb])
        k.op("dve", lambda e, c=c: e.tensor_tensor_scan(out=hh[:], data0=la[:], data1=bb[:], initial=lsm[:, 8 + c:9 + c],
                                                        op0=ALU.mult, op1=ALU.add),
             reads=[b_la, b_bb, b_lsm, b_hh], writes=[b_hh])
        if not full:
            k.op("dve", lambda e, c=c: e.tensor_copy(out=cout[:, c, 1:2], in_=hh[:, T - 1:T]), reads=[b_hh], writes=[b_cout])
        else:
            def ev_gr(nci, s, n, pb, bpb):
                ti = 0
                so = s - 128
                k.op("act", lambda e: e.activation(out=tmpf[ti][:, 0:n], in_=pb[:, 0:n], func=AF.Square), reads=[bpb],
                     writes=[b_tmpf[ti]])
                k.op("dve", lambda e: e.tensor_scalar(out=tmpf[ti][:, 0:n], in0=tmpf[ti][:, 0:n], scalar1=0.044715, scalar2=1.0,
                                                      op0=ALU.mult, op1=ALU.add), reads=[b_tmpf[ti]], writes=[b_tmpf[ti]])
                k.op("dve", lambda e: e.tensor_tensor(out=tmpf[ti][:, 0:n], in0=tmpf[ti][:, 0:n], in1=pb[:, 0:n], op=ALU.mult),
                     reads=[b_tmpf[ti], bpb], writes=[b_tmpf[ti]])
                k.op("act", lambda e: e.activation(out=tmpf[ti][:, 0:n], in_=tmpf[ti][:, 0:n], func=AF.Sigmoid, scale=C0),
                     reads=[b_tmpf[ti]], writes=[b_tmpf[ti]])
                k.op("dve", lambda e: e.tensor_tensor(out=tmpf[ti][:, 0:n], in0=tmpf[ti][:, 0:n], in1=pb[:, 0:n], op=ALU.mult),
                     reads=[b_tmpf[ti], bpb], writes=[b_tmpf[ti]])
                k.op("dve", lambda e: e.tensor_tensor(out=obr[:, nci, so:so + n], in0=tmpf[ti][:, 0:n], in1=hh[:, so:so + n],
                                                      op=ALU.mult), reads=[b_tmpf[ti], b_hh], writes=[b_obr])
            linear_fm(w1, bw1, [c], hT, b_hT, 8, own_groups, ev_gr)
    if not full:
        k.op("act", lambda e: e.activation(out=cout[:, :, 0], in_=lsm[:, 12:16], func=AF.Exp), reads=[b_lsm], writes=[b_cout])
        k.dma("sp", o_c, cout[:], reads=[b_cout], is_output=True)
    k.pop()

    if stop_after == "lru":
        return k.finish()
    k.push()
    kT = k.sb("kT", [128, T], BF16); b_kT = Buf()
    vA = k.sb("vA", [128, NBLK, 130], BF16); b_vA = Buf()
    wq, bwq = (load_w(0, d_win[2]) if full else (None, None))
    wk, bwk = load_w(1, d_win[3])
    wv, bwv = load_w(2, d_win[4])
    if full:
        qT = k.sb("qT", [128, T], BF16); b_qT = Buf()
        kprev = k.sb("kprev_s", [128, NPB * 128], BF16); b_kprev = Buf()
        vprev = k.sb("vprev_s", [128, NPB, 130], BF16); b_vprev = Buf()
        dbias = k.sb("dbias_s", [128, 2, 2, 128], F32); b_db = Buf()
        pT = [k.sb("pT%d" % i, [128, 2, 128], BF16) for i in range(2)]; b_pT = [Buf(), Buf()]
        sF = [k.sb("sF%d" % i, [128, 2, 128], F32) for i in range(2)]; b_sF = [Buf(), Buf()]
        acs = k.sb("acs", [128, 2, 130], F32); b_acs = Buf()
        otokb = k.sb("otokb", [128, 128], BF16); b_otokb = Buf()
        att = k.sb("att", [128, 32], F32); b_att = Buf()
        subln = k.sb("subln_s", [128, 128], F32); b_subln = Buf()
        dlam = k.sb("dlam_s", [128, 4, 64], F32); b_dlam = Buf()
        k.dma("sp", subln[:], d_subln, writes=[b_subln])
        k.dma("sp", dlam[:], d_dlam, writes=[b_dlam])
        k.dma("sp", att[:, 0:4], d_b31, writes=[b_att])
        k.dma("sp", att[:, 24:26], d_lamc, writes=[b_att])
        k.op("dve", lambda e: e.tensor_tensor(out=dlam[:, 0, :], in0=dlam[:, 0, :], in1=dlam[:, 1, :], op=ALU.mult),
             reads=[b_dlam], writes=[b_dlam])
        k.op("dve", lambda e: e.tensor_tensor(out=dlam[:, 2, :], in0=dlam[:, 2, :], in1=dlam[:, 3, :], op=ALU.mult),
             reads=[b_dlam], writes=[b_dlam])
        k.op("dve", lambda e: e.tensor_reduce(out=att[:, 14:15], in_=dlam[:, 0, :], axis=AX.X, op=ALU.add),
             reads=[b_dlam], writes=[b_att])
        k.op("dve", lambda e: e.tensor_reduce(out=att[:, 15:16], in_=dlam[:, 2, :], axis=AX.X, op=ALU.add),
             reads=[b_dlam], writes=[b_att])
        k.op("act", lambda e: e.activation(out=att[:, 14:16], in_=att[:, 14:16], func=AF.Exp), reads=[b_att], writes=[b_att])
        k.op("dve", lambda e: e.scalar_tensor_tensor(out=att[:, 13:14], in0=att[:, 15:16], scalar=att[:, 24:25], in1=att[:, 14:15],
                                                     op0=ALU.add, op1=ALU.subtract), reads=[b_att], writes=[b_att])
        k.op("dve", lambda e: e.tensor_scalar(out=subln[:], in0=subln[:], scalar1=att[:, 25:26], scalar2=None, op0=ALU.mult),
             reads=[b_subln, b_att], writes=[b_subln])
    for h in range(4):
        linear_fm(wk, bwk, [h], hT, b_hT, 8, own_groups, qknorm(lambda nci, s, n: kT[:, s - 128:s - 128 + n], b_kT, 1))
        k.op("dve", lambda e: e.memset(vA[:, :, 128:130], 1.0), writes=[b_vA])
        for blk in range(NBLK):
            pb, bpb = next_pb()
            for kc in range(8):
                k.op("pe", lambda e, kc=kc, blk=blk: e.matmul(pb[:, 0:128], lhsT=hT[:, kc, 128 + blk * 128:256 + blk * 128],
                                                             rhs=wv[:, kc, h * 128:(h + 1) * 128], start=(kc == 0), stop=(kc == 7)),
                     reads=[b_hT, bwv], writes=[bpb])
            k.op("act", lambda e, blk=blk: e.activation(out=vA[:, blk, 0:128], in_=pb[:, 0:128], func=AF.Identity),
                 reads=[bpb], writes=[b_vA])
        if not full:
            k.dma("sp", o_k[h], kT[:], reads=[b_kT], is_output=True)
            k.dma("sp", o_v[h], vA[:], reads=[b_vA], is_output=True)
            continue
        linear_fm(wq, bwq, [h], hT, b_hT, 8, own_groups, qknorm(lambda nci, s, n: qT[:, s - 128:s - 128 + n], b_qT, 0))
        k.dma("sp", kprev[:], d_kprev[h], writes=[b_kprev])
        k.dma("sp", vprev[:], d_vprev[h], writes=[b_vprev])
        k.dma("sp", dbias[:], d_dbias[h], writes=[b_db])
        for i in range(NBLK):
            kbl = [("p", j) for j in range(NPB)] + [("o", j) for j in range(i + 1)]
            acc = [pbank[0], pbank[1]]
            L = len(kbl)

            def d_info(idx):
                kind, j = kbl[idx]
                si = idx % 2
                if kind == "p":
                    return (si, kprev[:, j * 128:(j + 1) * 128], vprev[:, j, 0:129], b_kprev, b_vprev,
                            (1 if (i == 0 and j == NPB - 1) else None))
                return (si, kT[:, j * 128:(j + 1) * 128], vA[:, j, 0:129], b_kT, b_vA,
                        (0 if j == i else (1 if j == i - 1 else None)))

            def d_qk(idx):
                si, ksrc, vsrc, kb_, vb_, near = d_info(idx)
                pSm = [pbank[2 + si], pbank[4 + si]]
                bSm = [b_pb[2 + si], b_pb[4 + si]]
                for m in range(2):
                    k.op("pe", lambda e, m=m: e.matmul(pSm[m][:, 0:128], lhsT=ksrc[m * 64:(m + 1) * 64, :],
                                                       rhs=qT[m * 64:(m + 1) * 64, i * 128:(i + 1) * 128],
                                                       start=True, stop=True),
                         reads=[kb_, b_qT], writes=[bSm[m]])

            def d_exp(idx):
                si, ksrc, vsrc, kb_, vb_, near = d_info(idx)
                pSm = [pbank[2 + si], pbank[4 + si]]
                bSm = [b_pb[2 + si], b_pb[4 + si]]
                for m in range(2):
                    if near is None:
                        k.op("act", lambda e, m=m: e.activation(out=pT[si][:, m, :], in_=pSm[m][:, 0:128], func=AF.Exp,
                                                                scale=0.125, bias=att[:, h:h + 1]),
                             reads=[bSm[m], b_att], writes=[b_pT[si]])
                    else:
                        k.op("dve", lambda e, m=m: e.scalar_tensor_tensor(
                            out=sF[si][:, m, :], in0=pSm[m][:, 0:128], scalar=0.125, in1=dbias[:, near, m, :],
                            op0=ALU.mult, op1=ALU.add), reads=[bSm[m], b_db], writes=[b_sF[si]])
                        k.op("act", lambda e, m=m: e.activation(out=pT[si][:, m, :], in_=sF[si][:, m, :], func=AF.Exp),
                             reads=[b_sF[si]], writes=[b_pT[si]])

            def d_pv(idx):
                si, ksrc, vsrc, kb_, vb_, near = d_info(idx)
                for m in range(2):
                    k.op("pe", lambda e, m=m: e.matmul(acc[m][:, 0:129], lhsT=pT[si][:, m, :], rhs=vsrc,
                                                       start=(idx == 0), stop=(idx == L - 1)),
                         reads=[b_pT[si], vb_], writes=[b_pb[m]])

            d_qk(0)
            for idx in range(L):
                if idx + 1 < L:
                    d_qk(idx + 1)
                d_exp(idx)
                d_pv(idx)
            for m in range(2):
                k.op("act", lambda e, m=m: e.activation(out=acs[:, m, 0:129], in_=acc[m][:, 0:129], func=AF.Identity),
                     reads=[b_pb[m]], writes=[b_acs])
            k.op("dve", lambda e: e.reciprocal(out=att[:, 16:18], in_=acs[:, :, 128]), reads=[b_acs], writes=[b_att])
            k.op("dve", lambda e: e.tensor_tensor(out=att[:, 17:18], in0=att[:, 17:18], in1=att[:, 13:14], op=ALU.mult),
                 reads=[b_att], writes=[b_att])
            k.op("dve", lambda e: e.tensor_scalar(out=acs[:, 0, 0:128], in0=acs[:, 0, 0:128], scalar1=att[:, 16:17], scalar2=None,
                                                  op0=ALU.mult), reads=[b_acs, b_att], writes=[b_acs])
            k.op("dve", lambda e: e.scalar_tensor_tensor(out=acs[:, 0, 0:128], in0=acs[:, 1, 0:128], scalar=att[:, 17:18],
                                                         in1=acs[:, 0, 0:128], op0=ALU.mult, op1=ALU.add),
                 reads=[b_acs, b_att], writes=[b_acs])
            k.op("dve", lambda e: e.tensor_tensor(out=acs[:, 1, 0:128], in0=acs[:, 0, 0:128], in1=acs[:, 0, 0:128], op=ALU.mult),
                 reads=[b_acs], writes=[b_acs])
            k.op("dve", lambda e: e.tensor_reduce(out=att[:, 18:19], in_=acs[:, 1, 0:128], axis=AX.X, op=ALU.add),
                 reads=[b_acs], writes=[b_att])
            k.op("act", lambda e: e.activation(out=att[:, 18:19], in_=att[:, 18:19], func=AF.Sqrt, scale=1.0 / 128, bias=epsc),
                 reads=[b_att, b_cst], writes=[b_att])
            k.op("dve", lambda e: e.reciprocal(out=att[:, 18:19], in_=att[:, 18:19]), reads=[b_att], writes=[b_att])
            k.op("dve", lambda e: e.scalar_tensor_tensor(out=otokb[:], in0=acs[:, 0, 0:128], scalar=att[:, 18:19], in1=subln[:],
                                                         op0=ALU.mult, op1=ALU.mult),
                 reads=[b_acs, b_att, b_subln], writes=[b_otokb])
            pt, bpt = next_pb()
            k.op("pe", lambda e: e.matmul(pt[:, 0:128], lhsT=otokb[:], rhs=identb[:], start=True, stop=True),
                 reads=[b_otokb, b_id], writes=[bpt])
            k.op("act", lambda e, i=i: e.activation(out=obr[:, 4 + h, i * 128:(i + 1) * 128], in_=pt[:, 0:128], func=AF.Identity),
                 reads=[bpt], writes=[b_obr])
    k.pop()
    if not full or stop_after == "diff":
        return k.finish()

    k.push()
    sqT = k.sb("sqT", [128, 4, T], BF16); b_sqT = Buf()
    skT = k.sb("skT", [128, TT], BF16); b_skT = Buf()
    svA = k.sb("svA", [128, NBLK + 1, 2, 66], BF16); b_svA = Buf()
    sbias = k.sb("sbias_s", [128, 8, 2, 128], F32); b_sb = Buf()
    pT = [k.sb("spT%d" % i, [128, 2, 128], BF16) for i in range(2)]; b_pT = [Buf(), Buf()]
    sF = [k.sb("ssF%d" % i, [128, 2, 128], F32) for i in range(2)]; b_sF = [Buf(), Buf()]
    otokb = k.sb("sotokb", [128, 512], BF16); b_otokb = Buf()
    att = k.sb("satt", [128, 32], F32); b_att = Buf()
    k.dma("sp", sbias[:], d_sbias, writes=[b_sb])
    k.dma("sp", att[:, 4:12], d_sink, writes=[b_att])
    k.op("act", lambda e: e.activation(out=att[:, 4:12], in_=att[:, 4:12], func=AF.Exp), reads=[b_att], writes=[b_att])
    w, bw = load_w(0, d_win[5])
    linear_fm(w, bw, [0, 1, 2, 3], hT, b_hT, 8, own_groups,
              qknorm(lambda nci, s, n: sqT[:, nci, s - 128:s - 128 + n], b_sqT, 2))
    w, bw = load_w(1, d_win[6])
    linear_fm(w, bw, [0], hT, b_hT, 8, tgroups, qknorm(lambda nci, s, n: skT[:, s:s + n], b_skT, 3))
    k.op("dve", lambda e: e.memset(svA[:, :, :, 64:66], 1.0), writes=[b_svA])
    for blk in range(NBLK + 1):
        pb, bpb = next_pb()
        for kc in range(8):
            k.op("pe", lambda e, kc=kc, blk=blk: e.matmul(pb[:, 0:128], lhsT=hT[:, kc, blk * 128:(blk + 1) * 128],
                                                         rhs=w[:, kc, 128:256], start=(kc == 0), stop=(kc == 7)),
                 reads=[b_hT, bw], writes=[bpb])
        k.op("act", lambda e, blk=blk: e.activation(out=svA[:, blk, :, 0:64], in_=pb[:, 0:128].rearrange("p (h d) -> p h d", h=2),
                                                    func=AF.Identity), reads=[bpb], writes=[b_svA])
    k.op("dve", lambda e: e.tensor_scalar(out=svA[:, 0, :, :], in0=svA[:, 0, :, :], scalar1=flag, scalar2=None, op0=ALU.mult),
         reads=[b_svA, b_cst], writes=[b_svA])
    steps = [(i, hd) for i in range(NBLK) for hd in range(8)]

    def s_qk(st):
        i, hd = steps[st]
        kv, c, si = hd // 4, hd % 4, st % 2
        pS, bS = pbank[2 + si], b_pb[2 + si]
        for bi in range(2):
            k.op("pe", lambda e, bi=bi: e.matmul(
                pS[:, bi * 128:(bi + 1) * 128], lhsT=skT[kv * 64:(kv + 1) * 64, (i + bi) * 128:(i + bi + 1) * 128],
                rhs=sqT[kv * 64:(kv + 1) * 64, c, i * 128:(i + 1) * 128], start=True, stop=True),
                reads=[b_skT, b_sqT], writes=[bS])

    def s_soft(st):
        i, hd = steps[st]
        si = st % 2
        pS, bS = pbank[2 + si], b_pb[2 + si]
        pv = pS[:, 0:256].rearrange("p (m q) -> p m q", m=2)
        k.op("dve", lambda e: e.scalar_tensor_tensor(out=sF[si][:], in0=pv, scalar=0.125, in1=sbias[:, hd], op0=ALU.mult,
                                                     op1=ALU.add), reads=[bS, b_sb], writes=[b_sF[si]])
        k.op("act", lambda e: e.activation(out=pT[si][:], in_=sF[si][:], func=AF.Exp), reads=[b_sF[si]], writes=[b_pT[si]])

    def s_pv(st):
        i, hd = steps[st]
        kv, si = hd // 4, st % 2
        pa, ba_ = pbank[si], b_pb[si]
        for bi in range(2):
            k.op("pe", lambda e, bi=bi: e.matmul(pa[:, 0:65], lhsT=pT[si][:, bi, :], rhs=svA[:, i + bi, kv, 0:65],
                                                 start=(bi == 0), stop=(bi == 1)), reads=[b_pT[si], b_svA], writes=[ba_])
        k.op("dve", lambda e: e.tensor_tensor(out=att[:, 20 + hd:21 + hd], in0=pa[:, 64:65], in1=att[:, 4 + hd:5 + hd],
                                              op=ALU.add), reads=[ba_, b_att], writes=[b_att])
        k.op("dve", lambda e: e.reciprocal(out=att[:, 20 + hd:21 + hd], in_=att[:, 20 + hd:21 + hd]), reads=[b_att],
             writes=[b_att])
        k.op("dve", lambda e: e.tensor_scalar(out=otokb[:, hd * 64:(hd + 1) * 64], in0=pa[:, 0:64],
                                              scalar1=att[:, 20 + hd:21 + hd], scalar2=None, op0=ALU.mult),
             reads=[ba_, b_att], writes=[b_otokb])
        if hd == 7:
            for c in range(4):
                pt, bpt = next_pb()
                k.op("pe", lambda e, c=c: e.matmul(pt[:, 0:128], lhsT=otokb[:, c * 128:(c + 1) * 128], rhs=identb[:],
                                                   start=True, stop=True), reads=[b_otokb, b_id], writes=[bpt])
                k.op("act", lambda e, c=c: e.activation(out=obr[:, 8 + c, i * 128:(i + 1) * 128], in_=pt[:, 0:128],
                                                        func=AF.Identity), reads=[bpt], writes=[b_obr])

    s_qk(0)
    for st in range(len(steps)):
        if st + 1 < len(steps):
            s_qk(st + 1)
        s_soft(st)
        s_pv(st)
    k.pop()

    if debug:
        k.dma("sp", o_dbg, obr[:], reads=[b_obr], is_output=True)
    if stop_after == "swa":
        return k.finish()
    k.push()
    mg = k.sb("mg", [128, 8, G], BF16); b_mg = Buf()
    xg = k.sb("xg2", [128, 8, G], F32); b_xg = Buf()
    wo = k.sb("wo", [128, 8, 1024], BF16); b_wo = Buf()
    wr = k.sb("wr_s", [128, 8, 32], F32); b_wr = Buf()
    brt = k.sb("br_s", [128, 32], F32); b_br = Buf()
    lg = k.sb("lg", [128, 32], F32); b_lg = Buf()
    t8 = k.sb("t8", [128, 8], F32); b_t8 = Buf()
    rwt = k.sb("rwt", [128, 32], F32); b_rwt = Buf()
    k.dma("sp", wr[:], d_wr, writes=[b_wr])
    k.dma("sp", brt[:], d_br, writes=[b_br])
    k.dma("sp", o_gf, mod[:, 40:48], reads=[b_mod], is_output=True)
    for half in range(2):
        k.dma("sp", wst[:], d_wout[half], writes=[b_wst])
        k.op("pool", lambda e, half=half: e.tensor_copy(out=wo[:, :, half * 512:(half + 1) * 512], in_=wst[:]),
             reads=[b_wst], writes=[b_wo])
    for (s, n) in og:
        k.dma("sp", xg[:, :, 0:n], d_xT[:, :, 128 + s:128 + s + n], writes=[b_xg])
        for f in range(8):
            k.dma("sp", wst[:, :, 0:384], d_wgl[f], writes=[b_wst])
            k.dma("sp", wst[:, :, 384:512], d_wbr[f][:, 0:8, :], writes=[b_wst])
            k.op("pool", lambda e: e.tensor_copy(out=wbf[0][:], in_=wst[:]), reads=[b_wst], writes=[b_wbf[0]])
            k.dma("sp", wst[:, 0:4, 0:128], d_wbr[f][:, 8:12, :], writes=[b_wst])
            k.op("pool", lambda e: e.tensor_copy(out=wbf[1][:, 0:4, 0:128], in_=wst[:, 0:4, 0:128]), reads=[b_wst],
                 writes=[b_wbf[1]])
            wg, bwg = wbf[0], b_wbf[0]
            w2_, bw2_ = wbf[1], b_wbf[1]
            for nb in range(3):
                pg, bpg = next_pb()
                for kc in range(8):
                    k.op("pe", lambda e, kc=kc, nb=nb, pg=pg: e.matmul(pg[:, 0:n], lhsT=wg[:, kc, nb * 128:(nb + 1) * 128],
                                                                      rhs=hT[:, kc, 128 + s:128 + s + n],
                                                                      start=(kc == 0), stop=(kc == 7)),
                         reads=[bwg, b_hT], writes=[bpg])
                ti = nb % 2
                k.op("act", lambda e, ti=ti, pg=pg: e.activation(out=tmpf[ti][:, 0:n], in_=pg[:, 0:n], func=AF.Sigmoid),
                     reads=[bpg], writes=[b_tmpf[ti]])
                pp, bpp = next_pb()
                for kc in range(4):
                    if nb < 2:
                        lw, lb = wg[:, nb * 4 + kc, 384:512], bwg
                    else:
                        lw, lb = w2_[:, kc, 0:128], bw2_
                    k.op("pe", lambda e, kc=kc, lw=lw, nb=nb, pp=pp: e.matmul(pp[:, 0:n], lhsT=lw, rhs=obr[:, nb * 4 + kc, s:s + n],
                                                                             start=(kc == 0), stop=(kc == 3)),
                         reads=[lb, b_obr], writes=[bpp])
                if nb == 0:
                    k.op("dve", lambda e, ti=ti, pp=pp: e.tensor_tensor(out=rstd[:, 0:n], in0=pp[:, 0:n], in1=tmpf[ti][:, 0:n],
                                                                       op=ALU.mult), reads=[bpp, b_tmpf[ti]], writes=[b_rstd])
                else:
                    k.op("dve", lambda e, ti=ti, pp=pp: e.tensor_tensor(out=tmpf[ti][:, 0:n], in0=pp[:, 0:n],
                                                                       in1=tmpf[ti][:, 0:n], op=ALU.mult),
                         reads=[bpp, b_tmpf[ti]], writes=[b_tmpf[ti]])
                    if nb == 1:
                        k.op("dve", lambda e, ti=ti: e.tensor_tensor(out=rstd[:, 0:n], in0=rstd[:, 0:n], in1=tmpf[ti][:, 0:n],
                                                                    op=ALU.add), reads=[b_rstd, b_tmpf[ti]], writes=[b_rstd])
                    else:
                        k.op("dve", lambda e, ti=ti, f=f: e.tensor_tensor(out=mg[:, f, 0:n], in0=rstd[:, 0:n],
                                                                         in1=tmpf[ti][:, 0:n], op=ALU.add),
                             reads=[b_rstd, b_tmpf[ti]], writes=[b_mg])

        def ev_out(nci, s_, n_, pb, bpb):
            k.op("dve", lambda e: e.scalar_tensor_tensor(out=xg[:, nci, 0:n_], in0=pb[:, 0:n_], scalar=mod[:, 16 + nci:17 + nci],
                                                         in1=xg[:, nci, 0:n_], op0=ALU.mult, op1=ALU.add),
                 reads=[bpb, b_mod, b_xg], writes=[b_xg])
        linear_fm(wo, b_wo, list(range(8)), mg, b_mg, 8, [(0, n)], ev_out)
        k.dma("sp", o_xT[:, :, s:s + n], xg[:, :, 0:n], reads=[b_xg], is_output=True)
        norm_mod(xg, b_xg, n, 8, 24, lambda c, s=s, n=n: hT[:, c, 128 + s:128 + s + n], b_hT, inplace_f32=True)
        for tb in range(n // 128):
            pl, bpl = next_pb()
            for kc in range(8):
                k.op("pe", lambda e, kc=kc, tb=tb, pl=pl: e.matmul(pl[:, 0:32], lhsT=xg[:, kc, tb * 128:(tb + 1) * 128],
                                                                  rhs=wr[:, kc, :], start=(kc == 0), stop=(kc == 7)),
                     reads=[b_xg, b_wr], writes=[bpl])
            k.op("dve", lambda e, pl=pl: e.tensor_tensor(out=lg[:], in0=pl[:, 0:32], in1=brt[:], op=ALU.add), reads=[bpl, b_br],
                 writes=[b_lg])
            k.op("dve", lambda e: e.max(out=t8[:], in_=lg[:]), reads=[b_lg], writes=[b_t8])
            k.op("dve", lambda e: e.tensor_scalar(out=rwt[:], in0=lg[:], scalar1=t8[:, 3:4], scalar2=None, op0=ALU.is_ge),
                 reads=[b_lg, b_t8], writes=[b_rwt])
            k.op("dve", lambda e: e.tensor_scalar(out=lg[:], in0=lg[:], scalar1=t8[:, 0:1], scalar2=None, op0=ALU.subtract),
                 reads=[b_lg, b_t8], writes=[b_lg])
            k.op("act", lambda e: e.activation(out=lg[:], in_=lg[:], func=AF.Exp), reads=[b_lg], writes=[b_lg])
            k.op("dve", lambda e: e.tensor_tensor(out=rwt[:], in0=rwt[:], in1=lg[:], op=ALU.mult), reads=[b_rwt, b_lg],
                 writes=[b_rwt])
            k.op("dve", lambda e: e.tensor_reduce(out=t8[:, 4:5], in_=rwt[:], axis=AX.X, op=ALU.add), reads=[b_rwt],
                 writes=[b_t8])
            k.op("dve", lambda e: e.reciprocal(out=t8[:, 4:5], in_=t8[:, 4:5]), reads=[b_t8], writes=[b_t8])
            k.op("dve", lambda e: e.tensor_scalar(out=rwt[:], in0=rwt[:], scalar1=t8[:, 4:5], scalar2=None, op0=ALU.mult),
                 reads=[b_rwt, b_t8], writes=[b_rwt])
            r0 = s + tb * 128
            k.dma("sp", o_rw[r0:r0 + 128, :], rwt[:], reads=[b_rwt], is_output=True)
    for (s, n) in groups_of(T, 512):
        k.dma("sp", o_hT[:, :, s:s + n], hT[:, :, 128 + s:128 + s + n], reads=[b_hT], is_output=True)
    k.pop()
    return k.finish()

def build_moe(NT):
    k = KB()
    G = 512
    NG = NT // G
    d_hT = k.dram("hT", [NG, 128, 8, G], BF16, "ExternalInput")
    d_rw = k.dram("rw", [4, NT], F32, "ExternalInput")
    d_w1 = k.dram("w1", [4, 4, 128, 8, 512], F32, "ExternalInput")
    d_w2 = k.dram("w2", [4, 2, 128, 8, 512], F32, "ExternalInput")
    d_b1 = k.dram("b1", [128, 4, 16], F32, "ExternalInput")
    d_b2 = k.dram("b2", [128, 4, 8], F32, "ExternalInput")
    d_sel = k.dram("sel", [4, 4, 128], F32, "ExternalInput")
    o_y = k.dram("o_y", [NG, 128, 8, G], BF16, "ExternalOutput")
    scr = k.dram("yscr", [NG, 128, 8, G], BF16, "Internal")

    ALPHA = 1.702
    C7 = ALPHA * 7.0 / (1.0 + math.exp(-ALPHA * 7.0))
    w1 = k.sb("w1s", [128, 8, 2048], BF16); b_w1 = Buf()
    w2 = k.sb("w2s", [128, 8, 1024], BF16); b_w2 = Buf()
    wst = k.sb("wst", [128, 8, 512], F32); b_wst = Buf()
    hg = [k.sb("hg%d" % i, [128, 8, G], BF16) for i in range(2)]; b_hg = [Buf(), Buf()]
    act = [k.sb("act%d" % i, [128, 8, G], BF16) for i in range(2)]; b_act = [Buf(), Buf()]
    ysb = k.sb("ysb", [128, 8, G], F32); b_ysb = Buf()
    ypv = k.sb("ypv", [128, 8, G], BF16); b_ypv = Buf()
    y16 = k.sb("y16", [128, 8, G], BF16); b_y16 = Buf()
    wbc = [k.sb("wbc%d" % i, [128, G], F32) for i in range(2)]; b_wbc = [Buf(), Buf()]
    rwg = [k.sb("rws%d" % i, [4, G], F32) for i in range(2)]; b_rwg = [Buf(), Buf()]
    sel = k.sb("sels", [4, 4, 128], F32); b_sel = Buf()
    b1 = k.sb("b1s", [128, 4, 16], F32); b_b1 = Buf()
    b2 = k.sb("b2s", [128, 4, 8], F32); b_b2 = Buf()
    s1 = [k.sb("s1_%d" % i, [128, G], BF16) for i in range(2)]; b_s1 = [Buf(), Buf()]
    s1c = [k.sb("s1c%d" % i, [128, G], BF16) for i in range(2)]; b_s1c = [Buf(), Buf()]
    tl = [k.sb("tl%d" % i, [128, G], BF16) for i in range(2)]; b_tl = [Buf(), Buf()]
    uu = [k.sb("uu%d" % i, [128, G], BF16) for i in range(2)]; b_uu = [Buf(), Buf()]
    pbank = [k.ps("pb%d" % i, [128, 512]) for i in range(8)]
    b_pb = [Buf() for _ in range(8)]
    b_y = [Buf() for _ in range(NG)]

    k.dma("sp", sel[:], d_sel, writes=[b_sel])
    k.dma("sp", b1[:], d_b1, writes=[b_b1])
    k.dma("sp", b2[:], d_b2, writes=[b_b2])
    k.op("dve", lambda e: e.tensor_scalar(out=b1[:, :, 0:8], in0=b1[:, :, 0:8], scalar1=ALPHA, scalar2=None, op0=ALU.mult),
         reads=[b_b1], writes=[b_b1])
    k.op("dve", lambda e: e.tensor_scalar(out=b1[:, :, 8:16], in0=b1[:, :, 8:16], scalar1=1.0, scalar2=None, op0=ALU.add),
         reads=[b_b1], writes=[b_b1])
    k.op("dve", lambda e: e.tensor_scalar(out=b2[:], in0=b2[:], scalar1=ALPHA, scalar2=None, op0=ALU.mult),
         reads=[b_b2], writes=[b_b2])
    pi = [0]

    def npb(lo, hi):
        i = lo + (pi[0] % (hi - lo))
        pi[0] += 1
        return pbank[i], b_pb[i]

    def stage1(ex, g):
        gi = g % 2
        s = g * G
        k.dma("sp", hg[gi][:], d_hT[g], writes=[b_hg[gi]])
        k.dma("sp", rwg[gi][:], d_rw[:, s:s + G], writes=[b_rwg[gi]])
        pw = pbank[7]
        k.op("pe", lambda e: e.matmul(pw[:, :], lhsT=sel[:, ex, :], rhs=rwg[gi][:], start=True, stop=True),
             reads=[b_sel, b_rwg[gi]], writes=[b_pb[7]])
        k.op("act", lambda e: e.activation(out=wbc[gi][:], in_=pw[:, :], func=AF.Identity, scale=1.0 / ALPHA),
             reads=[b_pb[7]], writes=[b_wbc[gi]])
        for ci in range(8):
            ti = ci % 2
            pg, bpg = npb(0, 3)
            for kc in range(8):
                k.op("pe", lambda e, kc=kc: e.matmul(pg[:, :], lhsT=w1[:, kc, ci * 128:(ci + 1) * 128], rhs=hg[gi][:, kc, :],
                                                     start=(kc == 0), stop=(kc == 7)), reads=[b_w1, b_hg[gi]], writes=[bpg])
            pl, bpl = npb(0, 3)
            for kc in range(8):
                k.op("pe", lambda e, kc=kc: e.matmul(pl[:, :], lhsT=w1[:, kc, 1024 + ci * 128:1024 + (ci + 1) * 128],
                                                     rhs=hg[gi][:, kc, :], start=(kc == 0), stop=(kc == 7)),
                     reads=[b_w1, b_hg[gi]], writes=[bpl])
            k.op("act", lambda e: e.activation(out=s1[ti][:], in_=pg[:, :], func=AF.Silu, scale=ALPHA, bias=b1[:, ex, ci:ci + 1]),
                 reads=[bpg, b_b1], writes=[b_s1[ti]])
            k.op("pool", lambda e: e.tensor_scalar(out=s1c[ti][:], in0=s1[ti][:], scalar1=C7, scalar2=-1.0e30, op0=ALU.min,
                                                   op1=ALU.max), reads=[b_s1[ti]], writes=[b_s1c[ti]])
            k.op("dve", lambda e: e.tensor_scalar(out=tl[ti][:], in0=pl[:, :], scalar1=b1[:, ex, 8 + ci:9 + ci], scalar2=8.0,
                                                  op0=ALU.add, op1=ALU.min), reads=[bpl, b_b1], writes=[b_tl[ti]])
            k.op("dve", lambda e: e.scalar_tensor_tensor(out=uu[ti][:], in0=tl[ti][:], scalar=-6.0, in1=s1c[ti][:],
                                                         op0=ALU.max, op1=ALU.mult), reads=[b_tl[ti], b_s1c[ti]],
                 writes=[b_uu[ti]])
            k.op("pool", lambda e: e.tensor_tensor(out=act[gi][:, ci, :], in0=uu[ti][:], in1=wbc[gi][:], op=ALU.mult),
                 reads=[b_uu[ti], b_wbc[gi]], writes=[b_act[gi]])

    def stage2(ex, g):
        gi = g % 2
        s = g * G
        if ex > 0:
            k.dma("sp", ypv[:], scr[g], reads=[b_y[g]], writes=[b_ypv])
        for f in range(8):
            py, bpy = npb(3, 7)
            for kc in range(8):
                k.op("pe", lambda e, kc=kc: e.matmul(py[:, :], lhsT=w2[:, kc, f * 128:(f + 1) * 128], rhs=act[gi][:, kc, :],
                                                     start=(kc == 0), stop=(kc == 7)), reads=[b_w2, b_act[gi]], writes=[bpy])
            k.op("dve", lambda e: e.scalar_tensor_tensor(out=ysb[:, f, :], in0=wbc[gi][:], scalar=b2[:, ex, f:f + 1], in1=py[:, :],
                                                         op0=ALU.mult, op1=ALU.add), reads=[b_wbc[gi], b_b2, bpy], writes=[b_ysb])
            if ex > 0:
                k.op("pool", lambda e: e.tensor_tensor(out=ysb[:, f, :], in0=ysb[:, f, :], in1=ypv[:, f, :], op=ALU.add),
                     reads=[b_ysb, b_ypv], writes=[b_ysb])
        k.op("act", lambda e: e.activation(out=y16[:], in_=ysb[:], func=AF.Identity), reads=[b_ysb], writes=[b_y16])
        if ex < 3:
            k.dma("sp", scr[g], y16[:], reads=[b_y16], writes=[b_y[g]])
        else:
            k.dma("sp", o_y[g], y16[:], reads=[b_y16], is_output=True)

    for ex in range(4):
        for blk in range(4):
            k.dma("sp", wst[:], d_w1[ex, blk], writes=[b_wst])
            k.op("pool", lambda e, blk=blk: e.tensor_copy(out=w1[:, :, blk * 512:(blk + 1) * 512], in_=wst[:]),
                 reads=[b_wst], writes=[b_w1])
        for blk in range(2):
            k.dma("sp", wst[:], d_w2[ex, blk], writes=[b_wst])
            k.op("pool", lambda e, blk=blk: e.tensor_copy(out=w2[:, :, blk * 512:(blk + 1) * 512], in_=wst[:]),
                 reads=[b_wst], writes=[b_w2])
        stage1(ex, 0)
        for g in range(NG):
            if g + 1 < NG:
                stage1(ex, g + 1)
            stage2(ex, g)
    return k.finish()


def build_resid(T):
    k = KB()
    d_y = k.dram("yp", [8, 128, 8, T], BF16, "ExternalInput")
    d_x = k.dram("xT", [128, 8, T], F32, "ExternalInput")
    d_g = k.dram("gf", [128, 8], F32, "ExternalInput")
    o_x = k.dram("o_x", [128, 8, T], F32, "ExternalOutput")
    G = min(512, T)
    acc = [k.sb("acc%d" % i, [128, 8, G], F32) for i in range(2)]; b_acc = [Buf(), Buf()]
    yb = [k.sb("yb%d" % i, [128, 8, G], BF16) for i in range(3)]; b_yb = [Buf(), Buf(), Buf()]
    xb = [k.sb("xb%d" % i, [128, 8, G], F32) for i in range(2)]; b_xb = [Buf(), Buf()]
    gf = k.sb("gfs", [128, 8], F32); b_gf = Buf()
    k.dma("sp", gf[:], d_g, writes=[b_gf])
    n_y = 0
    for gi, (s, n) in enumerate(groups_of(T, G)):
        a = gi % 2
        k.dma("sp", xb[a][:, :, 0:n], d_x[:, :, s:s + n], writes=[b_xb[a]])
        k.dma("sp", yb[2][:, :, 0:n], d_y[0, :, :, s:s + n], writes=[b_yb[2]])
        k.op("dve", lambda e: e.tensor_copy(out=acc[a][:, :, 0:n], in_=yb[2][:, :, 0:n]), reads=[b_yb[2]], writes=[b_acc[a]])
        for r in range(1, 8):
            yi = n_y % 2
            n_y += 1
            k.dma("sp", yb[yi][:, :, 0:n], d_y[r, :, :, s:s + n], writes=[b_yb[yi]])
            k.op("dve", lambda e: e.tensor_tensor(out=acc[a][:, :, 0:n], in0=acc[a][:, :, 0:n], in1=yb[yi][:, :, 0:n], op=ALU.add),
                 reads=[b_acc[a], b_yb[yi]], writes=[b_acc[a]])
        for c in range(8):
            k.op("dve", lambda e, c=c: e.scalar_tensor_tensor(out=xb[a][:, c, 0:n], in0=acc[a][:, c, 0:n], scalar=gf[:, c:c + 1],
                                                             in1=xb[a][:, c, 0:n], op0=ALU.mult, op1=ALU.add),
                 reads=[b_acc[a], b_gf, b_xb[a]], writes=[b_xb[a]])
        k.dma("sp", o_x[:, :, s:s + n], xb[a][:, :, 0:n], reads=[b_xb[a]], is_output=True)
    return k.finish()

def _tile_w(W):
    K_, N_ = W.shape
    return np.ascontiguousarray(W.reshape(K_ // 128, 128, N_).transpose(1, 0, 2))


def _vec(v, nch):
    return np.ascontiguousarray(np.asarray(v).reshape(nch, 128).T)


def _t5_bucket(dist):
    dist = np.asarray(dist)
    lr = np.log(np.maximum(dist, 1).astype(np.float32) / np.float32(16)) / np.float32(math.log(128 / 16))
    large = 16 + (lr * np.float32(16)).astype(np.int32)
    return np.where(dist < 16, dist, np.minimum(large, 31))


def _run(nc, maps):
    res = run_bass_kernel_spmd(nc, maps, core_ids=list(range(len(maps))))
    return res.results


_DEBUG = {}


def kernel(x, c, w_ada, b_ada, norm_mix, norm_ffn, w_in, conv_w, conv_b, lru_wa, lru_ba, lru_wx, lru_bx, lru_lambda,
           diff_qnorm, diff_knorm, diff_lambda, diff_subln, swa_qnorm, swa_knorm, swa_sinks, rel_bias, w_branch, w_out,
           w_router, b_router, w1, b1, w2, b2):
    f32 = np.float32
    x = np.asarray(x, f32)
    Bn, S, _ = x.shape
    NC = 8
    CPB = NC // Bn
    T = S // CPB
    NBLK = T // 128
    NT = Bn * S
    depth = w_ada.shape[0]
    bf = ml_dtypes.bfloat16
    ident = np.eye(128, dtype=f32)
    bdones = np.zeros((128, 128), f32)
    bdones[:64, :64] = 1.0
    bdones[64:, 64:] = 1.0
    rel_bias = np.asarray(rel_bias, f32)
    qq = np.arange(128)[None, :]
    kk = np.arange(128)[:, None]
    dbias = np.zeros((4, 128, 2, 2, 128), f32)
    d0 = qq - kk
    d1 = 128 + qq - kk
    for h in range(4):
        diag = np.where(d0 >= 0, rel_bias[_t5_bucket(np.maximum(d0, 0)), h], f32(NEG))
        prev = rel_bias[_t5_bucket(d1), h]
        for m in range(2):
            dbias[h, :, 0, m, :] = diag
            dbias[h, :, 1, m, :] = prev
    b31 = np.ascontiguousarray(np.broadcast_to(rel_bias[31, 0:4][None, :], (128, 4))).astype(f32)
    sbias = np.zeros((128, 8, 2, 128), f32)
    for hd in range(8):
        sbias[:, hd, 0, :] = np.where(d1 < 128, rel_bias[_t5_bucket(np.clip(d1, 0, 127)), 4 + hd], f32(NEG))
        sbias[:, hd, 1, :] = np.where(d0 >= 0, rel_bias[_t5_bucket(np.clip(d0, 0, 127)), 4 + hd], f32(NEG))
    sel = np.zeros((4, 4, 128), f32)
    for e in range(4):
        sel[e, e, :] = 1.0
    perm = np.concatenate([np.concatenate([np.arange(cc * 64, (cc + 1) * 64), np.arange((cc + 4) * 64, (cc + 5) * 64)])
                           for cc in range(4)])

    xcur = x.reshape(NT, 1024)
    for l in range(depth):
        lam_init = 0.8 - 0.6 * math.exp(-0.3 * l)
        W = np.asarray(w_in[l], f32)
        blk6 = np.concatenate([W[:, 3072:3328], np.zeros((1024, 256), f32)], axis=1)
        win = np.stack([_tile_w(W[:, 0:512]), _tile_w(W[:, 512:1024]), _tile_w(W[:, 1024:1536]), _tile_w(W[:, 1536:2048]),
                        _tile_w(W[:, 2048:2560]), _tile_w(W[:, 2560:3072][:, perm]), _tile_w(blk6)])
        wada_full = np.stack([_tile_w(np.asarray(w_ada[l], f32)[:, i * 512:(i + 1) * 512]) for i in range(12)])
        bada = _vec(b_ada[l], 48)
        convw = np.ascontiguousarray(np.asarray(conv_w[l], f32).reshape(4, 4, 128).transpose(2, 1, 0))
        lrus = np.stack([_vec(conv_b[l], 4), _vec(lru_ba[l], 4), _vec(lru_bx[l], 4), _vec(lru_lambda[l], 4)], axis=2)
        wlru = np.zeros((128, 2, 512), f32)
        for wi, Wl in enumerate((lru_wa[l], lru_wx[l])):
            Wl = np.asarray(Wl, f32)
            for cc in range(4):
                for hb in range(2):
                    wlru[hb * 64:(hb + 1) * 64, wi, cc * 128 + hb * 64:cc * 128 + (hb + 1) * 64] = Wl[2 * cc + hb]
        qkg = np.stack([np.tile(np.asarray(g[l], f32), 2) for g in (diff_qnorm, diff_knorm, swa_qnorm, swa_knorm)], axis=1)
        common = {"wada": None, "bada": bada, "nmix": _vec(norm_mix[l], 8), "win": win, "convw": convw,
                  "lrus": np.ascontiguousarray(lrus), "wlru": wlru, "qkg": np.ascontiguousarray(qkg), "ident": ident,
                  "bdones": bdones}

        def xT_of(core):
            b, j = divmod(core, CPB)
            start = b * S + j * T
            xs = np.zeros((T + 128, 1024), f32)
            xs[128:] = xcur[start:start + T]
            if j > 0:
                xs[:128] = xcur[start - 128:start]
            return np.ascontiguousarray(xs.T.reshape(8, 128, T + 128).transpose(1, 0, 2))

        def base_map(core, full):
            b, j = divmod(core, CPB)
            m = dict(common)
            m["wada"] = wada_full if full else np.ascontiguousarray(wada_full[:4])
            m["xT"] = xT_of(core)
            m["cvec"] = _vec(np.asarray(c, f32)[b], 8)
            m["flag"] = np.full((128, 1), 1.0 if j > 0 else 0.0, f32)
            return m

        ncA = build_mixer("A", T)
        resA = _run(ncA, [base_map(core, False) for core in range(NC)])
        Wg = W[:, 3328:6400]
        wgl = np.stack([_tile_w(np.concatenate([Wg[:, n * 1024 + f * 128:n * 1024 + (f + 1) * 128] for n in range(3)], axis=1))
                        for f in range(8)])
        Wb = np.asarray(w_branch[l], f32)
        wbr = np.stack([np.concatenate([_tile_w(Wb[n][:, f * 128:(f + 1) * 128]) for n in range(3)], axis=1) for f in range(8)])
        Wo = np.asarray(w_out[l], f32)
        wout = np.stack([_tile_w(Wo[:, 0:512]), _tile_w(Wo[:, 512:1024])])
        extraB = {"nffn": _vec(norm_ffn[l], 8), "dbias": dbias, "b31": b31, "sbias": sbias,
                  "sinks": np.ascontiguousarray(np.broadcast_to(np.asarray(swa_sinks[l], f32)[None, :], (128, 8))),
                  "dlam": np.ascontiguousarray(np.broadcast_to(np.asarray(diff_lambda[l], f32)[None], (128, 4, 64))),
                  "subln": np.ascontiguousarray(np.broadcast_to(np.asarray(diff_subln[l], f32)[None, :], (128, 128))),
                  "lamc": np.ascontiguousarray(np.broadcast_to(np.array([[-lam_init, 1.0 - lam_init]], f32), (128, 2))),
                  "wgl": wgl, "wbr": wbr, "wout": wout, "wr": _tile_w(np.asarray(w_router[l], f32)),
                  "br": np.ascontiguousarray(np.broadcast_to(np.asarray(b_router[l], f32)[None, :], (128, 32)))}
        mapsB = []
        for core in range(NC):
            b, j = divmod(core, CPB)
            m = base_map(core, True)
            m.update(extraB)
            kprev = np.zeros((4, 128, 3 * T), bf)
            vprev = np.zeros((4, 128, 3 * NBLK, 130), bf)
            carry = np.zeros((128, 4, 3, 2), f32)
            carry[:, :, :, 0] = 1.0
            for q3 in range(3):
                jj = j - 3 + q3
                if jj >= 0:
                    r = resA[b * CPB + jj]
                    kprev[:, :, q3 * T:(q3 + 1) * T] = r["o_k"]
                    vprev[:, :, q3 * NBLK:(q3 + 1) * NBLK, :] = r["o_v"]
                    carry[:, :, q3, :] = r["o_c"]
            m["kprev"], m["vprev"], m["carry"] = kprev, vprev, carry
            mapsB.append(m)
        dbg = bool(_DEBUG.get("on"))
        ncB = build_mixer("B", T, debug=dbg)
        resB = _run(ncB, mapsB)
        if dbg:
            _DEBUG.setdefault("A", []).append(resA)
            _DEBUG.setdefault("B", []).append(resB)
        hT_all = np.concatenate([r["o_hT"] for r in resB], axis=2)
        hT_all = np.ascontiguousarray(hT_all.reshape(128, 8, NT // 512, 512).transpose(2, 0, 1, 3))
        rw_all = np.concatenate([r["o_rw"] for r in resB], axis=0)
        mapsE = []
        for ec in range(NC):
            es = range(4 * ec, 4 * ec + 4)
            W1 = [np.asarray(w1[l, e], f32) for e in es]
            w1t = np.stack([np.stack([_tile_w(Wx[:, 0::2][:, 0:512]), _tile_w(Wx[:, 0::2][:, 512:1024]),
                                      _tile_w(Wx[:, 1::2][:, 0:512]), _tile_w(Wx[:, 1::2][:, 512:1024])]) for Wx in W1])
            W2 = [np.asarray(w2[l, e], f32) for e in es]
            w2t = np.stack([np.stack([_tile_w(Wx[:, 0:512]), _tile_w(Wx[:, 512:1024])]) for Wx in W2])
            b1t = np.stack([np.concatenate([_vec(np.asarray(b1[l, e], f32)[0::2], 8), _vec(np.asarray(b1[l, e], f32)[1::2], 8)],
                                           axis=1) for e in es], axis=1)
            b2t = np.stack([_vec(b2[l, e], 8) for e in es], axis=1)
            mapsE.append({"hT": hT_all, "rw": np.ascontiguousarray(rw_all[:, 4 * ec:4 * ec + 4].T), "w1": w1t, "w2": w2t,
                          "b1": np.ascontiguousarray(b1t), "b2": np.ascontiguousarray(b2t), "sel": sel})
        resE = _run(build_moe(NT), mapsE)
        if dbg:
            _DEBUG.setdefault("E", []).append(resE)
        mapsR = []
        yfull = [np.asarray(resE[r]["o_y"]).transpose(1, 2, 0, 3).reshape(128, 8, NT) for r in range(NC)]
        for core in range(NC):
            yp = np.stack([yfull[r][:, :, core * T:(core + 1) * T] for r in range(NC)])
            mapsR.append({"yp": np.ascontiguousarray(yp), "xT": resB[core]["o_xT"], "gf": resB[core]["o_gf"]})
        resR = _run(build_resid(T), mapsR)
        if _DEBUG.get("depth") == l + 1:
            depth_stop = True
        else:
            depth_stop = False
        xcur = np.concatenate([np.ascontiguousarray(r["o_x"].transpose(2, 1, 0)).reshape(T, 1024) for r in resR], axis=0)
        if depth_stop:
            break
    return np.ascontiguousarray(xcur.reshape(Bn, S, 1024).astype(f32))
```

```python
import contextlib
import math
import numpy as np
import ml_dtypes
import concourse.bass as bass
import concourse.mybir as mybir
from concourse.bass_utils import run_bass_kernel_spmd

F32 = mybir.dt.float32
BF16 = mybir.dt.bfloat16
AF = mybir.ActivationFunctionType
ALU = mybir.AluOpType
AX = mybir.AxisListType

D = 1024
NEXP = 32
DFF = 1024
EPS = 1e-6
NEG = -30000.0


class Buf:
    __slots__ = ("w", "r")

    def __init__(self):
        self.w = None
        self.r = {}


class KB:
    def __init__(self):
        self.nc = bass.Bass("TRN2", target_bir_lowering=False)
        self.es = contextlib.ExitStack()
        nc = self.nc
        self.eng = {"pe": nc.tensor, "act": nc.scalar, "dve": nc.vector, "pool": nc.gpsimd, "sp": nc.sync}
        self.sems = []
        self.esem = {}
        self.cnt = {}
        for e in self.eng:
            self.esem[e] = self._newsem("e_" + e)
            self.cnt[e] = 0
        self.seen = {e: {} for e in self.eng}
        self.ring = {}
        self.rpos = {}
        for q, n in (("sp", 12), ("act", 4), ("pool", 8)):
            self.ring[q] = [[self._newsem("d_%s%d" % (q, i)), 0] for i in range(n)]
            self.rpos[q] = 0
        self.out_tickets = []
        self.stack = [self.es]

    def push(self):
        self.stack.append(contextlib.ExitStack())

    def pop(self):
        self.barrier()
        self.stack.pop().close()

    def barrier(self):
        deps = []
        for e in self.eng:
            if self.cnt[e] > 0:
                deps.append((self.esem[e], self.cnt[e]))
        for q in self.ring:
            for slot in self.ring[q]:
                if slot[1] > 0:
                    deps.append((slot[0], slot[1]))
        for e in self.eng:
            self._wait(e, deps)

    def _newsem(self, name):
        s = self.es.enter_context(self.nc.semaphore(name))
        self.sems.append(s)
        return len(self.sems) - 1

    def sb(self, name, shape, dt):
        return self.stack[-1].enter_context(self.nc.sbuf_tensor(name, list(shape), dt))

    def ps(self, name, shape, dt=F32):
        return self.es.enter_context(self.nc.psum_tensor(name, list(shape), dt))

    def dram(self, name, shape, dt, kind):
        return self.nc.dram_tensor(name, list(shape), dt, kind=kind).ap()

    def _wait(self, e, deps):
        h = self.eng[e]
        best = {}
        for (s, v) in deps:
            if best.get(s, 0) < v:
                best[s] = v
        for s, v in best.items():
            if s == self.esem[e] and e in ("pe", "sp"):
                continue
            if self.seen[e].get(s, 0) < v:
                h.wait_ge(self.sems[s], v)
                self.seen[e][s] = v

    def _deps(self, reads, writes):
        deps = []
        for b in reads:
            if b.w is not None:
                deps.append(b.w)
        for b in writes:
            if b.w is not None:
                deps.append(b.w)
            deps.extend(b.r.items())
        return deps

    def _mark(self, t, reads, writes):
        for b in reads:
            if b.r.get(t[0], 0) < t[1]:
                b.r[t[0]] = t[1]
        for b in writes:
            b.w = t
            b.r = {}

    def op(self, e, fn, reads=(), writes=()):
        self._wait(e, self._deps(reads, writes))
        ins = fn(self.eng[e])
        self.cnt[e] += 1
        ins.then_inc(self.sems[self.esem[e]], 1)
        t = (self.esem[e], self.cnt[e])
        self._mark(t, reads, writes)
        return t

    def dma(self, q, out, in_, reads=(), writes=(), is_output=False, **kw):
        slot = self.ring[q][self.rpos[q]]
        self.rpos[q] = (self.rpos[q] + 1) % len(self.ring[q])
        deps = self._deps(reads, writes)
        if slot[1] > 0:
            deps.append((slot[0], slot[1]))
        self._wait(q, deps)
        ins = self.eng[q].dma_start(out=out, in_=in_, **kw)
        slot[1] += 16
        ins.then_inc(self.sems[slot[0]], 16)
        t = (slot[0], slot[1])
        self._mark(t, reads, writes)
        if is_output:
            self.out_tickets.append(t)
        return t

    def finish(self):
        deps = list(self.out_tickets)
        for e in self.eng:
            if self.cnt[e] > 0:
                deps.append((self.esem[e], self.cnt[e]))
        for q in self.ring:
            for slot in self.ring[q]:
                if slot[1] > 0:
                    deps.append((slot[0], slot[1]))
        self._wait("sp", deps)
        self.es.close()
        return self.nc


def groups_of(total, g):
    out = []
    s = 0
    while s < total:
        n = min(g, total - s)
        out.append((s, n))
        s += n
    return out


def build_mixer(mode, T, stop_after=None, debug=False):
    k = KB()
    NBLK = T // 128
    TT = T + 128
    NPB = 3 * NBLK
    G = min(512, T)
    full = mode == "B"
    og = groups_of(T, G)
    own_groups = [(128 + s, n) for (s, n) in og]
    tgroups = [(0, 128)] + own_groups

    d_xT = k.dram("xT", [128, 8, TT], F32, "ExternalInput")
    d_cv = k.dram("cvec", [128, 8], F32, "ExternalInput")
    NMOD = 48 if full else 16
    d_wada = k.dram("wada", [NMOD // 4, 128, 8, 512], F32, "ExternalInput")
    d_bada = k.dram("bada", [128, 48], F32, "ExternalInput")
    d_nmix = k.dram("nmix", [128, 8], F32, "ExternalInput")
    d_win = k.dram("win", [7, 128, 8, 512], F32, "ExternalInput")
    d_convw = k.dram("convw", [128, 4, 4], F32, "ExternalInput")
    d_lrus = k.dram("lrus", [128, 4, 4], F32, "ExternalInput")
    d_wlru = k.dram("wlru", [128, 2, 512], F32, "ExternalInput")
    d_qkg = k.dram("qkg", [128, 4], F32, "ExternalInput")
    d_flag = k.dram("flag", [128, 1], F32, "ExternalInput")
    d_id = k.dram("ident", [128, 128], F32, "ExternalInput")
    d_bd = k.dram("bdones", [128, 128], F32, "ExternalInput")
    if full:
        d_nffn = k.dram("nffn", [128, 8], F32, "ExternalInput")
        d_kprev = k.dram("kprev", [4, 128, NPB * 128], BF16, "ExternalInput")
        d_vprev = k.dram("vprev", [4, 128, NPB, 130], BF16, "ExternalInput")
        d_carry = k.dram("carry", [128, 4, 3, 2], F32, "ExternalInput")
        d_dbias = k.dram("dbias", [4, 128, 2, 2, 128], F32, "ExternalInput")
        d_b31 = k.dram("b31", [128, 4], F32, "ExternalInput")
        d_sbias = k.dram("sbias", [128, 8, 2, 128], F32, "ExternalInput")
        d_sink = k.dram("sinks", [128, 8], F32, "ExternalInput")
        d_dlam = k.dram("dlam", [128, 4, 64], F32, "ExternalInput")
        d_subln = k.dram("subln", [128, 128], F32, "ExternalInput")
        d_wgl = k.dram("wgl", [8, 128, 8, 384], F32, "ExternalInput")
        d_wbr = k.dram("wbr", [8, 128, 12, 128], F32, "ExternalInput")
        d_wout = k.dram("wout", [2, 128, 8, 512], F32, "ExternalInput")
        d_wr = k.dram("wr", [128, 8, 32], F32, "ExternalInput")
        d_br = k.dram("br", [128, 32], F32, "ExternalInput")
        d_lamc = k.dram("lamc", [128, 2], F32, "ExternalInput")
        if debug:
            o_dbg = k.dram("o_dbg", [128, 12, T], BF16, "ExternalOutput")
        o_xT = k.dram("o_xT", [128, 8, T], F32, "ExternalOutput")
        o_hT = k.dram("o_hT", [128, 8, T], BF16, "ExternalOutput")
        o_rw = k.dram("o_rw", [T, 32], F32, "ExternalOutput")
        o_gf = k.dram("o_gf", [128, 8], F32, "ExternalOutput")
    else:
        o_k = k.dram("o_k", [4, 128, T], BF16, "ExternalOutput")
        o_v = k.dram("o_v", [4, 128, NBLK, 130], BF16, "ExternalOutput")
        o_c = k.dram("o_c", [128, 4, 2], F32, "ExternalOutput")

    hT = k.sb("hT_s", [128, 8, TT], BF16); b_hT = Buf()
    cst = k.sb("cst", [128, 64], F32); b_cst = Buf()
    ident = k.sb("ident_s", [128, 128], F32); b_id = Buf()
    identb = k.sb("identb", [128, 128], BF16)
    bdones = k.sb("bdones_s", [128, 128], BF16); b_bd = Buf()
    ones = k.sb("ones_s", [128, 128], BF16); b_ones = Buf()
    mod = k.sb("mod", [128, 48], F32); b_mod = Buf()
    sm = k.sb("small", [128, 16], F32); b_sm = Buf()
    wst = k.sb("wst", [128, 8, 512], F32); b_wst = Buf()
    wbf = [k.sb("wbf%d" % i, [128, 8, 512], BF16) for i in range(3)]; b_wbf = [Buf(), Buf(), Buf()]
    sq = k.sb("sqs", [128, 8, G], BF16); b_sq = Buf()
    rstd = k.sb("rstd", [128, G], F32); b_rstd = Buf()
    tmpf = [k.sb("tmpf%d" % i, [128, G], F32) for i in range(2)]; b_tmpf = [Buf(), Buf()]
    tmpb = [k.sb("tmpb%d" % i, [128, G], BF16) for i in range(2)]; b_tmpb = [Buf(), Buf()]
    lsm = k.sb("lsm", [128, 16], F32); b_lsm = Buf()
    if full:
        obr = k.sb("obr", [128, 12, T], BF16); b_obr = Buf()
    pbank = [k.ps("pb%d" % i, [128, 512]) for i in range(8)]
    b_pb = [Buf() for _ in range(8)]

    k.dma("sp", ident[:], d_id, writes=[b_id])
    k.dma("sp", tmpf[0][:, 0:128], d_bd, writes=[b_tmpf[0]])
    k.op("dve", lambda e: e.tensor_copy(out=bdones[:], in_=tmpf[0][:, 0:128]), reads=[b_tmpf[0]], writes=[b_bd])
    k.op("dve", lambda e: e.memset(ones[:], 1.0), writes=[b_ones])
    k.op("dve", lambda e: e.tensor_copy(out=identb[:], in_=ident[:]), reads=[b_id], writes=[b_id])
    k.dma("sp", cst[:, 0:8], d_cv, writes=[b_cst])
    k.dma("sp", cst[:, 8:16], d_nmix, writes=[b_cst])
    k.dma("sp", cst[:, 24:40], d_convw.rearrange("p a b -> p (a b)"), writes=[b_cst])
    k.dma("sp", cst[:, 40:56], d_lrus.rearrange("p a b -> p (a b)"), writes=[b_cst])
    k.dma("sp", cst[:, 56:60], d_qkg, writes=[b_cst])
    k.dma("sp", cst[:, 60:61], d_flag, writes=[b_cst])
    k.dma("sp", mod[:], d_bada, writes=[b_mod])
    if full:
        k.dma("sp", cst[:, 16:24], d_nffn, writes=[b_cst])
    k.op("dve", lambda e: e.memset(cst[:, 61:62], EPS), writes=[b_cst])
    k.op("dve", lambda e: e.memset(cst[:, 62:63], 1.0), writes=[b_cst])
    convw = lambda c, tap: cst[:, 24 + c * 4 + tap: 25 + c * 4 + tap]
    lrus = lambda c, i: cst[:, 40 + c * 4 + i: 41 + c * 4 + i]
    epsc = cst[:, 61:62]
    onec = cst[:, 62:63]
    flag = cst[:, 60:61]

    k.op("act", lambda e: e.activation(out=cst[:, 0:8], in_=cst[:, 0:8], func=AF.Silu), reads=[b_cst], writes=[b_cst])
    for blk in range(NMOD // 4):
        k.dma("sp", wst[:], d_wada[blk], writes=[b_wst])
        pb = pbank[blk % 2]
        for fi in range(4):
            for kc in range(8):
                k.op("pe", lambda e, fi=fi, kc=kc: e.matmul(pb[:, fi:fi + 1], lhsT=wst[:, kc, fi * 128:(fi + 1) * 128],
                                                           rhs=cst[:, kc:kc + 1], start=(kc == 0), stop=(kc == 7)),
                     reads=[b_wst, b_cst], writes=[b_pb[blk % 2]])
        f0 = blk * 4
        k.op("dve", lambda e: e.tensor_tensor(out=mod[:, f0:f0 + 4], in0=pb[:, 0:4], in1=mod[:, f0:f0 + 4], op=ALU.add),
             reads=[b_pb[blk % 2], b_mod], writes=[b_mod])
    k.op("dve", lambda e: e.scalar_tensor_tensor(out=sm[:, 0:8], in0=mod[:, 8:16], scalar=1.0, in1=cst[:, 8:16],
                                                 op0=ALU.add, op1=ALU.mult), reads=[b_mod, b_cst], writes=[b_sm])
    if full:
        k.op("dve", lambda e: e.scalar_tensor_tensor(out=sm[:, 8:16], in0=mod[:, 32:40], scalar=1.0, in1=cst[:, 16:24],
                                                     op0=ALU.add, op1=ALU.mult), reads=[b_mod, b_cst], writes=[b_sm])

    def norm_mod(src, b_src, n, Acol, Bcol, dst_fn, b_dst, inplace_f32=False):
        for c in range(8):
            k.op("act", lambda e, c=c: e.activation(out=sq[:, c, 0:n], in_=src[:, c, 0:n], func=AF.Square),
                 reads=[b_src], writes=[b_sq])
        pb = pbank[2]
        for c in range(8):
            k.op("pe", lambda e, c=c: e.matmul(pb[:, 0:n], lhsT=ones[:], rhs=sq[:, c, 0:n], start=(c == 0), stop=(c == 7)),
                 reads=[b_sq, b_ones], writes=[b_pb[2]])
        k.op("act", lambda e: e.activation(out=rstd[:, 0:n], in_=pb[:, 0:n], func=AF.Sqrt, scale=1.0 / D, bias=epsc),
             reads=[b_pb[2], b_cst], writes=[b_rstd])
        k.op("dve", lambda e: e.reciprocal(out=rstd[:, 0:n], in_=rstd[:, 0:n]), reads=[b_rstd], writes=[b_rstd])
        for c in range(8):
            ti = c % 2
            k.op("dve", lambda e, c=c, ti=ti: e.scalar_tensor_tensor(out=tmpf[ti][:, 0:n], in0=src[:, c, 0:n],
                                                                    scalar=sm[:, Acol + c:Acol + c + 1], in1=rstd[:, 0:n],
                                                                    op0=ALU.mult, op1=ALU.mult),
                 reads=[b_src, b_sm, b_rstd], writes=[b_tmpf[ti]])
            if inplace_f32:
                k.op("act", lambda e, c=c, ti=ti: e.activation(out=src[:, c, 0:n], in_=tmpf[ti][:, 0:n], func=AF.Identity,
                                                               bias=mod[:, Bcol + c:Bcol + c + 1]),
                     reads=[b_tmpf[ti], b_mod], writes=[b_src])
                k.op("dve", lambda e, c=c: e.tensor_copy(out=dst_fn(c), in_=src[:, c, 0:n]), reads=[b_src], writes=[b_dst])
            else:
                k.op("act", lambda e, c=c, ti=ti: e.activation(out=dst_fn(c), in_=tmpf[ti][:, 0:n], func=AF.Identity,
                                                               bias=mod[:, Bcol + c:Bcol + c + 1]),
                     reads=[b_tmpf[ti], b_mod], writes=[b_dst])

    k.push()
    xg = k.sb("xg", [128, 8, 512], F32); b_xg = Buf()
    for (s, n) in tgroups:
        k.dma("sp", xg[:, :, 0:n], d_xT[:, :, s:s + n], writes=[b_xg])
        norm_mod(xg, b_xg, n, 0, 0, lambda c, s=s, n=n: hT[:, c, s:s + n], b_hT)
    k.pop()

    def load_w(slot, src, rows=8, cols=512):
        k.dma("sp", wst[:, 0:rows, 0:cols], src, writes=[b_wst])
        k.op("pool", lambda e: e.tensor_copy(out=wbf[slot][:, 0:rows, 0:cols], in_=wst[:, 0:rows, 0:cols]),
             reads=[b_wst], writes=[b_wbf[slot]])
        return wbf[slot], b_wbf[slot]

    pstate = {"i": 0}

    def next_pb():
        i = 4 + (pstate["i"] % 4)
        pstate["i"] += 1
        return pbank[i], b_pb[i]

    def linear_fm(w, b_w, chunks, src, b_src, KC, groups, evac):
        for (s, n) in groups:
            for nci in chunks:
                pb, bpb = next_pb()
                for kc in range(KC):
                    k.op("pe", lambda e, kc=kc, nci=nci: e.matmul(pb[:, 0:n], lhsT=w[:, kc, nci * 128:(nci + 1) * 128],
                                                                 rhs=src[:, kc, s:s + n], start=(kc == 0), stop=(kc == KC - 1)),
                         reads=[b_w, b_src], writes=[bpb])
                evac(nci, s, n, pb, bpb)

    def qknorm(dst_fn, b_dst, gcol):
        def ev(nci, s, n, pb, bpb):
            ti = nci % 2
            k.op("act", lambda e: e.activation(out=tmpb[ti][:, 0:n], in_=pb[:, 0:n], func=AF.Square),
                 reads=[bpb], writes=[b_tmpb[ti]])
            k.op("act", lambda e: e.activation(out=tmpf[ti][:, 0:n], in_=pb[:, 0:n], func=AF.Identity),
                 reads=[bpb], writes=[b_tmpf[ti]])
            p2 = pbank[3]
            k.op("pe", lambda e: e.matmul(p2[:, 0:n], lhsT=bdones[:], rhs=tmpb[ti][:, 0:n], start=True, stop=True),
                 reads=[b_bd, b_tmpb[ti]], writes=[b_pb[3]])
            k.op("act", lambda e: e.activation(out=rstd[:, 0:n], in_=p2[:, 0:n], func=AF.Sqrt, scale=1.0 / 64, bias=epsc),
                 reads=[b_pb[3], b_cst], writes=[b_rstd])
            k.op("dve", lambda e: e.reciprocal(out=rstd[:, 0:n], in_=rstd[:, 0:n]), reads=[b_rstd], writes=[b_rstd])
            k.op("dve", lambda e: e.scalar_tensor_tensor(out=dst_fn(nci, s, n), in0=tmpf[ti][:, 0:n],
                                                         scalar=cst[:, 56 + gcol:57 + gcol], in1=rstd[:, 0:n],
                                                         op0=ALU.mult, op1=ALU.mult),
                 reads=[b_tmpf[ti], b_cst, b_rstd], writes=[b_dst])
        return ev

    k.push()
    xr = k.sb("xr", [128, TT], F32); b_xr = Buf()
    xc = k.sb("xc", [128, T], F32); b_xc = Buf()
    xcb = k.sb("xcb", [128, 1, T], BF16); b_xcb = Buf()
    la = k.sb("la", [128, T], F32); b_la = Buf()
    bb = k.sb("bbuf", [128, T], F32); b_bb = Buf()
    hh = k.sb("hh", [128, T], F32); b_hh = Buf()
    wl = k.sb("wl", [128, 2, 512], BF16); b_wl = Buf()
    cout = k.sb("cout", [128, 4, 2], F32); b_cout = Buf()
    carry = k.sb("carry_s", [128, 4, 3, 2], F32); b_carry = Buf()
    w0, bw0 = load_w(0, d_win[0])
    w1, bw1 = load_w(1, d_win[1])
    k.dma("sp", wst[:, 0:2, :], d_wlru, writes=[b_wst])
    k.op("pool", lambda e: e.tensor_copy(out=wl[:], in_=wst[:, 0:2, :]), reads=[b_wst], writes=[b_wl])
    k.op("act", lambda e: e.activation(out=lsm[:, 0:4], in_=cst[:, 40:56].rearrange("p (c i) -> p c i", i=4)[:, :, 3],
                                       func=AF.Exp, scale=-1.0), reads=[b_cst], writes=[b_lsm])
    k.op("act", lambda e: e.activation(out=lsm[:, 0:4], in_=lsm[:, 0:4], func=AF.Ln, bias=onec), reads=[b_lsm, b_cst],
         writes=[b_lsm])
    k.op("dve", lambda e: e.tensor_scalar(out=lsm[:, 0:4], in0=lsm[:, 0:4], scalar1=-8.0, scalar2=None, op0=ALU.mult),
         reads=[b_lsm], writes=[b_lsm])
    k.op("dve", lambda e: e.memset(lsm[:, 8:12], 0.0), writes=[b_lsm])
    if full:
        k.dma("sp", carry[:], d_carry, writes=[b_carry])
        for kk in range(3):
            k.op("dve", lambda e, kk=kk: e.tensor_tensor(out=lsm[:, 8:12], in0=lsm[:, 8:12], in1=carry[:, :, kk, 0], op=ALU.mult),
                 reads=[b_lsm, b_carry], writes=[b_lsm])
            k.op("dve", lambda e, kk=kk: e.tensor_tensor(out=lsm[:, 8:12], in0=lsm[:, 8:12], in1=carry[:, :, kk, 1], op=ALU.add),
                 reads=[b_lsm, b_carry], writes=[b_lsm])
    C0 = 2.0 * math.sqrt(2.0 / math.pi)
    for c in range(4):
        def ev_xr(nci, s, n, pb, bpb):
            k.op("act", lambda e: e.activation(out=xr[:, s:s + n], in_=pb[:, 0:n], func=AF.Identity), reads=[bpb], writes=[b_xr])
        linear_fm(w0, bw0, [c], hT, b_hT, 8, tgroups, ev_xr)
        k.op("dve", lambda e: e.tensor_scalar(out=xr[:, 0:128], in0=xr[:, 0:128], scalar1=flag, scalar2=None, op0=ALU.mult),
             reads=[b_xr, b_cst], writes=[b_xr])
        k.op("dve", lambda e, c=c: e.tensor_scalar(out=xc[:], in0=xr[:, 125:125 + T], scalar1=convw(c, 0), scalar2=lrus(c, 0),
                                                   op0=ALU.mult, op1=ALU.add), reads=[b_xr, b_cst], writes=[b_xc])
        for tap in range(1, 4):
            k.op("dve", lambda e, c=c, tap=tap: e.scalar_tensor_tensor(out=xc[:], in0=xr[:, 125 + tap:125 + tap + T],
                                                                      scalar=convw(c, tap), in1=xc[:], op0=ALU.mult,
                                                                      op1=ALU.add), reads=[b_xr, b_cst, b_xc], writes=[b_xc])
        k.op("pool", lambda e: e.tensor_copy(out=xcb[:, 0, :], in_=xc[:]), reads=[b_xc], writes=[b_xcb])

        def ev_ga(nci, s, n, pb, bpb, c=c):
            k.op("act", lambda e: e.activation(out=la[:, s:s + n], in_=pb[:, 0:n], func=AF.Sigmoid, bias=lrus(c, 1)),
                 reads=[bpb, b_cst], writes=[b_la])

        def ev_gx(nci, s, n, pb, bpb, c=c):
            k.op("act", lambda e: e.activation(out=bb[:, s:s + n], in_=pb[:, 0:n], func=AF.Sigmoid, bias=lrus(c, 2)),
                 reads=[bpb, b_cst], writes=[b_bb])
        linear_fm(wl[:, 0:1, :], b_wl, [c], xcb, b_xcb, 1, og, ev_ga)
        linear_fm(wl[:, 1:2, :], b_wl, [c], xcb, b_xcb, 1, og, ev_gx)
        k.op("dve", lambda e, c=c: e.tensor_scalar(out=la[:], in0=la[:], scalar1=lsm[:, c:c + 1], scalar2=None, op0=ALU.mult),
             reads=[b_la, b_lsm], writes=[b_la])
        if not full:
            k.op("dve", lambda e, c=c: e.tensor_reduce(out=lsm[:, 12 + c:13 + c], in_=la[:], axis=AX.X, op=ALU.add),
                 reads=[b_la], writes=[b_lsm])
        k.op("act", lambda e: e.activation(out=hh[:], in_=la[:], func=AF.Exp, scale=2.0), reads=[b_la], writes=[b_hh])
        k.op("act", lambda e: e.activation(out=hh[:], in_=hh[:], func=AF.Sqrt, scale=-1.0, bias=onec), reads=[b_hh, b_cst],
             writes=[b_hh])
        k.op("act", lambda e: e.activation(out=la[:], in_=la[:], func=AF.Exp), reads=[b_la], writes=[b_la])
        k.op("dve", lambda e: e.tensor_tensor(out=bb[:], in0=bb[:], in1=xc[:], op=ALU.mult), reads=[b_bb, b_xc], writes=[b_bb])
        k.op("dve", lambda e: e.tensor_tensor(out=bb[:], in0=bb[:], in1=hh[:], op=ALU.mult), reads=[b_bb, b_hh], writes=[b_bb])
        k.op("dve", lambda e, c=c: e.tensor_tensor_scan(out=hh[:], data0=la[:], data1=bb[:], initial=lsm[:, 8 + c:9 + c],
                                                        op0=ALU.mult, op1=ALU.add),
             reads=[b_la, b_bb, b_lsm, b_hh], writes=[b_hh])
        if not full:
            k.op("dve", lambda e, c=c: e.tensor_copy(out=cout[:, c, 1:2], in_=hh[:, T - 1:T]), reads=[b_hh], writes=[b_cout])
        else:
            def ev_gr(nci, s, n, pb, bpb):
                ti = 0
                so = s - 128
                k.op("act", lambda e: e.activation(out=tmpf[ti][:, 0:n], in_=pb[:, 0:n], func=AF.Square), reads=[bpb],
                     writes=[b_tmpf[ti]])
                k.op("dve", lambda e: e.tensor_scalar(out=tmpf[ti][:, 0:n], in0=tmpf[ti][:, 0:n], scalar1=0.044715, scalar2=1.0,
                                                      op0=ALU.mult, op1=ALU.add), reads=[b_tmpf[ti]], writes=[b_tmpf[ti]])
                k.op("dve", lambda e: e.tensor_tensor(out=tmpf[ti][:, 0:n], in0=tmpf[ti][:, 0:n], in1=pb[:, 0:n], op=ALU.mult),
                     reads=[b_tmpf[ti], bpb], writes=[b_tmpf[ti]])
                k.op("act", lambda e: e.activation(out=tmpf[ti][:, 0:n], in_=tmpf[ti][:, 0:n], func=AF.Sigmoid, scale=C0),
                     reads=[b_tmpf[ti]], writes=[b_tmpf[ti]])
                k.op("dve", lambda e: e.tensor_tensor(out=tmpf[ti][:, 0:n], in0=tmpf[ti][:, 0:n], in1=pb[:, 0:n], op=ALU.mult),
                     reads=[b_tmpf[ti], bpb], writes=[b_tmpf[ti]])
                k.op("dve", lambda e: e.tensor_tensor(out=obr[:, nci, so:so + n], in0=tmpf[ti][:, 0:n], in1=hh[:, so:so + n],
                                                      op=ALU.mult), reads=[b_tmpf[ti], b_hh], writes=[b_obr])
            linear_fm(w1, bw1, [c], hT, b_hT, 8, own_groups, ev_gr)
    if not full:
        k.op("act", lambda e: e.activation(out=cout[:, :, 0], in_=lsm[:, 12:16], func=AF.Exp), reads=[b_lsm], writes=[b_cout])
        k.dma("sp", o_c, cout[:], reads=[b_cout], is_output=True)
    k.pop()

    if stop_after == "lru":
        return k.finish()
    k.push()
    kT = k.sb("kT", [128, T], BF16); b_kT = Buf()
    vA = k.sb("vA", [128, NBLK, 130], BF16); b_vA = Buf()
    wq, bwq = (load_w(0, d_win[2]) if full else (None, None))
    wk, bwk = load_w(1, d_win[3])
    wv, bwv = load_w(2, d_win[4])
    if full:
        qT = k.sb("qT", [128, T], BF16); b_qT = Buf()
        kprev = k.sb("kprev_s", [128, NPB * 128], BF16); b_kprev = Buf()
        vprev = k.sb("vprev_s", [128, NPB, 130], BF16); b_vprev = Buf()
        dbias = k.sb("dbias_s", [128, 2, 2, 128], F32); b_db = Buf()
        pT = [k.sb("pT%d" % i, [128, 2, 128], BF16) for i in range(2)]; b_pT = [Buf(), Buf()]
        sF = [k.sb("sF%d" % i, [128, 2, 128], F32) for i in range(2)]; b_sF = [Buf(), Buf()]
        acs = k.sb("acs", [128, 2, 130], F32); b_acs = Buf()
        otokb = k.sb("otokb", [128, 128], BF16); b_otokb = Buf()
        att = k.sb("att", [128, 32], F32); b_att = Buf()
        subln = k.sb("subln_s", [128, 128], F32); b_subln = Buf()
        dlam = k.sb("dlam_s", [128, 4, 64], F32); b_dlam = Buf()
        k.dma("sp", subln[:], d_subln, writes=[b_subln])
        k.dma("sp", dlam[:], d_dlam, writes=[b_dlam])
        k.dma("sp", att[:, 0:4], d_b31, writes=[b_att])
        k.dma("sp", att[:, 24:26], d_lamc, writes=[b_att])
        k.op("dve", lambda e: e.tensor_tensor(out=dlam[:, 0, :], in0=dlam[:, 0, :], in1=dlam[:, 1, :], op=ALU.mult),
             reads=[b_dlam], writes=[b_dlam])
        k.op("dve", lambda e: e.tensor_tensor(out=dlam[:, 2, :], in0=dlam[:, 2, :], in1=dlam[:, 3, :], op=ALU.mult),
             reads=[b_dlam], writes=[b_dlam])
        k.op("dve", lambda e: e.tensor_reduce(out=att[:, 14:15], in_=dlam[:, 0, :], axis=AX.X, op=ALU.add),
             reads=[b_dlam], writes=[b_att])
        k.op("dve", lambda e: e.tensor_reduce(out=att[:, 15:16], in_=dlam[:, 2, :], axis=AX.X, op=ALU.add),
             reads=[b_dlam], writes=[b_att])
        k.op("act", lambda e: e.activation(out=att[:, 14:16], in_=att[:, 14:16], func=AF.Exp), reads=[b_att], writes=[b_att])
        k.op("dve", lambda e: e.scalar_tensor_tensor(out=att[:, 13:14], in0=att[:, 15:16], scalar=att[:, 24:25], in1=att[:, 14:15],
                                                     op0=ALU.add, op1=ALU.subtract), reads=[b_att], writes=[b_att])
        k.op("dve", lambda e: e.tensor_scalar(out=subln[:], in0=subln[:], scalar1=att[:, 25:26], scalar2=None, op0=ALU.mult),
             reads=[b_subln, b_att], writes=[b_subln])
    for h in range(4):
        linear_fm(wk, bwk, [h], hT, b_hT, 8, own_groups, qknorm(lambda nci, s, n: kT[:, s - 128:s - 128 + n], b_kT, 1))
        k.op("dve", lambda e: e.memset(vA[:, :, 128:130], 1.0), writes=[b_vA])
        for blk in range(NBLK):
            pb, bpb = next_pb()
            for kc in range(8):
                k.op("pe", lambda e, kc=kc, blk=blk: e.matmul(pb[:, 0:128], lhsT=hT[:, kc, 128 + blk * 128:256 + blk * 128],
                                                             rhs=wv[:, kc, h * 128:(h + 1) * 128], start=(kc == 0), stop=(kc == 7)),
                     reads=[b_hT, bwv], writes=[bpb])
            k.op("act", lambda e, blk=blk: e.activation(out=vA[:, blk, 0:128], in_=pb[:, 0:128], func=AF.Identity),
                 reads=[bpb], writes=[b_vA])
        if not full:
            k.dma("sp", o_k[h], kT[:], reads=[b_kT], is_output=True)
            k.dma("sp", o_v[h], vA[:], reads=[b_vA], is_output=True)
            continue
        linear_fm(wq, bwq, [h], hT, b_hT, 8, own_groups, qknorm(lambda nci, s, n: qT[:, s - 128:s - 128 + n], b_qT, 0))
        k.dma("sp", kprev[:], d_kprev[h], writes=[b_kprev])
        k.dma("sp", vprev[:], d_vprev[h], writes=[b_vprev])
        k.dma("sp", dbias[:], d_dbias[h], writes=[b_db])
        for i in range(NBLK):
            kbl = [("p", j) for j in range(NPB)] + [("o", j) for j in range(i + 1)]
            acc = [pbank[0], pbank[1]]
            L = len(kbl)

            def d_info(idx):
                kind, j = kbl[idx]
                si = idx % 2
                if kind == "p":
                    return (si, kprev[:, j * 128:(j + 1) * 128], vprev[:, j, 0:129], b_kprev, b_vprev,
                            (1 if (i == 0 and j == NPB - 1) else None))
                return (si, kT[:, j * 128:(j + 1) * 128], vA[:, j, 0:129], b_kT, b_vA,
                        (0 if j == i else (1 if j == i - 1 else None)))

            def d_qk(idx):
                si, ksrc, vsrc, kb_, vb_, near = d_info(idx)
                pSm = [pbank[2 + si], pbank[4 + si]]
                bSm = [b_pb[2 + si], b_pb[4 + si]]
                for m in range(2):
                    k.op("pe", lambda e, m=m: e.matmul(pSm[m][:, 0:128], lhsT=ksrc[m * 64:(m + 1) * 64, :],
                                                       rhs=qT[m * 64:(m + 1) * 64, i * 128:(i + 1) * 128],
                                                       start=True, stop=True),
                         reads=[kb_, b_qT], writes=[bSm[m]])

            def d_exp(idx):
                si, ksrc, vsrc, kb_, vb_, near = d_info(idx)
                pSm = [pbank[2 + si], pbank[4 + si]]
                bSm = [b_pb[2 + si], b_pb[4 + si]]
                for m in range(2):
                    if near is None:
                        k.op("act", lambda e, m=m: e.activation(out=pT[si][:, m, :], in_=pSm[m][:, 0:128], func=AF.Exp,
                                                                scale=0.125, bias=att[:, h:h + 1]),
                             reads=[bSm[m], b_att], writes=[b_pT[si]])
                    else:
                        k.op("dve", lambda e, m=m: e.scalar_tensor_tensor(
                            out=sF[si][:, m, :], in0=pSm[m][:, 0:128], scalar=0.125, in1=dbias[:, near, m, :],
                            op0=ALU.mult, op1=ALU.add), reads=[bSm[m], b_db], writes=[b_sF[si]])
                        k.op("act", lambda e, m=m: e.activation(out=pT[si][:, m, :], in_=sF[si][:, m, :], func=AF.Exp),
                             reads=[b_sF[si]], writes=[b_pT[si]])

            def d_pv(idx):
                si, ksrc, vsrc, kb_, vb_, near = d_info(idx)
                for m in range(2):
                    k.op("pe", lambda e, m=m: e.matmul(acc[m][:, 0:129], lhsT=pT[si][:, m, :], rhs=vsrc,
                                                       start=(idx == 0), stop=(idx == L - 1)),
                         reads=[b_pT[si], vb_], writes=[b_pb[m]])

            d_qk(0)
            for idx in range(L):
                if idx + 1 < L:
                    d_qk(idx + 1)
                d_exp(idx)
                d_pv(idx)
            for m in range(2):
                k.op("act", lambda e, m=m: e.activation(out=acs[:, m, 0:129], in_=acc[m][:, 0:129], func=AF.Identity),
                     reads=[b_pb[m]], writes=[b_acs])
            k.op("dve", lambda e: e.reciprocal(out=att[:, 16:18], in_=acs[:, :, 128]), reads=[b_acs], writes=[b_att])
            k.op("dve", lambda e: e.tensor_tensor(out=att[:, 17:18], in0=att[:, 17:18], in1=att[:, 13:14], op=ALU.mult),
                 reads=[b_att], writes=[b_att])
            k.op("dve", lambda e: e.tensor_scalar(out=acs[:, 0, 0:128], in0=acs[:, 0, 0:128], scalar1=att[:, 16:17], scalar2=None,
                                                  op0=ALU.mult), reads=[b_acs, b_att], writes=[b_acs])
            k.op("dve", lambda e: e.scalar_tensor_tensor(out=acs[:, 0, 0:128], in0=acs[:, 1, 0:128], scalar=att[:, 17:18],
                                                         in1=acs[:, 0, 0:128], op0=ALU.mult, op1=ALU.add),
                 reads=[b_acs, b_att], writes=[b_acs])
            k.op("dve", lambda e: e.tensor_tensor(out=acs[:, 1, 0:128], in0=acs[:, 0, 0:128], in1=acs[:, 0, 0:128], op=ALU.mult),
                 reads=[b_acs], writes=[b_acs])
            k.op("dve", lambda e: e.tensor_reduce(out=att[:, 18:19], in_=acs[:, 1, 0:128], axis=AX.X, op=ALU.add),
                 reads=[b_acs], writes=[b_att])
            k.op("act", lambda e: e.activation(out=att[:, 18:19], in_=att[:, 18:19], func=AF.Sqrt, scale=1.0 / 128, bias=epsc),
                 reads=[b_att, b_cst], writes=[b_att])
            k.op("dve", lambda e: e.reciprocal(out=att[:, 18:19], in_=att[:, 18:19]), reads=[b_att], writes=[b_att])
            k.op("dve", lambda e: e.scalar_tensor_tensor(out=otokb[:], in0=acs[:, 0, 0:128], scalar=att[:, 18:19], in1=subln[:],
                                                         op0=ALU.mult, op1=ALU.mult),
                 reads=[b_acs, b_att, b_subln], writes=[b_otokb])
            pt, bpt = next_pb()
            k.op("pe", lambda e: e.matmul(pt[:, 0:128], lhsT=otokb[:], rhs=identb[:], start=True, stop=True),
                 reads=[b_otokb, b_id], writes=[bpt])
            k.op("act", lambda e, i=i: e.activation(out=obr[:, 4 + h, i * 128:(i + 1) * 128], in_=pt[:, 0:128], func=AF.Identity),
                 reads=[bpt], writes=[b_obr])
    k.pop()
    if not full or stop_after == "diff":
        return k.finish()

    k.push()
    sqT = k.sb("sqT", [128, 4, T], BF16); b_sqT = Buf()
    skT = k.sb("skT", [128, TT], BF16); b_skT = Buf()
    svA = k.sb("svA", [128, NBLK + 1, 2, 66], BF16); b_svA = Buf()
    sbias = k.sb("sbias_s", [128, 8, 2, 128], F32); b_sb = Buf()
    pT = [k.sb("spT%d" % i, [128, 2, 128], BF16) for i in range(2)]; b_pT = [Buf(), Buf()]
    sF = [k.sb("ssF%d" % i, [128, 2, 128], F32) for i in range(2)]; b_sF = [Buf(), Buf()]
    otokb = k.sb("sotokb", [128, 512], BF16); b_otokb = Buf()
    att = k.sb("satt", [128, 32], F32); b_att = Buf()
    k.dma("sp", sbias[:], d_sbias, writes=[b_sb])
    k.dma("sp", att[:, 4:12], d_sink, writes=[b_att])
    k.op("act", lambda e: e.activation(out=att[:, 4:12], in_=att[:, 4:12], func=AF.Exp), reads=[b_att], writes=[b_att])
    w, bw = load_w(0, d_win[5])
    linear_fm(w, bw, [0, 1, 2, 3], hT, b_hT, 8, own_groups,
              qknorm(lambda nci, s, n: sqT[:, nci, s - 128:s - 128 + n], b_sqT, 2))
    w, bw = load_w(1, d_win[6])
    linear_fm(w, bw, [0], hT, b_hT, 8, tgroups, qknorm(lambda nci, s, n: skT[:, s:s + n], b_skT, 3))
    k.op("dve", lambda e: e.memset(svA[:, :, :, 64:66], 1.0), writes=[b_svA])
    for blk in range(NBLK + 1):
        pb, bpb = next_pb()
        for kc in range(8):
            k.op("pe", lambda e, kc=kc, blk=blk: e.matmul(pb[:, 0:128], lhsT=hT[:, kc, blk * 128:(blk + 1) * 128],
                                                         rhs=w[:, kc, 128:256], start=(kc == 0), stop=(kc == 7)),
                 reads=[b_hT, bw], writes=[bpb])
        k.op("act", lambda e, blk=blk: e.activation(out=svA[:, blk, :, 0:64], in_=pb[:, 0:128].rearrange("p (h d) -> p h d", h=2),
                                                    func=AF.Identity), reads=[bpb], writes=[b_svA])
    k.op("dve", lambda e: e.tensor_scalar(out=svA[:, 0, :, :], in0=svA[:, 0, :, :], scalar1=flag, scalar2=None, op0=ALU.mult),
         reads=[b_svA, b_cst], writes=[b_svA])
    steps = [(i, hd) for i in range(NBLK) for hd in range(8)]

    def s_qk(st):
        i, hd = steps[st]
        kv, c, si = hd // 4, hd % 4, st % 2
        pS, bS = pbank[2 + si], b_pb[2 + si]
        for bi in range(2):
            k.op("pe", lambda e, bi=bi: e.matmul(
                pS[:, bi * 128:(bi + 1) * 128], lhsT=skT[kv * 64:(kv + 1) * 64, (i + bi) * 128:(i + bi + 1) * 128],
                rhs=sqT[kv * 64:(kv + 1) * 64, c, i * 128:(i + 1) * 128], start=True, stop=True),
                reads=[b_skT, b_sqT], writes=[bS])

    def s_soft(st):
        i, hd = steps[st]
        si = st % 2
        pS, bS = pbank[2 + si], b_pb[2 + si]
        pv = pS[:, 0:256].rearrange("p (m q) -> p m q", m=2)
        k.op("dve", lambda e: e.scalar_tensor_tensor(out=sF[si][:], in0=pv, scalar=0.125, in1=sbias[:, hd], op0=ALU.mult,
                                                     op1=ALU.add), reads=[bS, b_sb], writes=[b_sF[si]])
        k.op("act", lambda e: e.activation(out=pT[si][:], in_=sF[si][:], func=AF.Exp), reads=[b_sF[si]], writes=[b_pT[si]])

    def s_pv(st):
        i, hd = steps[st]
        kv, si = hd // 4, st % 2
        pa, ba_ = pbank[si], b_pb[si]
        for bi in range(2):
            k.op("pe", lambda e, bi=bi: e.matmul(pa[:, 0:65], lhsT=pT[si][:, bi, :], rhs=svA[:, i + bi, kv, 0:65],
                                                 start=(bi == 0), stop=(bi == 1)), reads=[b_pT[si], b_svA], writes=[ba_])
        k.op("dve", lambda e: e.tensor_tensor(out=att[:, 20 + hd:21 + hd], in0=pa[:, 64:65], in1=att[:, 4 + hd:5 + hd],
                                              op=ALU.add), reads=[ba_, b_att], writes=[b_att])
        k.op("dve", lambda e: e.reciprocal(out=att[:, 20 + hd:21 + hd], in_=att[:, 20 + hd:21 + hd]), reads=[b_att],
             writes=[b_att])
        k.op("dve", lambda e: e.tensor_scalar(out=otokb[:, hd * 64:(hd + 1) * 64], in0=pa[:, 0:64],
                                              scalar1=att[:, 20 + hd:21 + hd], scalar2=None, op0=ALU.mult),
             reads=[ba_, b_att], writes=[b_otokb])
        if hd == 7:
            for c in range(4):
                pt, bpt = next_pb()
                k.op("pe", lambda e, c=c: e.matmul(pt[:, 0:128], lhsT=otokb[:, c * 128:(c + 1) * 128], rhs=identb[:],
                                                   start=True, stop=True), reads=[b_otokb, b_id], writes=[bpt])
                k.op("act", lambda e, c=c: e.activation(out=obr[:, 8 + c, i * 128:(i + 1) * 128], in_=pt[:, 0:128],
                                                        func=AF.Identity), reads=[bpt], writes=[b_obr])

    s_qk(0)
    for st in range(len(steps)):
        if st + 1 < len(steps):
            s_qk(st + 1)
        s_soft(st)
        s_pv(st)
    k.pop()

    if debug:
        k.dma("sp", o_dbg, obr[:], reads=[b_obr], is_output=True)
    if stop_after == "swa":
        return k.finish()
    k.push()
    mg = k.sb("mg", [128, 8, G], BF16); b_mg = Buf()
    xg = k.sb("xg2", [128, 8, G], F32); b_xg = Buf()
    wo = k.sb("wo", [128, 8, 1024], BF16); b_wo = Buf()
    wr = k.sb("wr_s", [128, 8, 32], F32); b_wr = Buf()
    brt = k.sb("br_s", [128, 32], F32); b_br = Buf()
    lg = k.sb("lg", [128, 32], F32); b_lg = Buf()
    t8 = k.sb("t8", [128, 8], F32); b_t8 = Buf()
    rwt = k.sb("rwt", [128, 32], F32); b_rwt = Buf()
    k.dma("sp", wr[:], d_wr, writes=[b_wr])
    k.dma("sp", brt[:], d_br, writes=[b_br])
    k.dma("sp", o_gf, mod[:, 40:48], reads=[b_mod], is_output=True)
    for half in range(2):
        k.dma("sp", wst[:], d_wout[half], writes=[b_wst])
        k.op("pool", lambda e, half=half: e.tensor_copy(out=wo[:, :, half * 512:(half + 1) * 512], in_=wst[:]),
             reads=[b_wst], writes=[b_wo])
    for (s, n) in og:
        k.dma("sp", xg[:, :, 0:n], d_xT[:, :, 128 + s:128 + s + n], writes=[b_xg])
        for f in range(8):
            k.dma("sp", wst[:, :, 0:384], d_wgl[f], writes=[b_wst])
            k.dma("sp", wst[:, :, 384:512], d_wbr[f][:, 0:8, :], writes=[b_wst])
            k.op("pool", lambda e: e.tensor_copy(out=wbf[0][:], in_=wst[:]), reads=[b_wst], writes=[b_wbf[0]])
            k.dma("sp", wst[:, 0:4, 0:128], d_wbr[f][:, 8:12, :], writes=[b_wst])
            k.op("pool", lambda e: e.tensor_copy(out=wbf[1][:, 0:4, 0:128], in_=wst[:, 0:4, 0:128]), reads=[b_wst],
                 writes=[b_wbf[1]])
            wg, bwg = wbf[0], b_wbf[0]
            w2_, bw2_ = wbf[1], b_wbf[1]
            for nb in range(3):
                pg, bpg = next_pb()
                for kc in range(8):
                    k.op("pe", lambda e, kc=kc, nb=nb, pg=pg: e.matmul(pg[:, 0:n], lhsT=wg[:, kc, nb * 128:(nb + 1) * 128],
                                                                      rhs=hT[:, kc, 128 + s:128 + s + n],
                                                                      start=(kc == 0), stop=(kc == 7)),
                         reads=[bwg, b_hT], writes=[bpg])
                ti = nb % 2
                k.op("act", lambda e, ti=ti, pg=pg: e.activation(out=tmpf[ti][:, 0:n], in_=pg[:, 0:n], func=AF.Sigmoid),
                     reads=[bpg], writes=[b_tmpf[ti]])
                pp, bpp = next_pb()
                for kc in range(4):
                    if nb < 2:
                        lw, lb = wg[:, nb * 4 + kc, 384:512], bwg
                    else:
                        lw, lb = w2_[:, kc, 0:128], bw2_
                    k.op("pe", lambda e, kc=kc, lw=lw, nb=nb, pp=pp: e.matmul(pp[:, 0:n], lhsT=lw, rhs=obr[:, nb * 4 + kc, s:s + n],
                                                                             start=(kc == 0), stop=(kc == 3)),
                         reads=[lb, b_obr], writes=[bpp])
                if nb == 0:
                    k.op("dve", lambda e, ti=ti, pp=pp: e.tensor_tensor(out=rstd[:, 0:n], in0=pp[:, 0:n], in1=tmpf[ti][:, 0:n],
                                                                       op=ALU.mult), reads=[bpp, b_tmpf[ti]], writes=[b_rstd])
                else:
                    k.op("dve", lambda e, ti=ti, pp=pp: e.tensor_tensor(out=tmpf[ti][:, 0:n], in0=pp[:, 0:n],
                                                                       in1=tmpf[ti][:, 0:n], op=ALU.mult),
                         reads=[bpp, b_tmpf[ti]], writes=[b_tmpf[ti]])
                    if nb == 1:
                        k.op("dve", lambda e, ti=ti: e.tensor_tensor(out=rstd[:, 0:n], in0=rstd[:, 0:n], in1=tmpf[ti][:, 0:n],
                                                                    op=ALU.add), reads=[b_rstd, b_tmpf[ti]], writes=[b_rstd])
                    else:
                        k.op("dve", lambda e, ti=ti, f=f: e.tensor_tensor(out=mg[:, f, 0:n], in0=rstd[:, 0:n],
                                                                         in1=tmpf[ti][:, 0:n], op=ALU.add),
                             reads=[b_rstd, b_tmpf[ti]], writes=[b_mg])

        def ev_out(nci, s_, n_, pb, bpb):
            k.op("dve", lambda e: e.scalar_tensor_tensor(out=xg[:, nci, 0:n_], in0=pb[:, 0:n_], scalar=mod[:, 16 + nci:17 + nci],
                                                         in1=xg[:, nci, 0:n_], op0=ALU.mult, op1=ALU.add),
                 reads=[bpb, b_mod, b_xg], writes=[b_xg])
        linear_fm(wo, b_wo, list(range(8)), mg, b_mg, 8, [(0, n)], ev_out)
        k.dma("sp", o_xT[:, :, s:s + n], xg[:, :, 0:n], reads=[b_xg], is_output=True)
        norm_mod(xg, b_xg, n, 8, 24, lambda c, s=s, n=n: hT[:, c, 128 + s:128 + s + n], b_hT, inplace_f32=True)
        for tb in range(n // 128):
            pl, bpl = next_pb()
            for kc in range(8):
                k.op("pe", lambda e, kc=kc, tb=tb, pl=pl: e.matmul(pl[:, 0:32], lhsT=xg[:, kc, tb * 128:(tb + 1) * 128],
                                                                  rhs=wr[:, kc, :], start=(kc == 0), stop=(kc == 7)),
                     reads=[b_xg, b_wr], writes=[bpl])
            k.op("dve", lambda e, pl=pl: e.tensor_tensor(out=lg[:], in0=pl[:, 0:32], in1=brt[:], op=ALU.add), reads=[bpl, b_br],
                 writes=[b_lg])
            k.op("dve", lambda e: e.max(out=t8[:], in_=lg[:]), reads=[b_lg], writes=[b_t8])
            k.op("dve", lambda e: e.tensor_scalar(out=rwt[:], in0=lg[:], scalar1=t8[:, 3:4], scalar2=None, op0=ALU.is_ge),
                 reads=[b_lg, b_t8], writes=[b_rwt])
            k.op("dve", lambda e: e.tensor_scalar(out=lg[:], in0=lg[:], scalar1=t8[:, 0:1], scalar2=None, op0=ALU.subtract),
                 reads=[b_lg, b_t8], writes=[b_lg])
            k.op("act", lambda e: e.activation(out=lg[:], in_=lg[:], func=AF.Exp), reads=[b_lg], writes=[b_lg])
            k.op("dve", lambda e: e.tensor_tensor(out=rwt[:], in0=rwt[:], in1=lg[:], op=ALU.mult), reads=[b_rwt, b_lg],
                 writes=[b_rwt])
            k.op("dve", lambda e: e.tensor_reduce(out=t8[:, 4:5], in_=rwt[:], axis=AX.X, op=ALU.add), reads=[b_rwt],
                 writes=[b_t8])
            k.op("dve", lambda e: e.reciprocal(out=t8[:, 4:5], in_=t8[:, 4:5]), reads=[b_t8], writes=[b_t8])
            k.op("dve", lambda e: e.tensor_scalar(out=rwt[:], in0=rwt[:], scalar1=t8[:, 4:5], scalar2=None, op0=ALU.mult),
                 reads=[b_rwt, b_t8], writes=[b_rwt])
            r0 = s + tb * 128
            k.dma("sp", o_rw[r0:r0 + 128, :], rwt[:], reads=[b_rwt], is_output=True)
    for (s, n) in groups_of(T, 512):
        k.dma("sp", o_hT[:, :, s:s + n], hT[:, :, 128 + s:128 + s + n], reads=[b_hT], is_output=True)
    k.pop()
    return k.finish()

def build_moe(NT):
    k = KB()
    G = 512
    NG = NT // G
    d_hT = k.dram("hT", [NG, 128, 8, G], BF16, "ExternalInput")
    d_rw = k.dram("rw", [4, NT], F32, "ExternalInput")
    d_w1 = k.dram("w1", [4, 4, 128, 8, 512], F32, "ExternalInput")
    d_w2 = k.dram("w2", [4, 2, 128, 8, 512], F32, "ExternalInput")
    d_b1 = k.dram("b1", [128, 4, 16], F32, "ExternalInput")
    d_b2 = k.dram("b2", [128, 4, 8], F32, "ExternalInput")
    d_sel = k.dram("sel", [4, 4, 128], F32, "ExternalInput")
    o_y = k.dram("o_y", [NG, 128, 8, G], BF16, "ExternalOutput")
    scr = k.dram("yscr", [NG, 128, 8, G], BF16, "Internal")

    ALPHA = 1.702
    C7 = ALPHA * 7.0 / (1.0 + math.exp(-ALPHA * 7.0))
    w1L = [k.sb("w1s%d" % i, [128, 8, 2048], BF16) for i in range(2)]; b_w1L = [Buf(), Buf()]
    w2L = [k.sb("w2s%d" % i, [128, 8, 1024], BF16) for i in range(2)]; b_w2L = [Buf(), Buf()]
    wst = k.sb("wst", [128, 8, 512], F32); b_wst = Buf()
    hg = [k.sb("hg%d" % i, [128, 8, G], BF16) for i in range(2)]; b_hg = [Buf(), Buf()]
    act = [k.sb("act%d" % i, [128, 8, G], BF16) for i in range(2)]; b_act = [Buf(), Buf()]
    ysb = k.sb("ysb", [128, 8, G], F32); b_ysb = Buf()
    ypv = k.sb("ypv", [128, 8, G], BF16); b_ypv = Buf()
    y16 = k.sb("y16", [128, 8, G], BF16); b_y16 = Buf()
    wbc = [k.sb("wbc%d" % i, [128, G], F32) for i in range(2)]; b_wbc = [Buf(), Buf()]
    rwg = [k.sb("rws%d" % i, [4, G], F32) for i in range(2)]; b_rwg = [Buf(), Buf()]
    sel = k.sb("sels", [4, 4, 128], F32); b_sel = Buf()
    b1 = k.sb("b1s", [128, 4, 16], F32); b_b1 = Buf()
    b2 = k.sb("b2s", [128, 4, 8], F32); b_b2 = Buf()
    s1 = [k.sb("s1_%d" % i, [128, G], BF16) for i in range(2)]; b_s1 = [Buf(), Buf()]
    s1c = [k.sb("s1c%d" % i, [128, G], BF16) for i in range(2)]; b_s1c = [Buf(), Buf()]
    tl = [k.sb("tl%d" % i, [128, G], BF16) for i in range(2)]; b_tl = [Buf(), Buf()]
    uu = [k.sb("uu%d" % i, [128, G], BF16) for i in range(2)]; b_uu = [Buf(), Buf()]
    pbank = [k.ps("pb%d" % i, [128, 512]) for i in range(8)]
    b_pb = [Buf() for _ in range(8)]
    b_y = [Buf() for _ in range(NG)]

    k.dma("sp", sel[:], d_sel, writes=[b_sel])
    k.dma("sp", b1[:], d_b1, writes=[b_b1])
    k.dma("sp", b2[:], d_b2, writes=[b_b2])
    k.op("dve", lambda e: e.tensor_scalar(out=b1[:, :, 0:8], in0=b1[:, :, 0:8], scalar1=ALPHA, scalar2=None, op0=ALU.mult),
         reads=[b_b1], writes=[b_b1])
    k.op("dve", lambda e: e.tensor_scalar(out=b1[:, :, 8:16], in0=b1[:, :, 8:16], scalar1=1.0, scalar2=None, op0=ALU.add),
         reads=[b_b1], writes=[b_b1])
    k.op("dve", lambda e: e.tensor_scalar(out=b2[:], in0=b2[:], scalar1=ALPHA, scalar2=None, op0=ALU.mult),
         reads=[b_b2], writes=[b_b2])
    pi = [0]

    def npb(lo, hi):
        i = lo + (pi[0] % (hi - lo))
        pi[0] += 1
        return pbank[i], b_pb[i]

    def stage1(ex, g):
        w1, b_w1 = w1L[ex % 2], b_w1L[ex % 2]
        gi = g % 2
        s = g * G
        k.dma("sp", hg[gi][:], d_hT[g], writes=[b_hg[gi]])
        k.dma("sp", rwg[gi][:], d_rw[:, s:s + G], writes=[b_rwg[gi]])
        pw = pbank[7]
        k.op("pe", lambda e: e.matmul(pw[:, :], lhsT=sel[:, ex, :], rhs=rwg[gi][:], start=True, stop=True),
             reads=[b_sel, b_rwg[gi]], writes=[b_pb[7]])
        k.op("act", lambda e: e.activation(out=wbc[gi][:], in_=pw[:, :], func=AF.Identity, scale=1.0 / ALPHA),
             reads=[b_pb[7]], writes=[b_wbc[gi]])
        for ci in range(8):
            ti = ci % 2
            pg, bpg = npb(0, 3)
            for kc in range(8):
                k.op("pe", lambda e, kc=kc: e.matmul(pg[:, :], lhsT=w1[:, kc, ci * 128:(ci + 1) * 128], rhs=hg[gi][:, kc, :],
                                                     start=(kc == 0), stop=(kc == 7)), reads=[b_w1, b_hg[gi]], writes=[bpg])
            pl, bpl = npb(0, 3)
            for kc in range(8):
                k.op("pe", lambda e, kc=kc: e.matmul(pl[:, :], lhsT=w1[:, kc, 1024 + ci * 128:1024 + (ci + 1) * 128],
                                                     rhs=hg[gi][:, kc, :], start=(kc == 0), stop=(kc == 7)),
                     reads=[b_w1, b_hg[gi]], writes=[bpl])
            k.op("act", lambda e: e.activation(out=s1[ti][:], in_=pg[:, :], func=AF.Silu, scale=ALPHA, bias=b1[:, ex, ci:ci + 1]),
                 reads=[bpg, b_b1], writes=[b_s1[ti]])
            k.op("pool", lambda e: e.tensor_scalar(out=s1c[ti][:], in0=s1[ti][:], scalar1=C7, scalar2=-1.0e30, op0=ALU.min,
                                                   op1=ALU.max), reads=[b_s1[ti]], writes=[b_s1c[ti]])
            k.op("dve", lambda e: e.tensor_scalar(out=tl[ti][:], in0=pl[:, :], scalar1=b1[:, ex, 8 + ci:9 + ci], scalar2=8.0,
                                                  op0=ALU.add, op1=ALU.min), reads=[bpl, b_b1], writes=[b_tl[ti]])
            k.op("dve", lambda e: e.scalar_tensor_tensor(out=uu[ti][:], in0=tl[ti][:], scalar=-6.0, in1=s1c[ti][:],
                                                         op0=ALU.max, op1=ALU.mult), reads=[b_tl[ti], b_s1c[ti]],
                 writes=[b_uu[ti]])
            k.op("pool", lambda e: e.tensor_tensor(out=act[gi][:, ci, :], in0=uu[ti][:], in1=wbc[gi][:], op=ALU.mult),
                 reads=[b_uu[ti], b_wbc[gi]], writes=[b_act[gi]])

    def stage2(ex, g):
        w2, b_w2 = w2L[ex % 2], b_w2L[ex % 2]
        gi = g % 2
        s = g * G
        if ex > 0:
            k.dma("sp", ypv[:], scr[g], reads=[b_y[g]], writes=[b_ypv])
        for f in range(8):
            py, bpy = npb(3, 7)
            for kc in range(8):
                k.op("pe", lambda e, kc=kc: e.matmul(py[:, :], lhsT=w2[:, kc, f * 128:(f + 1) * 128], rhs=act[gi][:, kc, :],
                                                     start=(kc == 0), stop=(kc == 7)), reads=[b_w2, b_act[gi]], writes=[bpy])
            k.op("dve", lambda e: e.scalar_tensor_tensor(out=ysb[:, f, :], in0=wbc[gi][:], scalar=b2[:, ex, f:f + 1], in1=py[:, :],
                                                         op0=ALU.mult, op1=ALU.add), reads=[b_wbc[gi], b_b2, bpy], writes=[b_ysb])
            if ex > 0:
                k.op("pool", lambda e: e.tensor_tensor(out=ysb[:, f, :], in0=ysb[:, f, :], in1=ypv[:, f, :], op=ALU.add),
                     reads=[b_ysb, b_ypv], writes=[b_ysb])
        k.op("act", lambda e: e.activation(out=y16[:], in_=ysb[:], func=AF.Identity), reads=[b_ysb], writes=[b_y16])
        if ex < 3:
            k.dma("sp", scr[g], y16[:], reads=[b_y16], writes=[b_y[g]])
        else:
            k.dma("sp", o_y[g], y16[:], reads=[b_y16], is_output=True)

    def wload(ex, bi):
        if bi < 4:
            k.dma("sp", wst[:], d_w1[ex, bi], writes=[b_wst])
            k.op("pool", lambda e: e.tensor_copy(out=w1L[ex % 2][:, :, bi * 512:(bi + 1) * 512], in_=wst[:]),
                 reads=[b_wst], writes=[b_w1L[ex % 2]])
        else:
            k.dma("sp", wst[:], d_w2[ex, bi - 4], writes=[b_wst])
            k.op("pool", lambda e: e.tensor_copy(out=w2L[ex % 2][:, :, (bi - 4) * 512:(bi - 3) * 512], in_=wst[:]),
                 reads=[b_wst], writes=[b_w2L[ex % 2]])

    for bi in range(6):
        wload(0, bi)
    pre = {2 + 4 * bi: bi for bi in range(6)} if NG >= 24 else {}
    for ex in range(4):
        if ex > 0 and not pre:
            for bi in range(6):
                wload(ex, bi)
        stage1(ex, 0)
        for g in range(NG):
            if g + 1 < NG:
                stage1(ex, g + 1)
            stage2(ex, g)
            if ex + 1 < 4 and g in pre:
                wload(ex + 1, pre[g])
    return k.finish()


def build_resid(T):
    k = KB()
    d_y = k.dram("yp", [8, 128, 8, T], BF16, "ExternalInput")
    d_x = k.dram("xT", [128, 8, T], F32, "ExternalInput")
    d_g = k.dram("gf", [128, 8], F32, "ExternalInput")
    o_x = k.dram("o_x", [128, 8, T], F32, "ExternalOutput")
    G = min(512, T)
    acc = [k.sb("acc%d" % i, [128, 8, G], F32) for i in range(2)]; b_acc = [Buf(), Buf()]
    yb = [k.sb("yb%d" % i, [128, 8, G], BF16) for i in range(3)]; b_yb = [Buf(), Buf(), Buf()]
    xb = [k.sb("xb%d" % i, [128, 8, G], F32) for i in range(2)]; b_xb = [Buf(), Buf()]
    gf = k.sb("gfs", [128, 8], F32); b_gf = Buf()
    k.dma("sp", gf[:], d_g, writes=[b_gf])
    n_y = 0
    for gi, (s, n) in enumerate(groups_of(T, G)):
        a = gi % 2
        k.dma("sp", xb[a][:, :, 0:n], d_x[:, :, s:s + n], writes=[b_xb[a]])
        k.dma("sp", yb[2][:, :, 0:n], d_y[0, :, :, s:s + n], writes=[b_yb[2]])
        k.op("dve", lambda e: e.tensor_copy(out=acc[a][:, :, 0:n], in_=yb[2][:, :, 0:n]), reads=[b_yb[2]], writes=[b_acc[a]])
        for r in range(1, 8):
            yi = n_y % 2
            n_y += 1
            k.dma("sp", yb[yi][:, :, 0:n], d_y[r, :, :, s:s + n], writes=[b_yb[yi]])
            k.op("dve", lambda e: e.tensor_tensor(out=acc[a][:, :, 0:n], in0=acc[a][:, :, 0:n], in1=yb[yi][:, :, 0:n], op=ALU.add),
                 reads=[b_acc[a], b_yb[yi]], writes=[b_acc[a]])
        for c in range(8):
            k.op("dve", lambda e, c=c: e.scalar_tensor_tensor(out=xb[a][:, c, 0:n], in0=acc[a][:, c, 0:n], scalar=gf[:, c:c + 1],
                                                             in1=xb[a][:, c, 0:n], op0=ALU.mult, op1=ALU.add),
                 reads=[b_acc[a], b_gf, b_xb[a]], writes=[b_xb[a]])
        k.dma("sp", o_x[:, :, s:s + n], xb[a][:, :, 0:n], reads=[b_xb[a]], is_output=True)
    return k.finish()

def _tile_w(W):
    K_, N_ = W.shape
    return np.ascontiguousarray(W.reshape(K_ // 128, 128, N_).transpose(1, 0, 2))


def _vec(v, nch):
    return np.ascontiguousarray(np.asarray(v).reshape(nch, 128).T)


def _t5_bucket(dist):
    dist = np.asarray(dist)
    lr = np.log(np.maximum(dist, 1).astype(np.float32) / np.float32(16)) / np.float32(math.log(128 / 16))
    large = 16 + (lr * np.float32(16)).astype(np.int32)
    return np.where(dist < 16, dist, np.minimum(large, 31))


def _run(nc, maps):
    res = run_bass_kernel_spmd(nc, maps, core_ids=list(range(len(maps))))
    return res.results


_DEBUG = {}


def kernel(x, c, w_ada, b_ada, norm_mix, norm_ffn, w_in, conv_w, conv_b, lru_wa, lru_ba, lru_wx, lru_bx, lru_lambda,
           diff_qnorm, diff_knorm, diff_lambda, diff_subln, swa_qnorm, swa_knorm, swa_sinks, rel_bias, w_branch, w_out,
           w_router, b_router, w1, b1, w2, b2):
    f32 = np.float32
    x = np.asarray(x, f32)
    Bn, S, _ = x.shape
    NC = 8
    CPB = NC // Bn
    T = S // CPB
    NBLK = T // 128
    NT = Bn * S
    depth = w_ada.shape[0]
    bf = ml_dtypes.bfloat16
    ident = np.eye(128, dtype=f32)
    bdones = np.zeros((128, 128), f32)
    bdones[:64, :64] = 1.0
    bdones[64:, 64:] = 1.0
    rel_bias = np.asarray(rel_bias, f32)
    qq = np.arange(128)[None, :]
    kk = np.arange(128)[:, None]
    dbias = np.zeros((4, 128, 2, 2, 128), f32)
    d0 = qq - kk
    d1 = 128 + qq - kk
    for h in range(4):
        diag = np.where(d0 >= 0, rel_bias[_t5_bucket(np.maximum(d0, 0)), h], f32(NEG))
        prev = rel_bias[_t5_bucket(d1), h]
        for m in range(2):
            dbias[h, :, 0, m, :] = diag
            dbias[h, :, 1, m, :] = prev
    b31 = np.ascontiguousarray(np.broadcast_to(rel_bias[31, 0:4][None, :], (128, 4))).astype(f32)
    sbias = np.zeros((128, 8, 2, 128), f32)
    for hd in range(8):
        sbias[:, hd, 0, :] = np.where(d1 < 128, rel_bias[_t5_bucket(np.clip(d1, 0, 127)), 4 + hd], f32(NEG))
        sbias[:, hd, 1, :] = np.where(d0 >= 0, rel_bias[_t5_bucket(np.clip(d0, 0, 127)), 4 + hd], f32(NEG))
    sel = np.zeros((4, 4, 128), f32)
    for e in range(4):
        sel[e, e, :] = 1.0
    perm = np.concatenate([np.concatenate([np.arange(cc * 64, (cc + 1) * 64), np.arange((cc + 4) * 64, (cc + 5) * 64)])
                           for cc in range(4)])

    xcur = x.reshape(NT, 1024)
    for l in range(depth):
        lam_init = 0.8 - 0.6 * math.exp(-0.3 * l)
        W = np.asarray(w_in[l], f32)
        blk6 = np.concatenate([W[:, 3072:3328], np.zeros((1024, 256), f32)], axis=1)
        win = np.stack([_tile_w(W[:, 0:512]), _tile_w(W[:, 512:1024]), _tile_w(W[:, 1024:1536]), _tile_w(W[:, 1536:2048]),
                        _tile_w(W[:, 2048:2560]), _tile_w(W[:, 2560:3072][:, perm]), _tile_w(blk6)])
        wada_full = np.stack([_tile_w(np.asarray(w_ada[l], f32)[:, i * 512:(i + 1) * 512]) for i in range(12)])
        bada = _vec(b_ada[l], 48)
        convw = np.ascontiguousarray(np.asarray(conv_w[l], f32).reshape(4, 4, 128).transpose(2, 1, 0))
        lrus = np.stack([_vec(conv_b[l], 4), _vec(lru_ba[l], 4), _vec(lru_bx[l], 4), _vec(lru_lambda[l], 4)], axis=2)
        wlru = np.zeros((128, 2, 512), f32)
        for wi, Wl in enumerate((lru_wa[l], lru_wx[l])):
            Wl = np.asarray(Wl, f32)
            for cc in range(4):
                for hb in range(2):
                    wlru[hb * 64:(hb + 1) * 64, wi, cc * 128 + hb * 64:cc * 128 + (hb + 1) * 64] = Wl[2 * cc + hb]
        qkg = np.stack([np.tile(np.asarray(g[l], f32), 2) for g in (diff_qnorm, diff_knorm, swa_qnorm, swa_knorm)], axis=1)
        common = {"wada": None, "bada": bada, "nmix": _vec(norm_mix[l], 8), "win": win, "convw": convw,
                  "lrus": np.ascontiguousarray(lrus), "wlru": wlru, "qkg": np.ascontiguousarray(qkg), "ident": ident,
                  "bdones": bdones}

        def xT_of(core):
            b, j = divmod(core, CPB)
            start = b * S + j * T
            xs = np.zeros((T + 128, 1024), f32)
            xs[128:] = xcur[start:start + T]
            if j > 0:
                xs[:128] = xcur[start - 128:start]
            return np.ascontiguousarray(xs.T.reshape(8, 128, T + 128).transpose(1, 0, 2))

        def base_map(core, full):
            b, j = divmod(core, CPB)
            m = dict(common)
            m["wada"] = wada_full if full else np.ascontiguousarray(wada_full[:4])
            m["xT"] = xT_of(core)
            m["cvec"] = _vec(np.asarray(c, f32)[b], 8)
            m["flag"] = np.full((128, 1), 1.0 if j > 0 else 0.0, f32)
            return m

        ncA = build_mixer("A", T)
        resA = _run(ncA, [base_map(core, False) for core in range(NC)])
        Wg = W[:, 3328:6400]
        wgl = np.stack([_tile_w(np.concatenate([Wg[:, n * 1024 + f * 128:n * 1024 + (f + 1) * 128] for n in range(3)], axis=1))
                        for f in range(8)])
        Wb = np.asarray(w_branch[l], f32)
        wbr = np.stack([np.concatenate([_tile_w(Wb[n][:, f * 128:(f + 1) * 128]) for n in range(3)], axis=1) for f in range(8)])
        Wo = np.asarray(w_out[l], f32)
        wout = np.stack([_tile_w(Wo[:, 0:512]), _tile_w(Wo[:, 512:1024])])
        extraB = {"nffn": _vec(norm_ffn[l], 8), "dbias": dbias, "b31": b31, "sbias": sbias,
                  "sinks": np.ascontiguousarray(np.broadcast_to(np.asarray(swa_sinks[l], f32)[None, :], (128, 8))),
                  "dlam": np.ascontiguousarray(np.broadcast_to(np.asarray(diff_lambda[l], f32)[None], (128, 4, 64))),
                  "subln": np.ascontiguousarray(np.broadcast_to(np.asarray(diff_subln[l], f32)[None, :], (128, 128))),
                  "lamc": np.ascontiguousarray(np.broadcast_to(np.array([[-lam_init, 1.0 - lam_init]], f32), (128, 2))),
                  "wgl": wgl, "wbr": wbr, "wout": wout, "wr": _tile_w(np.asarray(w_router[l], f32)),
                  "br": np.ascontiguousarray(np.broadcast_to(np.asarray(b_router[l], f32)[None, :], (128, 32)))}
        mapsB = []
        for core in range(NC):
            b, j = divmod(core, CPB)
            m = base_map(core, True)
            m.update(extraB)
            kprev = np.zeros((4, 128, 3 * T), bf)
            vprev = np.zeros((4, 128, 3 * NBLK, 130), bf)
            carry = np.zeros((128, 4, 3, 2), f32)
            carry[:, :, :, 0] = 1.0
            for q3 in range(3):
                jj = j - 3 + q3
                if jj >= 0:
                    r = resA[b * CPB + jj]
                    kprev[:, :, q3 * T:(q3 + 1) * T] = r["o_k"]
                    vprev[:, :, q3 * NBLK:(q3 + 1) * NBLK, :] = r["o_v"]
                    carry[:, :, q3, :] = r["o_c"]
            m["kprev"], m["vprev"], m["carry"] = kprev, vprev, carry
            mapsB.append(m)
        dbg = bool(_DEBUG.get("on"))
        ncB = build_mixer("B", T, debug=dbg)
        resB = _run(ncB, mapsB)
        if dbg:
            _DEBUG.setdefault("A", []).append(resA)
            _DEBUG.setdefault("B", []).append(resB)
        hT_all = np.concatenate([r["o_hT"] for r in resB], axis=2)
        hT_all = np.ascontiguousarray(hT_all.reshape(128, 8, NT // 512, 512).transpose(2, 0, 1, 3))
        rw_all = np.concatenate([r["o_rw"] for r in resB], axis=0)
        mapsE = []
        for ec in range(NC):
            es = range(4 * ec, 4 * ec + 4)
            W1 = [np.asarray(w1[l, e], f32) for e in es]
            w1t = np.stack([np.stack([_tile_w(Wx[:, 0::2][:, 0:512]), _tile_w(Wx[:, 0::2][:, 512:1024]),
                                      _tile_w(Wx[:, 1::2][:, 0:512]), _tile_w(Wx[:, 1::2][:, 512:1024])]) for Wx in W1])
            W2 = [np.asarray(w2[l, e], f32) for e in es]
            w2t = np.stack([np.stack([_tile_w(Wx[:, 0:512]), _tile_w(Wx[:, 512:1024])]) for Wx in W2])
            b1t = np.stack([np.concatenate([_vec(np.asarray(b1[l, e], f32)[0::2], 8), _vec(np.asarray(b1[l, e], f32)[1::2], 8)],
                                           axis=1) for e in es], axis=1)
            b2t = np.stack([_vec(b2[l, e], 8) for e in es], axis=1)
            mapsE.append({"hT": hT_all, "rw": np.ascontiguousarray(rw_all[:, 4 * ec:4 * ec + 4].T), "w1": w1t, "w2": w2t,
                          "b1": np.ascontiguousarray(b1t), "b2": np.ascontiguousarray(b2t), "sel": sel})
        resE = _run(build_moe(NT), mapsE)
        if dbg:
            _DEBUG.setdefault("E", []).append(resE)
        mapsR = []
        yfull = [np.asarray(resE[r]["o_y"]).transpose(1, 2, 0, 3).reshape(128, 8, NT) for r in range(NC)]
        for core in range(NC):
            yp = np.stack([yfull[r][:, :, core * T:(core + 1) * T] for r in range(NC)])
            mapsR.append({"yp": np.ascontiguousarray(yp), "xT": resB[core]["o_xT"], "gf": resB[core]["o_gf"]})
        resR = _run(build_resid(T), mapsR)
        if _DEBUG.get("depth") == l + 1:
            depth_stop = True
        else:
            depth_stop = False
        xcur = np.concatenate([np.ascontiguousarray(r["o_x"].transpose(2, 1, 0)).reshape(T, 1024) for r in resR], axis=0)
        if depth_stop:
            break
    return np.ascontiguousarray(xcur.reshape(Bn, S, 1024).astype(f32))
```
